# Optimizing a Trainium2 kernel written in Bass

```python
import math
import jax, jax.numpy as jnp
from jax import lax
import numpy as np

D_MODEL = 4096
BATCH = 4
SEQ = 4096
DEPTH = 1

ATTN_HEADS = 16
ATTN_KV_HEADS = 4
ATTN_HEAD_DIM = 128
WINDOW = 128
ATTN_BLOCK = 128
N_BUCKETS = 32
MAX_DISTANCE = 128
RET_HEADS = 8
RET_QK_DIM = 128
RET_V_DIM = 256
RET_CHUNK = 128
ROPE_BASE = 10000.0
N_EXPERTS = 32
TOP_K = 4
D_FF = 1024
SWIGLU_LIMIT = 7.0
SWIGLU_ALPHA = 1.702

EPS = 1e-6
NEG_INF = -1e30

ATTN_Q_W = ATTN_HEADS * ATTN_HEAD_DIM
ATTN_KV_W = ATTN_KV_HEADS * ATTN_HEAD_DIM
RET_QK_W = RET_HEADS * RET_QK_DIM
RET_V_W = RET_HEADS * RET_V_DIM
PROJ_WIDTHS = (ATTN_Q_W, ATTN_KV_W, ATTN_KV_W, RET_QK_W, RET_QK_W, RET_V_W, RET_V_W, D_MODEL, D_MODEL)
IN_PROJ_W = sum(PROJ_WIDTHS)
N_ADA = 6

kernel_name = "hybrid_gated_swa_retention_moe"


def rms_norm(x, w):
    xf = x.astype(jnp.float32)
    return xf * lax.rsqrt(jnp.mean(xf * xf, axis=-1, keepdims=True) + EPS) * w.astype(jnp.float32)


def t5_bucket(rel):
    nb = N_BUCKETS // 2
    max_exact = nb // 2
    base = jnp.where(rel > 0, nb, 0)
    n = jnp.abs(rel)
    nf = jnp.maximum(n, 1).astype(jnp.float32)
    large = max_exact + (jnp.log(nf / max_exact) / math.log(MAX_DISTANCE / max_exact) * (nb - max_exact)).astype(jnp.int32)
    large = jnp.minimum(large, nb - 1)
    return base + jnp.where(n < max_exact, n, large)


def windowed_gqa_attention(q, k, v, bias_table, sink, q_norm_w, k_norm_w):
    B, S = q.shape[0], q.shape[1]
    blk = ATTN_BLOCK
    nb = S // blk
    G = ATTN_HEADS // ATTN_KV_HEADS
    side = -(-WINDOW // blk)
    nkb = 2 * side + 1
    q = rms_norm(q, q_norm_w) * (ATTN_HEAD_DIM ** -0.5)
    k = rms_norm(k, k_norm_w)
    v = v.astype(jnp.float32)
    qb = q.reshape(B, nb, blk, ATTN_KV_HEADS, G, ATTN_HEAD_DIM)
    pad = ((0, 0), (side * blk, side * blk), (0, 0), (0, 0))
    kp = jnp.pad(k, pad).reshape(B, nb + 2 * side, blk, ATTN_KV_HEADS, ATTN_HEAD_DIM)
    vp = jnp.pad(v, pad).reshape(B, nb + 2 * side, blk, ATTN_KV_HEADS, ATTN_HEAD_DIM)
    kb = jnp.concatenate([kp[:, o:o + nb] for o in range(nkb)], axis=2)
    vb = jnp.concatenate([vp[:, o:o + nb] for o in range(nkb)], axis=2)
    logits = jnp.einsum('bnqhgd,bnkhd->bnhgqk', qb, kb)
    qi = jnp.arange(blk, dtype=jnp.int32)[:, None]
    kj = jnp.arange(nkb * blk, dtype=jnp.int32)[None, :]
    rel = kj - side * blk - qi
    bias = bias_table[t5_bucket(rel)].astype(jnp.float32)
    bias = jnp.transpose(bias, (2, 0, 1)).reshape(ATTN_KV_HEADS, G, blk, nkb * blk)
    key_pos = jnp.arange(nb, dtype=jnp.int32)[:, None] * blk + kj - side * blk
    in_range = (key_pos >= 0) & (key_pos < S)
    valid = (jnp.abs(rel) <= WINDOW)[None] & in_range[:, None, :]
    logits = jnp.where(valid[None, :, None, None], logits + bias, NEG_INF)
    sink_l = sink.astype(jnp.float32).reshape(1, 1, ATTN_KV_HEADS, G, 1, 1)
    m = jnp.maximum(jnp.max(logits, axis=-1, keepdims=True), sink_l)
    p = jnp.exp(logits - m)
    denom = jnp.sum(p, axis=-1, keepdims=True) + jnp.exp(sink_l - m)
    out = jnp.einsum('bnhgqk,bnkhd->bnqhgd', p / denom, vb)
    return out.reshape(B, S, ATTN_Q_W)


def rope(x, pos):
    d = x.shape[-1]
    half = d // 2
    inv = ROPE_BASE ** (-jnp.arange(0, d, 2, dtype=jnp.float32) / d)
    ang = pos[:, None] * inv[None, :]
    cos = jnp.cos(ang)[:, None, :]
    sin = jnp.sin(ang)[:, None, :]
    x1, x2 = x[..., :half], x[..., half:]
    return jnp.concatenate([x1 * cos - x2 * sin, x1 * sin + x2 * cos], axis=-1)


def retention_direction(q, k, v, log_gamma, strict):
    B, S, H, dk = q.shape
    dv = v.shape[-1]
    C = RET_CHUNK
    nc = S // C
    qc = q.reshape(B, nc, C, H, dk)
    kc = k.reshape(B, nc, C, H, dk)
    vc = v.reshape(B, nc, C, H, dv)
    i = jnp.arange(C, dtype=jnp.int32)
    diff = (i[:, None] - i[None, :]).astype(jnp.float32)
    mask = (i[:, None] > i[None, :]) if strict else (i[:, None] >= i[None, :])
    decay = jnp.where(mask[None], jnp.exp(jnp.where(mask, diff, 0.0)[None] * log_gamma[:, None, None]), 0.0)
    scores = jnp.einsum('bnihd,bnjhd->bnhij', qc, kc) * decay
    intra = jnp.einsum('bnhij,bnjhe->bnihe', scores, vc)
    fi = i.astype(jnp.float32)
    k_decay = jnp.exp((C - 1 - fi)[:, None] * log_gamma[None, :])
    q_decay = jnp.exp((fi + 1.0)[:, None] * log_gamma[None, :])
    kv = jnp.einsum('bnjhd,bnjhe->nbhde', kc * k_decay[:, :, None], vc)
    chunk_decay = jnp.exp(C * log_gamma)[:, None, None]

    def step(state, kv_n):
        return state * chunk_decay + kv_n, state

    _, prev = lax.scan(step, jnp.zeros_like(kv[0]), kv)
    cross = jnp.einsum('bnihd,nbhde->bnihe', qc * q_decay[:, :, None], prev)
    return (intra + cross).reshape(B, S, H, dv)


def retention_branch(q, k, v, g, decay_fwd, decay_bwd, gn_w, gn_b):
    B, S = q.shape[0], q.shape[1]
    pos = jnp.arange(S, dtype=jnp.float32)
    q = rope(q.astype(jnp.float32), pos)
    k = rope(k.astype(jnp.float32), pos) * (RET_QK_DIM ** -0.5)
    v = v.astype(jnp.float32)
    lg_f = -jnp.exp(decay_fwd.astype(jnp.float32))
    lg_b = -jnp.exp(decay_bwd.astype(jnp.float32))
    y_f = retention_direction(q, k, v, lg_f, strict=False)
    y_b = retention_direction(q[:, ::-1], k[:, ::-1], v[:, ::-1], lg_b, strict=True)[:, ::-1]
    y = y_f + y_b
    mu = jnp.mean(y, axis=-1, keepdims=True)
    var = jnp.mean(jnp.square(y - mu), axis=-1, keepdims=True)
    y = ((y - mu) * lax.rsqrt(var + EPS)).reshape(B, S, RET_V_W)
    y = y * gn_w.astype(jnp.float32) + gn_b.astype(jnp.float32)
    return jax.nn.silu(g.astype(jnp.float32)) * y


def moe_ffn(h, router_w, router_b, w1, b1, w2, b2):
    B, S, D = h.shape
    t = h.reshape(B * S, D)
    logits = (t @ router_w + router_b).astype(jnp.float32)
    top_v, top_i = lax.top_k(logits, TOP_K)
    top_w = jax.nn.softmax(top_v, axis=-1)
    combine = jnp.einsum('tk,tke->te', top_w, jax.nn.one_hot(top_i, N_EXPERTS, dtype=jnp.float32))
    y = jnp.zeros((B * S, D), jnp.float32)
    for e in range(N_EXPERTS):
        hh = t @ w1[e] + b1[e]
        gate = jnp.minimum(hh[:, :D_FF], SWIGLU_LIMIT)
        up = jnp.clip(hh[:, D_FF:], -SWIGLU_LIMIT, SWIGLU_LIMIT)
        act = gate * jax.nn.sigmoid(SWIGLU_ALPHA * gate) * (up + 1.0)
        y = y + combine[:, e:e + 1] * (act @ w2[e] + b2[e])
    return y.reshape(B, S, D)


def setup_inputs(seed: int = 0) -> dict:
    key = jax.random.key(seed)
    ks = jax.random.split(key, 24)
    f32 = jnp.float32
    D, L, E, F = D_MODEL, DEPTH, N_EXPERTS, D_FF
    nrm = lambda k, shape, s: jax.random.normal(k, shape, f32) * s
    h_idx = jnp.arange(RET_HEADS, dtype=f32)
    base_decay = jnp.log(-jnp.log1p(-(2.0 ** (-5.0 - h_idx))))
    return {
        "x": nrm(ks[0], (BATCH, SEQ, D), 1.0),
        "c": nrm(ks[1], (BATCH, D), 1.0),
        "rel_bias": nrm(ks[2], (N_BUCKETS, ATTN_HEADS), 0.5),
        "ada_w": nrm(ks[3], (L, D, N_ADA * D), 0.5 * D ** -0.5),
        "ada_b": nrm(ks[4], (L, N_ADA * D), 0.02),
        "norm_mix_w": 1.0 + nrm(ks[5], (L, D), 0.02),
        "w_in": nrm(ks[6], (L, D, IN_PROJ_W), D ** -0.5),
        "q_norm_w": 1.0 + nrm(ks[7], (L, ATTN_HEAD_DIM), 0.02),
        "k_norm_w": 1.0 + nrm(ks[8], (L, ATTN_HEAD_DIM), 0.02),
        "attn_sink": nrm(ks[9], (L, ATTN_HEADS), 0.5),
        "ret_decay_fwd": base_decay[None, :] + nrm(ks[10], (L, RET_HEADS), 0.05),
        "ret_decay_bwd": base_decay[None, :] + nrm(ks[11], (L, RET_HEADS), 0.05),
        "ret_gn_w": 1.0 + nrm(ks[12], (L, RET_V_W), 0.02),
        "ret_gn_b": nrm(ks[13], (L, RET_V_W), 0.02),
        "w_up_attn": nrm(ks[14], (L, ATTN_Q_W, D), ATTN_Q_W ** -0.5),
        "w_up_ret": nrm(ks[15], (L, RET_V_W, D), RET_V_W ** -0.5),
        "w_out": nrm(ks[16], (L, D, D), D ** -0.5),
        "norm_ffn_w": 1.0 + nrm(ks[17], (L, D), 0.02),
        "router_w": nrm(ks[18], (L, D, E), D ** -0.5),
        "router_b": nrm(ks[19], (L, E), 0.01),
        "expert_w1": nrm(ks[20], (L, E, D, 2 * F), D ** -0.5),
        "expert_b1": nrm(ks[21], (L, E, 2 * F), 0.01),
        "expert_w2": nrm(ks[22], (L, E, F, D), F ** -0.5),
        "expert_b2": nrm(ks[23], (L, E, D), 0.01),
    }


def reference(x, c, rel_bias, ada_w, ada_b, norm_mix_w, w_in, q_norm_w, k_norm_w, attn_sink,
              ret_decay_fwd, ret_decay_bwd, ret_gn_w, ret_gn_b, w_up_attn, w_up_ret, w_out,
              norm_ffn_w, router_w, router_b, expert_w1, expert_b1, expert_w2, expert_b2):
    B, S, D = x.shape
    split_idx = [int(v) for v in np.cumsum(PROJ_WIDTHS)[:-1]]
    cs = jax.nn.silu(c)
    for l in range(DEPTH):
        mod = (cs @ ada_w[l] + ada_b[l])[:, None, :]
        sh_m, sc_m, g_m, sh_f, sc_f, g_f = jnp.split(mod, N_ADA, axis=-1)
        h = rms_norm(x, norm_mix_w[l]) * (1.0 + sc_m) + sh_m
        proj = h @ w_in[l]
        aq, ak, av, rq, rk, rv, rg, ga, gr = jnp.split(proj, split_idx, axis=-1)
        attn = windowed_gqa_attention(
            aq.reshape(B, S, ATTN_HEADS, ATTN_HEAD_DIM),
            ak.reshape(B, S, ATTN_KV_HEADS, ATTN_HEAD_DIM),
            av.reshape(B, S, ATTN_KV_HEADS, ATTN_HEAD_DIM),
            rel_bias, attn_sink[l], q_norm_w[l], k_norm_w[l])
        ret = retention_branch(
            rq.reshape(B, S, RET_HEADS, RET_QK_DIM),
            rk.reshape(B, S, RET_HEADS, RET_QK_DIM),
            rv.reshape(B, S, RET_HEADS, RET_V_DIM),
            rg, ret_decay_fwd[l], ret_decay_bwd[l], ret_gn_w[l], ret_gn_b[l])
        merged = jax.nn.sigmoid(ga) * (attn @ w_up_attn[l]) + jax.nn.sigmoid(gr) * (ret @ w_up_ret[l])
        x = x + g_m * (merged @ w_out[l])
        h = rms_norm(x, norm_ffn_w[l]) * (1.0 + sc_f) + sh_f
        x = x + g_f * moe_ffn(h, router_w[l], router_b[l], expert_w1[l], expert_b1[l], expert_w2[l], expert_b2[l])
    return x
```

```python
import math
from contextlib import ExitStack
import numpy as np
import concourse.bass as bass
import concourse.mybir as mybir
from concourse.bass_utils import run_bass_kernel_spmd

F32 = mybir.dt.float32
BF16 = mybir.dt.bfloat16
AF = mybir.ActivationFunctionType
ALU = mybir.AluOpType
AX = mybir.AxisListType

D = 4096
SEQ = 4096
TOK = 2048
KC = 32
NEXP = 32
DFF = 1024
EPS = 1e-6
PW = (2048, 512, 512, 1024, 1024, 2048, 2048, 4096, 4096)
OFF = [0]
for _w in PW:
    OFF.append(OFF[-1] + _w)
(O_AQ, O_AK, O_AV, O_RQ, O_RK, O_RV, O_RG, O_GA, O_GR, O_END) = OFF


class Buf:
    __slots__ = ("name", "writers", "readers", "dsems")

    def __init__(self, name):
        self.name = name
        self.writers = {}
        self.readers = {}
        self.dsems = {}


class Eng:
    def __init__(self, name, eng, sem, inorder, self_sync):
        self.name = name
        self.eng = eng
        self.sem = sem
        self.count = 0
        self.seen = {}
        self.inorder = inorder
        self.self_sync = self_sync
        self.pending = False


class Sched:
    def __init__(self, nc, stack):
        self.nc = nc
        self.stack = stack
        mk = lambda n: stack.enter_context(nc.semaphore(n))
        self.pe = Eng("pe", nc.tensor, mk("s_pe"), True, False)
        self.act = Eng("act", nc.scalar, mk("s_act"), True, True)
        self.dve = Eng("dve", nc.vector, mk("s_dve"), True, True)
        self.pool = Eng("pool", nc.gpsimd, mk("s_pool"), True, True)
        self.sp = Eng("sp", nc.sync, None, False, False)
        self.engs = [self.pe, self.act, self.dve, self.pool, self.sp]
        self.dma_bufs = []
        self.free_dsems = {}
        self.dma_owners = []
        self.nwaits = 0
        self.nins = 0

    def _wait(self, E, need):
        for sem, val in need.items():
            if E.seen.get(sem, 0) < val:
                E.eng.wait_ge(sem, val)
                E.seen[sem] = val
                self.nwaits += 1

    def _need(self, E, reads, writes):
        need = {}

        def merge(d, skip_self):
            for s, v in d.items():
                if skip_self and s is E.sem:
                    continue
                if need.get(s, 0) < v:
                    need[s] = v
        for b in reads:
            merge(b.writers, not E.self_sync)
        for b in writes:
            merge(b.writers, not E.self_sync)
            merge(b.readers, E.inorder)
        return need

    def _record(self, tok, reads, writes):
        s, v = tok
        for b in reads:
            if b.readers.get(s, 0) < v:
                b.readers[s] = v
        for b in writes:
            if b.writers.get(s, 0) < v:
                b.writers[s] = v
            b.readers = {}

    def op(self, E, emit, reads=(), writes=(), signal=True):
        self._wait(E, self._need(E, reads, writes))
        ins = emit()
        self.nins += 1
        if signal:
            E.count += 1
            ins.then_inc(E.sem, 1)
            E.pending = False
            tok = (E.sem, E.count)
        else:
            E.pending = True
            tok = (E.sem, E.count + 1)
        self._record(tok, reads, writes)
        return ins

    def dma(self, E, out, in_, owner, reads=(), writes=(), **kw):
        self._wait(E, self._need(E, reads, writes))
        ent = owner.dsems.get(E.name)
        if ent is None:
            fl = self.free_dsems.setdefault(E.name, [])
            if fl:
                ent = fl.pop()
            else:
                ent = [self.stack.enter_context(self.nc.semaphore(f"d{len(self.dma_bufs)}_{E.name}")), 0]
                self.dma_bufs.append(ent)
            owner.dsems[E.name] = ent
            self.dma_owners.append(owner)
        ins = E.eng.dma_start(out=out, in_=in_, **kw)
        ins.then_inc(ent[0], 16)
        ent[1] += 16
        self.nins += 1
        self._record((ent[0], ent[1]), reads, writes)
        return ins

    def barrier(self):
        assert not self.pe.pending
        need = {}
        for E in self.engs:
            if E.sem is not None and E.count:
                need[E.sem] = E.count
        for ent in self.dma_bufs:
            if ent[1]:
                need[ent[0]] = ent[1]
        for E in self.engs:
            self._wait(E, need)
        for o in self.dma_owners:
            for qn, ent in o.dsems.items():
                self.free_dsems.setdefault(qn, []).append(ent)
            o.dsems = {}
        self.dma_owners = []


class K:
    def set_half(self, hf):
        self.hf = hf
        h = self.hin[hf]
        self.x_own, self.x_oth = h["x_own"], h["x_oth"]
        self.cosq, self.sinq, self.cosk, self.sink_ = h["cosq"], h["sinq"], h["cosk"], h["sink"]
        self.biasT, self.dec_f, self.dec_b, self.out = h["biasT"], h["dec_f"], h["dec_b"], h["out"]
        for nm in ("x_own", "x_oth", "out"):
            self.dbufs[nm] = self.dbufs[f"{nm}{hf}"]

    def __init__(self, stop_after=99, debug=(), from_phase=1, ext_in=(), halves=(0, 1), skip=(), n_exp=NEXP):
        self.halves = tuple(halves)
        self.skip = set(skip)
        self._n_exp = n_exp
        self.from_phase = from_phase
        self.ext_in = set(ext_in)
        self.n_exp = self._n_exp
        self.stop_after = stop_after
        self.debug = set(debug)
        self.nc = bass.Bass("TRN2", target_bir_lowering=False)
        self.inputs = {}
        self.dbufs = {}

    USED = {"x_own": (3, 7), "x_oth": (2, 2), "cosq": (3, 3), "sinq": (3, 3), "cosk": (2, 3), "sink": (2, 3),
            "biasT": (4, 4), "dec_f": (5, 5), "dec_b": (5, 5), "cT": (1, 1), "ada_w": (1, 1), "ada_b": (1, 1),
            "nw_m": (1, 1), "nw_f": (1, 1), "w_in": (2, 3), "sinkr": (4, 4), "cpos": (5, 5), "cneg": (5, 5),
            "ccol": (5, 5), "crow": (5, 5), "gnw": (5, 5), "gnb": (5, 5), "w_ua": (6, 6), "w_ur": (6, 6),
            "w_out": (7, 7), "router_w": (8, 8), "rb": (8, 8), "w1": (9, 9), "b1T": (9, 9), "w2": (9, 9), "b2": (9, 9)}

    def inp(self, name, shape, dt=F32):
        base = name.rstrip("01") if name[:-1] in self.USED else name
        lo, hi = self.USED.get(base, (0, 99))
        if hi < self.from_phase or lo > self.stop_after:
            self.dbufs[name] = Buf(name)
            return self.nc.dram_tensor(name, [1] * len(shape), dt, kind="Internal").ap()
        t = self.nc.dram_tensor(name, list(shape), dt, kind="ExternalInput").ap()
        self.inputs[name] = (tuple(shape), dt)
        self.dbufs[name] = Buf(name)
        return t

    def scratch(self, name, shape, dt):
        kind = "ExternalOutput" if name in self.debug else "Internal"
        if name in self.ext_in:
            kind = "ExternalInput"
            self.inputs[name] = (tuple(shape), dt)
        t = self.nc.dram_tensor(name, list(shape), dt, kind=kind).ap()
        self.dbufs[name] = Buf(name)
        return t

    def sb(self, st, name, shape, dt):
        self._uid = getattr(self, "_uid", 0) + 1
        return st.enter_context(self.nc.sbuf_tensor(f"{name}_{self._uid}", list(shape), dt))

    def mm(self, ps_ap, psb, pairs, reads):
        S, nc = self.S, self.nc
        n = len(pairs)
        for i, (l, r) in enumerate(pairs):
            S.op(S.pe, lambda l=l, r=r, i=i: nc.tensor.matmul(ps_ap, lhsT=l, rhs=r, start=(i == 0), stop=(i == n - 1)),
                 reads=reads, writes=[psb], signal=(i == n - 1))

    def build(self):
        nc = self.nc
        with ExitStack() as gst:
            self.S = S = Sched(nc, gst)
            self.gst = gst
            self.declare()
            self.consts()
            phases = [self.p1_mod, self.p2_other, self.p3_own, self.p4_attn, self.p5_ret,
                      self.p6_up, self.p7_out, self.p8_norm2, self.p9_moe]
            for hi, hf in enumerate(self.halves):
                self.set_half(hf)
                for i, ph in enumerate(phases):
                    if i + 1 > self.stop_after:
                        break
                    if i + 1 < self.from_phase or (i == 0 and hi > 0) or (i + 1) in self.skip:
                        continue
                    ph()
                    S.barrier()
            self.finish()
        return nc

    def declare(self):
        nc = self.nc
        I = self.inp
        self.hin = [{}, {}]
        for hf in self.halves:
            for nm, shp in (("x_own", [TOK, D]), ("x_oth", [TOK, D]), ("cosq", [128, TOK]), ("sinq", [128, TOK]),
                            ("cosk", [128, SEQ]), ("sink", [128, SEQ]), ("biasT", [4, 128, 1536]),
                            ("dec_f", [128, 8]), ("dec_b", [128, 8])):
                self.hin[hf][nm] = I(f"{nm}{hf}", shp)
            self.hin[hf]["out"] = self.nc.dram_tensor(f"out{hf}", [TOK, D], F32, kind="ExternalOutput").ap()
            self.dbufs[f"out{hf}"] = Buf(f"out{hf}")
        self.cT = I("cT", [128, KC])
        self.ada_w = I("ada_w", [D, 6 * D])
        self.ada_b = I("ada_b", [1, 6 * D])
        self.nw_m = I("nw_m", [128, KC])
        self.nw_f = I("nw_f", [128, KC])
        self.w_in = I("w_in", [D, O_END])
        self.qnw = I("qnw", [128, 1])
        self.knw = I("knw", [128, 1])
        self.rperm = I("rperm", [128, 128])
        self.ident = I("ident", [128, 128])
        self.sinkr = I("sinkr", [128, 16])
        self.cpos = I("cpos", [128, 128])
        self.cneg = I("cneg", [128, 128])
        self.ccol = I("ccol", [128, 2])
        self.crow = I("crow", [128, 256])
        self.gnw = I("gnw", [128, 2048])
        self.gnb = I("gnb", [128, 2048])
        self.w_ua = I("w_ua", [2048, D])
        self.w_ur = I("w_ur", [2048, D])
        self.w_out = I("w_out", [D, D])
        self.router_w = I("router_w", [D, NEXP])
        self.rb = I("rb", [128, NEXP])
        self.w1 = I("w1", [self.n_exp, D, 2 * DFF])
        self.b1T = I("b1T", [128, self.n_exp * 16])
        self.w2 = I("w2", [self.n_exp, DFF, D])
        self.b2 = I("b2", [NEXP, D])
        Sc = self.scratch
        self.mrg = Sc("mrg", [D, TOK], BF16)
        self.x1 = Sc("x1", [TOK, D], F32)
        self.h2d = Sc("h2d", [128, KC, 1024], BF16)
        self.yd = Sc("yd", [TOK, D], F32)
        self.combd = Sc("combd", [TOK, NEXP], F32)
        self.combTd = Sc("combTd", [NEXP, TOK], BF16)
        self.htd = Sc("htd", [128, KC, TOK], BF16)
        self.modd = Sc("modd", [6 * D], F32)
        self.qT = Sc("qT", [16, 128, TOK], BF16)
        self.kT = Sc("kT", [4, 128, TOK + 128], BF16)
        self.av = Sc("av", [TOK + 128, 512], BF16)
        self.rqT = Sc("rqT", [8, 128, TOK], BF16)
        self.rkT = Sc("rkT", [8, 128, SEQ], BF16)
        self.rkt = Sc("rkt", [SEQ, 1024], BF16)
        self.rv = Sc("rv", [SEQ, 2048], BF16)
        self.rg = Sc("rg", [TOK, 2048], BF16)
        self.sga = Sc("sga", [D, TOK], BF16)
        self.sgr = Sc("sgr", [D, TOK], BF16)
        self.HT = self.sb(self.gst, "HT", [128, KC, TOK], BF16)
        self.HTb = [Buf(f"HT{k}") for k in range(KC)]
        self.ps = [self.gst.enter_context(nc.psum_tensor(f"ps{i}", [128, 512], F32)) for i in range(8)]
        self.psb = [Buf(f"ps{i}") for i in range(8)]

    def consts(self):
        nc, S, st = self.nc, self.S, self.gst
        sb = self.sb
        self.ones_bf = sb(st, "ones_bf", [128, 128], BF16)
        self.ident_bf = sb(st, "ident_bf", [128, 128], BF16)
        self.rperm_bf = sb(st, "rperm_bf", [128, 128], BF16)
        self.eps_t = sb(st, "eps_t", [128, 1], F32)
        self.am = sb(st, "am", [128, KC], F32)
        self.bm = sb(st, "bm", [128, KC], F32)
        self.af = sb(st, "af", [128, KC], F32)
        self.bf = sb(st, "bf", [128, KC], F32)
        self.qnw_t = sb(st, "qnw_t", [128, 1], F32)
        self.knw_t = sb(st, "knw_t", [128, 1], F32)
        self.combT = sb(st, "combT", [32, TOK], BF16)
        self.combTb = Buf("combT")
        self.cb = Buf("consts")
        cb = self.cb
        S.op(S.dve, lambda: nc.vector.memset(self.ones_bf[:], 1.0), writes=[cb])
        S.op(S.dve, lambda: nc.vector.memset(self.eps_t[:], EPS), writes=[cb])
        S.dma(S.pool, self.ident_bf[:], self.ident, owner=cb, writes=[cb])
        S.dma(S.pool, self.rperm_bf[:], self.rperm, owner=cb, writes=[cb])
        S.dma(S.sp, self.qnw_t[:], self.qnw, owner=cb, writes=[cb])
        S.dma(S.sp, self.knw_t[:], self.knw, owner=cb, writes=[cb])
        import os
        if os.environ.get("K_INIT_AB"):
            for t_ in (self.am, self.af):
                S.op(S.dve, lambda: nc.vector.memset(t_[:], 1.0), writes=[cb])
            for t_ in (self.bm, self.bf):
                S.op(S.dve, lambda: nc.vector.memset(t_[:], 0.0), writes=[cb])
        S.op(S.dve, lambda: nc.vector.tensor_scalar(out=self.qnw_t[:], in0=self.qnw_t[:], scalar1=128.0 ** -0.5,
                                                     scalar2=None, op0=ALU.mult), reads=[cb], writes=[cb])

    def p1_mod(self):
        nc, S = self.nc, self.S
        with ExitStack() as st:
            sb = self.sb
            NB = 256
            wt = [sb(st, f"p1w{i}", [128, KC, NB], BF16) for i in range(2)]
            wb = [Buf(f"p1w{i}") for i in range(2)]
            cs = sb(st, "p1cs", [128, KC], F32)
            csb = sb(st, "p1csb", [128, KC], BF16)
            cbuf = Buf("p1cs")
            row = [sb(st, f"p1row{i}", [1, NB], F32) for i in range(2)]
            rowb = [Buf(f"p1row{i}") for i in range(2)]
            adab = [sb(st, f"p1adab{i}", [1, NB], F32) for i in range(2)]
            adabb = [Buf(f"p1adab{i}") for i in range(2)]
            S.dma(S.sp, cs[:], self.cT, owner=cbuf, writes=[cbuf])
            S.op(S.act, lambda: nc.scalar.activation(out=csb[:], in_=cs[:], func=AF.Silu), reads=[cbuf], writes=[cbuf])
            wv = self.ada_w.rearrange("(kc p) n -> p kc n", p=128)
            md = self.dbufs["modd"]
            moddv = self.modd.rearrange("(a n) -> a n", a=1)
            nblk = 6 * D // NB
            for nb in range(nblk):
                b = nb % 2
                S.dma(S.pool, wt[b][:], wv[:, :, nb * NB:(nb + 1) * NB], owner=wb[b], writes=[wb[b]])
                S.dma(S.sp, adab[b][:], self.ada_b[0:1, nb * NB:(nb + 1) * NB], owner=adabb[b], writes=[adabb[b]])
                pi = nb % 2
                self.mm(self.ps[pi][0:1, 0:NB], self.psb[pi],
                        [(csb[:, kc:kc + 1], wt[b][:, kc, :]) for kc in range(KC)], reads=[cbuf, wb[b]])
                S.op(S.dve, lambda: nc.vector.tensor_tensor(out=row[b][:], in0=self.ps[pi][0:1, 0:NB],
                                                            in1=adab[b][:], op=ALU.add),
                     reads=[self.psb[pi], adabb[b]], writes=[rowb[b]])
                S.dma(S.sp, moddv[0:1, nb * NB:(nb + 1) * NB], row[b][:], owner=rowb[b], reads=[rowb[b]], writes=[md])
            tmp = sb(st, "p1tmp", [128, 4, KC], F32)
            tb = Buf("p1tmp")
            nwm = sb(st, "p1nwm", [128, KC], F32)
            nwf = sb(st, "p1nwf", [128, KC], F32)
            S.dma(S.sp, nwm[:], self.nw_m, owner=tb, writes=[tb])
            S.dma(S.sp, nwf[:], self.nw_f, owner=tb, writes=[tb])
            for j, ci in enumerate((0, 1, 3, 4)):
                S.dma(S.sp, tmp[:, j, :], self.modd[ci * D:(ci + 1) * D].rearrange("(j p) -> p j", p=128),
                      owner=tb, reads=[md], writes=[tb], allow_slow_non_contiguous=True)
            cb = self.cb
            S.op(S.dve, lambda: nc.vector.scalar_tensor_tensor(out=self.am[:], in0=tmp[:, 1, :], scalar=1.0, in1=nwm[:],
                                                               op0=ALU.add, op1=ALU.mult), reads=[tb], writes=[cb])
            S.op(S.dve, lambda: nc.vector.tensor_copy(out=self.bm[:], in_=tmp[:, 0, :]), reads=[tb], writes=[cb])
            S.op(S.dve, lambda: nc.vector.scalar_tensor_tensor(out=self.af[:], in0=tmp[:, 3, :], scalar=1.0, in1=nwf[:],
                                                               op0=ALU.add, op1=ALU.mult), reads=[tb], writes=[cb])
            S.op(S.dve, lambda: nc.vector.tensor_copy(out=self.bf[:], in_=tmp[:, 2, :]), reads=[tb], writes=[cb])
            S.barrier()

    def norm_T(self, xsrc, xname, ntiles, a_t, b_t):
        nc, S = self.nc, self.S
        with ExitStack() as st:
            sb = self.sb
            xt = [sb(st, f"nx{i}", [128, D], F32) for i in range(2)]
            xb = [Buf(f"nx{i}") for i in range(2)]
            xn = [sb(st, f"nxn{i}", [128, D], BF16) for i in range(2)]
            xnb = [Buf(f"nxn{i}") for i in range(2)]
            junk = sb(st, "njunk", [128, D], BF16)
            jb = Buf("njunk")
            stt = [sb(st, f"nst{i}", [128, 4], F32) for i in range(2)]
            stb = [Buf(f"nst{i}") for i in range(2)]
            xd = self.dbufs[xname]
            import os
            NTS = int(os.environ.get("NT_STOP", "9"))
            ntiles = int(os.environ.get("NT_TILES", ntiles))
            for t in range(ntiles):
                b = t % 2
                S.dma(S.sp, xt[b][:], xsrc[t * 128:(t + 1) * 128, :], owner=xb[b], reads=[xd], writes=[xb[b]])
                s_ = stt[b]
                if NTS < 1:
                    continue
                S.op(S.act, lambda: nc.scalar.activation(out=junk[:], in_=xt[b][:], func=AF.Square, accum_out=s_[:, 0:1]),
                     reads=[xb[b]], writes=[jb, stb[b]])
                if NTS < 2:
                    continue
                S.op(S.act, lambda: nc.scalar.activation(out=s_[:, 1:2], in_=s_[:, 0:1], func=AF.Sqrt,
                                                         bias=self.eps_t[:], scale=1.0 / D),
                     reads=[stb[b], self.cb], writes=[stb[b]])
                S.op(S.dve, lambda: nc.vector.reciprocal(out=s_[:, 2:3], in_=s_[:, 1:2]), reads=[stb[b]], writes=[stb[b]])
                if NTS < 3:
                    continue
                S.op(S.dve, lambda: nc.vector.tensor_scalar(out=xn[b][:], in0=xt[b][:], scalar1=s_[:, 2:3], scalar2=None,
                                                            op0=ALU.mult), reads=[xb[b], stb[b]], writes=[xnb[b]])
                if NTS < 4:
                    continue
                for g in range(8):
                    pi = g % 4
                    pv = self.ps[pi][:].bitcast(BF16)
                    for j in range(4):
                        kc = g * 4 + j
                        S.op(S.pe, lambda kc=kc, j=j: nc.tensor.transpose(out=pv[:, j * 128:(j + 1) * 128],
                                                                         in_=xn[b][:, kc * 128:(kc + 1) * 128],
                                                                         identity=self.ident_bf[:]),
                             reads=[xnb[b], self.cb], writes=[self.psb[pi]], signal=(j == 3))
                    if NTS < 5:
                        continue
                    for j in range(4):
                        kc = g * 4 + j
                        dst = self.HT[:, kc, t * 128:(t + 1) * 128]
                        ev = os.environ.get("NT_EV", "alt")
                        if (g % 2 == 0 and ev == "alt") or ev == "act":
                            S.op(S.act, lambda kc=kc, j=j, dst=dst: nc.scalar.activation(
                                out=dst, in_=pv[:, j * 128:(j + 1) * 128], func=AF.Identity,
                                bias=b_t[:, kc:kc + 1], scale=a_t[:, kc:kc + 1]),
                                reads=[self.psb[pi], self.cb], writes=[self.HTb[kc]])
                        else:
                            S.op(S.dve, lambda kc=kc, j=j, dst=dst: nc.vector.tensor_scalar(
                                out=dst, in0=pv[:, j * 128:(j + 1) * 128], scalar1=a_t[:, kc:kc + 1],
                                scalar2=b_t[:, kc:kc + 1], op0=ALU.mult, op1=ALU.add),
                                reads=[self.psb[pi], self.cb], writes=[self.HTb[kc]])
            S.barrier()

    def project(self, w_ap, wname, jobs, tag):
        nc, S = self.nc, self.S
        wv = w_ap.rearrange("(kc p) n -> p kc n", p=128)
        wd = self.dbufs[wname]
        for i, (c0, fn) in enumerate(jobs):
            b = i % 2
            S.dma(S.pool, self.wt[b][:], wv[:, :, c0:c0 + 256], owner=self.wtb[b], reads=[wd], writes=[self.wtb[b]])
            fn(self.wt[b], self.wtb[b], c0)

    def _psrot(self):
        self._pr = (self._pr + 1) % 4
        return self._pr

    def fm_block(self, wt, wb, sub, t0, n):
        pi = self._psrot()
        self.mm(self.ps[pi][:, 0:n], self.psb[pi],
                [(wt[:, kc, sub * 128:(sub + 1) * 128], self.HT[:, kc, t0:t0 + n]) for kc in range(KC)],
                reads=[wb] + self.HTb)
        return pi

    def tm_block(self, wt, wb, t0):
        pi = self._psrot()
        self.mm(self.ps[pi][:, 0:256], self.psb[pi],
                [(self.HT[:, kc, t0:t0 + 128], wt[:, kc, :]) for kc in range(KC)],
                reads=[wb] + self.HTb)
        return pi

    def ep_alloc(self, st):
        sb = self.sb
        self.e_bf = [sb(st, f"e_bf{i}", [128, 512], BF16) for i in range(2)]
        self.e_bfb = [Buf(f"e_bf{i}") for i in range(2)]
        self.e_f = [sb(st, f"e_f{i}", [128, 512], F32) for i in range(4)]
        self.e_fb = [Buf(f"e_f{i}") for i in range(4)]
        self.e_o = [sb(st, f"e_o{i}", [128, 512], BF16) for i in range(3)]
        self.e_ob = [Buf(f"e_o{i}") for i in range(3)]
        self.e_tab = [sb(st, f"e_tab{i}", [128, 2, 512], F32) for i in range(2)]
        self.e_tabb = [Buf(f"e_tab{i}") for i in range(2)]
        self.e_kt = [sb(st, f"e_kt{i}", [128, 512], BF16) for i in range(2)]
        self.e_ktb = [Buf(f"e_kt{i}") for i in range(2)]
        self._eo = 0
        self._ebf = 0
        self._ef = 0
        self._etab = 0
        self._ekt = 0

    def _rot(self, attr, n):
        v = getattr(self, attr)
        setattr(self, attr, (v + 1) % n)
        return v

    def ep_qknorm(self, pi, n, wcol, dst_ap, dname):
        nc, S = self.nc, self.S
        P1 = self.ps[pi][:, 0:n]
        bi = self._rot("_ebf", 2)
        sq, sqb = self.e_bf[bi], self.e_bfb[bi]
        S.op(S.act, lambda: nc.scalar.activation(out=sq[:, 0:n], in_=P1, func=AF.Square), reads=[self.psb[pi]], writes=[sqb])
        p2 = 4 + (pi % 2)
        self.mm(self.ps[p2][:, 0:n], self.psb[p2], [(self.ones_bf[:], sq[:, 0:n])], reads=[sqb, self.cb])
        fi = self._rot("_ef", 4)
        rt, rtb = self.e_f[fi], self.e_fb[fi]
        S.op(S.act, lambda: nc.scalar.activation(out=rt[:, 0:n], in_=self.ps[p2][:, 0:n], func=AF.Sqrt,
                                                 bias=self.eps_t[:], scale=1.0 / 128),
             reads=[self.psb[p2], self.cb], writes=[rtb])
        S.op(S.dve, lambda: nc.vector.reciprocal(out=rt[:, 0:n], in_=rt[:, 0:n]), reads=[rtb], writes=[rtb])
        oi = self._rot("_eo", 3)
        o, ob = self.e_o[oi], self.e_ob[oi]
        S.op(S.dve, lambda: nc.vector.scalar_tensor_tensor(out=o[:, 0:n], in0=P1, scalar=wcol[:, 0:1], in1=rt[:, 0:n],
                                                           op0=ALU.mult, op1=ALU.mult),
             reads=[self.psb[pi], rtb, self.cb], writes=[ob])
        S.dma(S.sp, dst_ap, o[:, 0:n], owner=ob, reads=[ob], writes=[self.dbufs[dname]])

    def ep_rope(self, pi, n, cos_ap, sin_ap, tabkey, dst_ap, dname, tok_dst=None):
        nc, S = self.nc, self.S
        P1 = self.ps[pi][:, 0:n]
        if self._tabkey != tabkey:
            ti = self._rot("_etab", 2)
            tab, tabb = self.e_tab[ti], self.e_tabb[ti]
            S.dma(S.sp, tab[:, 0, 0:n], cos_ap, owner=tabb, writes=[tabb])
            S.dma(S.sp, tab[:, 1, 0:n], sin_ap, owner=tabb, writes=[tabb])
            self._tabkey = tabkey
            self._tab = (tab, tabb)
        tab, tabb = self._tab
        bi = self._rot("_ebf", 2)
        xb, xbb = self.e_bf[bi], self.e_bfb[bi]
        S.op(S.act, lambda: nc.scalar.activation(out=xb[:, 0:n], in_=P1, func=AF.Copy), reads=[self.psb[pi]], writes=[xbb])
        p2 = 4 + (pi % 2)
        self.mm(self.ps[p2][:, 0:n], self.psb[p2], [(self.rperm_bf[:], xb[:, 0:n])], reads=[xbb, self.cb])
        f1 = self._rot("_ef", 4)
        t1, t1b = self.e_f[f1], self.e_fb[f1]
        S.op(S.dve, lambda: nc.vector.tensor_tensor(out=t1[:, 0:n], in0=P1, in1=tab[:, 0, 0:n], op=ALU.mult),
             reads=[self.psb[pi], tabb, xbb], writes=[t1b])
        f2 = self._rot("_ef", 4)
        t2, t2b = self.e_f[f2], self.e_fb[f2]
        S.op(S.dve, lambda: nc.vector.tensor_tensor(out=t2[:, 0:n], in0=self.ps[p2][:, 0:n], in1=tab[:, 1, 0:n], op=ALU.mult),
             reads=[self.psb[p2], tabb], writes=[t2b])
        oi = self._rot("_eo", 3)
        o, ob = self.e_o[oi], self.e_ob[oi]
        S.op(S.dve, lambda: nc.vector.tensor_tensor(out=o[:, 0:n], in0=t1[:, 0:n], in1=t2[:, 0:n], op=ALU.add),
             reads=[t1b, t2b], writes=[ob])
        S.dma(S.sp, dst_ap, o[:, 0:n], owner=ob, reads=[ob], writes=[self.dbufs[dname]])
        if tok_dst is not None:
            p3 = 6 + (pi % 2)
            pv = self.ps[p3][:].bitcast(BF16)
            nb = n // 128
            for j in range(nb):
                S.op(S.pe, lambda j=j: nc.tensor.transpose(out=pv[:, j * 128:(j + 1) * 128], in_=o[:, j * 128:(j + 1) * 128],
                                                           identity=self.ident_bf[:]),
                     reads=[ob, self.cb], writes=[self.psb[p3]], signal=(j == nb - 1))
            ki = self._rot("_ekt", 2)
            kt, ktb = self.e_kt[ki], self.e_ktb[ki]
            S.op(S.act, lambda: nc.scalar.activation(out=kt[:, 0:n], in_=pv[:, 0:n], func=AF.Copy),
                 reads=[self.psb[p3]], writes=[ktb])
            dst, dn = tok_dst
            S.dma(S.sp, dst, kt[:, 0:n].rearrange("p (j d) -> p j d", d=128), owner=ktb, reads=[ktb], writes=[self.dbufs[dn]])

    def ep_act(self, pi, n, func, dst_ap, dname):
        nc, S = self.nc, self.S
        oi = self._rot("_eo", 3)
        o, ob = self.e_o[oi], self.e_ob[oi]
        S.op(S.act, lambda: nc.scalar.activation(out=o[:, 0:n], in_=self.ps[pi][:, 0:n], func=func),
             reads=[self.psb[pi]], writes=[ob])
        S.dma(S.sp, dst_ap, o[:, 0:n], owner=ob, reads=[ob], writes=[self.dbufs[dname]])

    def proj_phase(self, own):
        nc, S = self.nc, self.S
        with ExitStack() as st:
            self.wt = [self.sb(st, f"wt{i}", [128, KC, 256], BF16) for i in range(2)]
            self.wtb = [Buf(f"wt{i}") for i in range(2)]
            self.ep_alloc(st)
            self._pr = 0
            self._tabkey = None
            base = 0 if own else TOK
            jobs = []

            def fm_job(c0, ep):
                def fn(wt, wb, c0_, ep=ep):
                    for sub in range(2):
                        ep(wt, wb, sub, c0_ + sub * 128)
                jobs.append((c0, fn))

            def tm_job(c0, ep, ntile):
                def fn(wt, wb, c0_, ep=ep):
                    for t in range(ntile):
                        pi = self.tm_block(wt, wb, t * 128)
                        ep(pi, t, c0_)
                jobs.append((c0, fn))

            def ep_rk(wt, wb, sub, col):
                h = (col - O_RK) // 128
                for tb in range(4):
                    pi = self.fm_block(wt, wb, sub, tb * 512, 512)
                    l0 = base + tb * 512
                    self.ep_rope(pi, 512, self.cosk[:, l0:l0 + 512], self.sink_[:, l0:l0 + 512], ("k", l0),
                                 self.rkT[h, :, l0:l0 + 512], "rkT",
                                 tok_dst=(self.rkt[l0:l0 + 512, h * 128:(h + 1) * 128].rearrange("(j p) d -> p j d", p=128), "rkt"))
            for c0 in range(O_RK, O_RV, 256):
                fm_job(c0, ep_rk)

            def ep_rv(pi, t, c0_):
                l0 = base + t * 128
                self.ep_act(pi, 256, AF.Copy, self.rv[l0:l0 + 128, c0_ - O_RV:c0_ - O_RV + 256], "rv")
            for c0 in range(O_RV, O_RG, 256):
                tm_job(c0, ep_rv, 16)

            def ep_ak(wt, wb, sub, col):
                g = (col - O_AK) // 128
                if own:
                    for tb in range(4):
                        pi = self.fm_block(wt, wb, sub, tb * 512, 512)
                        self.ep_qknorm(pi, 512, self.knw_t, self.kT[g, :, tb * 512:(tb + 1) * 512], "kT")
                else:
                    pi = self.fm_block(wt, wb, sub, 0, 128)
                    self.ep_qknorm(pi, 128, self.knw_t, self.kT[g, :, TOK:TOK + 128], "kT")
            for c0 in range(O_AK, O_AV, 256):
                fm_job(c0, ep_ak)

            def ep_av(pi, t, c0_):
                l0 = base + t * 128
                self.ep_act(pi, 256, AF.Copy, self.av[l0:l0 + 128, c0_ - O_AV:c0_ - O_AV + 256], "av")
            for c0 in range(O_AV, O_RQ, 256):
                tm_job(c0, ep_av, 16 if own else 1)

            if own:
                def ep_aq(wt, wb, sub, col):
                    h = (col - O_AQ) // 128
                    for tb in range(4):
                        pi = self.fm_block(wt, wb, sub, tb * 512, 512)
                        self.ep_qknorm(pi, 512, self.qnw_t, self.qT[h, :, tb * 512:(tb + 1) * 512], "qT")
                for c0 in range(O_AQ, O_AK, 256):
                    fm_job(c0, ep_aq)

                def ep_rq(wt, wb, sub, col):
                    h = (col - O_RQ) // 128
                    for tb in range(4):
                        pi = self.fm_block(wt, wb, sub, tb * 512, 512)
                        l0 = tb * 512
                        self.ep_rope(pi, 512, self.cosq[:, l0:l0 + 512], self.sinq[:, l0:l0 + 512], ("q", l0),
                                     self.rqT[h, :, l0:l0 + 512], "rqT")
                for c0 in range(O_RQ, O_RK, 256):
                    fm_job(c0, ep_rq)

                def ep_rg(pi, t, c0_):
                    self.ep_act(pi, 256, AF.Silu, self.rg[t * 128:(t + 1) * 128, c0_ - O_RG:c0_ - O_RG + 256], "rg")
                for c0 in range(O_RG, O_GA, 256):
                    tm_job(c0, ep_rg, 16)

                def ep_g(wt, wb, sub, col):
                    if col < O_GR:
                        dst, dn, f0 = self.sga, "sga", col - O_GA
                    else:
                        dst, dn, f0 = self.sgr, "sgr", col - O_GR
                    for tb in range(4):
                        pi = self.fm_block(wt, wb, sub, tb * 512, 512)
                        self.ep_act(pi, 512, AF.Sigmoid, dst[f0:f0 + 128, tb * 512:(tb + 1) * 512], dn)
                for c0 in range(O_GA, O_END, 256):
                    fm_job(c0, ep_g)

            self.project(self.w_in, "w_in", jobs, "in")
            S.barrier()

    def p2_other(self):
        import os
        sk = os.environ.get("KSKIP", "")
        if "normT" not in sk:
            self.norm_T(self.x_oth, "x_oth", 16, self.am, self.bm)
        if "proj" not in sk:
            self.proj_phase(False)

    def p3_own(self):
        self.norm_T(self.x_own, "x_own", 16, self.am, self.bm)
        self.proj_phase(True)


    def p4_attn(self):
        nc, S = self.nc, self.S
        with ExitStack() as st:
            sb = self.sb
            qg = sb(st, "a_q", [128, 4, TOK], BF16); qgb = Buf("a_q")
            kg = sb(st, "a_k", [128, TOK + 128], BF16); kgb = Buf("a_k")
            vg = sb(st, "a_v", [128, 17, 128], BF16); vgb = Buf("a_v")
            bg = sb(st, "a_b", [128, 1536], F32); bgb = Buf("a_b")
            es = sb(st, "a_es", [128, 16], F32); esb = Buf("a_es")
            lg = [sb(st, f"a_lg{i}", [128, 512], F32) for i in range(3)]
            lgb = [Buf(f"a_lg{i}") for i in range(3)]
            pt = [sb(st, f"a_pt{i}", [128, 512], BF16) for i in range(6)]
            ptb = [Buf(f"a_pt{i}") for i in range(6)]
            dn = [sb(st, f"a_dn{i}", [128, 512], F32) for i in range(2)]
            dnb = [Buf(f"a_dn{i}") for i in range(2)]
            S.dma(S.sp, es[:], self.sinkr, owner=esb, writes=[esb])
            S.op(S.act, lambda: nc.scalar.activation(out=es[:], in_=es[:], func=AF.Exp), reads=[esb], writes=[esb])
            u = 0
            for g in range(4):
                S.dma(S.sp, qg[:], self.qT[4 * g:4 * g + 4].rearrange("h d t -> d h t"), owner=qgb,
                      reads=[self.dbufs["qT"]], writes=[qgb])
                S.dma(S.sp, kg[:], self.kT[g], owner=kgb, reads=[self.dbufs["kT"]], writes=[kgb])
                S.dma(S.sp, vg[:], self.av[:, g * 128:(g + 1) * 128].rearrange("(n p) d -> p n d", p=128), owner=vgb,
                      reads=[self.dbufs["av"]], writes=[vgb])
                S.dma(S.sp, bg[:], self.biasT[g], owner=bgb, writes=[bgb])
                for qb in range(16):
                    kbs = [kb for kb in range(3) if qb + kb - 1 >= 0]
                    sbank = [0, 1, 2] if u % 2 == 0 else [5, 6, 7]
                    pts = []
                    for kb in kbs:
                        blk = qb + kb - 1
                        pi = sbank[kb]
                        self.mm(self.ps[pi][:].rearrange("p (h q) -> p h q", h=4), self.psb[pi],
                                [(kg[:, blk * 128:(blk + 1) * 128], qg[:, :, qb * 128:(qb + 1) * 128])],
                                reads=[kgb, qgb])
                        S.op(S.dve, lambda: nc.vector.tensor_tensor(out=lg[kb][:], in0=self.ps[pi][:],
                                                                    in1=bg[:, kb * 512:(kb + 1) * 512], op=ALU.add),
                             reads=[self.psb[pi], bgb], writes=[lgb[kb]])
                        pj = (u % 2) * 3 + kb
                        S.op(S.act, lambda: nc.scalar.activation(out=pt[pj][:], in_=lg[kb][:], func=AF.Exp),
                             reads=[lgb[kb]], writes=[ptb[pj]])
                        pts.append((blk, pj))
                    self.mm(self.ps[3][:], self.psb[3], [(vg[:, blk, :], pt[pj][:]) for blk, pj in pts],
                            reads=[vgb] + [ptb[pj] for _, pj in pts])
                    self.mm(self.ps[4][:], self.psb[4], [(self.ones_bf[:], pt[pj][:]) for blk, pj in pts],
                            reads=[self.cb] + [ptb[pj] for _, pj in pts])
                    d_ = dn[u % 2]; db_ = dnb[u % 2]
                    for hh in range(4):
                        h = 4 * g + hh
                        S.op(S.dve, lambda: nc.vector.tensor_scalar(out=d_[:, hh * 128:(hh + 1) * 128],
                                                                    in0=self.ps[4][:, hh * 128:(hh + 1) * 128],
                                                                    scalar1=es[:, h:h + 1], scalar2=None, op0=ALU.add),
                             reads=[self.psb[4], esb], writes=[db_])
                    S.op(S.dve, lambda: nc.vector.reciprocal(out=d_[:], in_=d_[:]), reads=[db_], writes=[db_])
                    S.op(S.dve, lambda: nc.vector.tensor_tensor(
                        out=self.HT[:, 4 * g:4 * g + 4, qb * 128:(qb + 1) * 128],
                        in0=self.ps[3][:].rearrange("p (h q) -> p h q", h=4),
                        in1=d_[:].rearrange("p (h q) -> p h q", h=4), op=ALU.mult),
                        reads=[self.psb[3], db_], writes=self.HTb[4 * g:4 * g + 4])
                    u += 1
            S.barrier()

    def p5_ret(self):
        nc, S = self.nc, self.S
        with ExitStack() as st:
            sb = self.sb
            tb = Buf("r_tabs")
            lgf = sb(st, "r_lgf", [128, 8], F32)
            lgb_ = sb(st, "r_lgb", [128, 8], F32)
            cpos = sb(st, "r_cpos", [128, 128], F32)
            cneg = sb(st, "r_cneg", [128, 128], F32)
            ccol = sb(st, "r_ccol", [128, 2], F32)
            crow = sb(st, "r_crow", [128, 256], F32)
            cdf = sb(st, "r_cdf", [128, 8], F32)
            cdb = sb(st, "r_cdb", [128, 8], F32)
            tmp = sb(st, "r_tmp", [128, 128], F32)
            for dst, src_ in ((lgf, self.dec_f), (lgb_, self.dec_b), (cpos, self.cpos), (cneg, self.cneg),
                              (ccol, self.ccol), (crow, self.crow)):
                S.dma(S.sp, dst[:], src_, owner=tb, writes=[tb])
            for t_ in (lgf, lgb_):
                S.op(S.act, lambda: nc.scalar.activation(out=t_[:], in_=t_[:], func=AF.Exp), reads=[tb], writes=[tb])
                S.op(S.dve, lambda: nc.vector.tensor_scalar(out=t_[:], in0=t_[:], scalar1=-1.0, scalar2=None, op0=ALU.mult),
                     reads=[tb], writes=[tb])
            S.op(S.act, lambda: nc.scalar.activation(out=cdf[:], in_=lgf[:], func=AF.Exp, scale=128.0), reads=[tb], writes=[tb])
            S.op(S.act, lambda: nc.scalar.activation(out=cdb[:], in_=lgb_[:], func=AF.Exp, scale=128.0), reads=[tb], writes=[tb])
            hb = Buf("r_htab")
            DT = sb(st, "r_DT", [128, 128], F32)
            kdf = sb(st, "r_kdf", [128, 1], F32)
            kdb = sb(st, "r_kdb", [128, 1], F32)
            qdf = sb(st, "r_qdf", [128, 128], F32)
            qdb = sb(st, "r_qdb", [128, 128], F32)
            gnw = sb(st, "r_gnw", [128, 256], F32)
            gnb = sb(st, "r_gnb", [128, 256], F32)

            kt = sb(st, "r_kt", [128, 32, 128], BF16); ktb = Buf("r_kt")
            vv = sb(st, "r_v", [128, 32, 256], BF16); vvb = Buf("r_v")
            qT = sb(st, "r_qT", [128, TOK], BF16); qTb = Buf("r_qT")
            kT = sb(st, "r_kT", [128, TOK], BF16); kTb = Buf("r_kT")
            rgt = [sb(st, f"r_rg{i}", [128, 256], BF16) for i in range(2)]; rgb = [Buf(f"r_rg{i}") for i in range(2)]
            kdec = sb(st, "r_kdec", [128, 8, 128], BF16); kdecb = [Buf(f"r_kdec{i}") for i in range(8)]
            KV = sb(st, "r_KV", [128, 8, 256], F32); KVb = [Buf(f"r_KV{i}") for i in range(8)]
            Sf = sb(st, "r_Sf", [128, 16, 256], BF16); Sfb = [Buf(f"r_Sf{i}") for i in range(16)]
            Sb_ = sb(st, "r_Sb", [128, 16, 256], BF16); Sbb = [Buf(f"r_Sb{i}") for i in range(16)]
            run = sb(st, "r_run", [128, 2, 256], F32); runb = [Buf("r_runf"), Buf("r_runb")]
            scd = [sb(st, f"r_scd{i}", [128, 128], BF16) for i in range(2)]; scdb = [Buf(f"r_scd{i}") for i in range(2)]
            qfb = [sb(st, f"r_qfb{i}", [128, 2, 128], BF16) for i in range(2)]; qfbb = [Buf(f"r_qfb{i}") for i in range(2)]
            stt = [sb(st, f"r_st{i}", [128, 12], F32) for i in range(2)]; sttb = [Buf(f"r_st{i}") for i in range(2)]
            yn = [sb(st, f"r_yn{i}", [128, 256], F32) for i in range(2)]; ynb = [Buf(f"r_yn{i}") for i in range(2)]
            yo = [sb(st, f"r_yo{i}", [128, 256], BF16) for i in range(2)]; yob = [Buf(f"r_yo{i}") for i in range(2)]
            dd = self.dbufs

            def states(h, chunks, kd, cd, ri, store):
                for g0 in range(0, len(chunks), 8):
                    grp = chunks[g0:g0 + 8]
                    for i, n in enumerate(grp):
                        eng, ns = (S.dve, nc.vector) if i % 2 else (S.pool, nc.gpsimd)
                        S.op(eng, lambda: ns.tensor_scalar(out=kdec[:, i, :], in0=kt[:, n, :], scalar1=kd[:, 0:1], scalar2=None,
                                                           op0=ALU.mult), reads=[ktb, hb], writes=[kdecb[i]])
                    for i, n in enumerate(grp):
                        pi = i % 4
                        half = i // 4
                        pa = self.ps[pi][:, half * 256:(half + 1) * 256]
                        self.mm(pa, self.psb[pi], [(kdec[:, i, :], vv[:, n, :])], reads=[kdecb[i], vvb])
                        S.op(S.act, lambda: nc.scalar.activation(out=KV[:, i, :], in_=pa, func=AF.Copy),
                             reads=[self.psb[pi]], writes=[KVb[i]])
                    for i, n in enumerate(grp):
                        S.op(S.dve, lambda: nc.vector.scalar_tensor_tensor(out=run[:, ri, :], in0=run[:, ri, :], scalar=cd,
                                                                           in1=KV[:, i, :], op0=ALU.mult, op1=ALU.add),
                             reads=[KVb[i], tb, runb[ri]], writes=[runb[ri]])
                        store(n)

            for h in range(8):
                S.dma(S.sp, kt[:], self.rkt[:, h * 128:(h + 1) * 128].rearrange("(n p) d -> p n d", p=128), owner=ktb,
                      reads=[dd["rkt"]], writes=[ktb])
                S.dma(S.sp, vv[:], self.rv[:, h * 256:(h + 1) * 256].rearrange("(n p) e -> p n e", p=128), owner=vvb,
                      reads=[dd["rv"]], writes=[vvb])
                S.dma(S.sp, qT[:], self.rqT[h], owner=qTb, reads=[dd["rqT"]], writes=[qTb])
                S.dma(S.sp, kT[:], self.rkT[h, :, 0:TOK], owner=kTb, reads=[dd["rkT"]], writes=[kTb])
                S.dma(S.sp, gnw[:], self.gnw[:, h * 256:(h + 1) * 256], owner=hb, writes=[hb])
                S.dma(S.sp, gnb[:], self.gnb[:, h * 256:(h + 1) * 256], owner=hb, writes=[hb])
                S.op(S.dve, lambda: nc.vector.tensor_scalar(out=tmp[:], in0=cpos[:], scalar1=lgf[:, h:h + 1], scalar2=None,
                                                            op0=ALU.mult), reads=[tb], writes=[hb])
                S.op(S.dve, lambda: nc.vector.scalar_tensor_tensor(out=tmp[:], in0=cneg[:], scalar=lgb_[:, h:h + 1], in1=tmp[:],
                                                                   op0=ALU.mult, op1=ALU.add), reads=[tb, hb], writes=[hb])
                S.op(S.act, lambda: nc.scalar.activation(out=DT[:], in_=tmp[:], func=AF.Exp), reads=[hb], writes=[hb])
                S.op(S.act, lambda: nc.scalar.activation(out=kdf[:], in_=ccol[:, 0:1], func=AF.Exp, scale=lgf[:, h:h + 1]),
                     reads=[tb], writes=[hb])
                S.op(S.act, lambda: nc.scalar.activation(out=kdb[:], in_=ccol[:, 1:2], func=AF.Exp, scale=lgb_[:, h:h + 1]),
                     reads=[tb], writes=[hb])
                S.op(S.act, lambda: nc.scalar.activation(out=qdf[:], in_=crow[:, 0:128], func=AF.Exp, scale=lgf[:, h:h + 1]),
                     reads=[tb], writes=[hb])
                S.op(S.act, lambda: nc.scalar.activation(out=qdb[:], in_=crow[:, 128:256], func=AF.Exp, scale=lgb_[:, h:h + 1]),
                     reads=[tb], writes=[hb])
                S.op(S.dve, lambda: nc.vector.memset(run[:, 1, :], 0.0), writes=[runb[1]])

                def store_b(n):
                    if n - 1 <= 15:
                        S.op(S.act, lambda: nc.scalar.activation(out=Sb_[:, n - 1, :], in_=run[:, 1, :], func=AF.Copy),
                             reads=[runb[1]], writes=[Sbb[n - 1]])
                states(h, list(range(31, 0, -1)), kdb, cdb[:, h:h + 1], 1, store_b)
                S.op(S.dve, lambda: nc.vector.memset(run[:, 0, :], 0.0), writes=[runb[0]])
                S.op(S.act, lambda: nc.scalar.activation(out=Sf[:, 0, :], in_=run[:, 0, :], func=AF.Copy),
                     reads=[runb[0]], writes=[Sfb[0]])

                def store_f(n):
                    S.op(S.act, lambda: nc.scalar.activation(out=Sf[:, n + 1, :], in_=run[:, 0, :], func=AF.Copy),
                         reads=[runb[0]], writes=[Sfb[n + 1]])
                states(h, list(range(0, 15)), kdf, cdf[:, h:h + 1], 0, store_f)
                for n in range(16):
                    b = n % 2
                    c0, c1 = n * 128, (n + 1) * 128
                    S.dma(S.sp, rgt[b][:], self.rg[c0:c1, h * 256:(h + 1) * 256], owner=rgb[b], reads=[dd["rg"]], writes=[rgb[b]])
                    p_s = 4 + b
                    self.mm(self.ps[p_s][:, 0:128], self.psb[p_s], [(kT[:, c0:c1], qT[:, c0:c1])], reads=[kTb, qTb])
                    S.op(S.dve, lambda: nc.vector.tensor_tensor(out=scd[b][:], in0=self.ps[p_s][:, 0:128], in1=DT[:], op=ALU.mult),
                         reads=[self.psb[p_s], hb], writes=[scdb[b]])
                    S.op(S.pool, lambda: nc.gpsimd.tensor_tensor(out=qfb[b][:, 0, :], in0=qT[:, c0:c1], in1=qdf[:], op=ALU.mult),
                         reads=[qTb, hb], writes=[qfbb[b]])
                    S.op(S.pool, lambda: nc.gpsimd.tensor_tensor(out=qfb[b][:, 1, :], in0=qT[:, c0:c1], in1=qdb[:], op=ALU.mult),
                         reads=[qTb, hb], writes=[qfbb[b]])
                    p_y = 6 + b
                    self.mm(self.ps[p_y][:, 0:256], self.psb[p_y],
                            [(scd[b][:], vv[:, n, :]), (qfb[b][:, 0, :], Sf[:, n, :]), (qfb[b][:, 1, :], Sb_[:, n, :])],
                            reads=[scdb[b], vvb, qfbb[b], Sfb[n], Sbb[n]])
                    y = self.ps[p_y][:, 0:256]
                    s_ = stt[b]
                    S.op(S.dve, lambda: nc.vector.bn_stats(out=s_[:, 0:6], in_=y), reads=[self.psb[p_y]], writes=[sttb[b]])
                    S.op(S.dve, lambda: nc.vector.bn_aggr(out=s_[:, 6:8], in_=s_[:, 0:6]), reads=[sttb[b]], writes=[sttb[b]])
                    S.op(S.act, lambda: nc.scalar.activation(out=s_[:, 8:9], in_=s_[:, 7:8], func=AF.Sqrt, bias=self.eps_t[:], scale=1.0),
                         reads=[sttb[b], self.cb], writes=[sttb[b]])
                    S.op(S.dve, lambda: nc.vector.reciprocal(out=s_[:, 9:10], in_=s_[:, 8:9]), reads=[sttb[b]], writes=[sttb[b]])
                    S.op(S.dve, lambda: nc.vector.tensor_scalar(out=yn[b][:], in0=y, scalar1=s_[:, 6:7], scalar2=s_[:, 9:10],
                                                                op0=ALU.subtract, op1=ALU.mult),
                         reads=[self.psb[p_y], sttb[b]], writes=[ynb[b]])
                    S.op(S.pool, lambda: nc.gpsimd.tensor_tensor(out=yn[b][:], in0=yn[b][:], in1=gnw[:], op=ALU.mult),
                         reads=[ynb[b], hb], writes=[ynb[b]])
                    S.op(S.pool, lambda: nc.gpsimd.tensor_tensor(out=yn[b][:], in0=yn[b][:], in1=gnb[:], op=ALU.add),
                         reads=[ynb[b], hb], writes=[ynb[b]])
                    S.op(S.pool, lambda: nc.gpsimd.tensor_tensor(out=yo[b][:], in0=yn[b][:], in1=rgt[b][:], op=ALU.mult),
                         reads=[ynb[b], rgb[b]], writes=[yob[b]])
                    p_t = 2 + b
                    pv = self.ps[p_t][:].bitcast(BF16)
                    for eb in range(2):
                        S.op(S.pe, lambda: nc.tensor.transpose(out=pv[:, eb * 128:(eb + 1) * 128], in_=yo[b][:, eb * 128:(eb + 1) * 128],
                                                               identity=self.ident_bf[:]),
                             reads=[yob[b], self.cb], writes=[self.psb[p_t]], signal=(eb == 1))
                    S.op(S.act, lambda: nc.scalar.activation(out=self.HT[:, 16 + 2 * h:18 + 2 * h, c0:c1],
                                                             in_=pv[:, 0:256].rearrange("p (e i) -> p e i", e=2), func=AF.Copy),
                         reads=[self.psb[p_t]], writes=self.HTb[16 + 2 * h:18 + 2 * h])
            S.barrier()
            if "htd" in self.debug:
                S.dma(S.sp, self.htd, self.HT[:], owner=self.HTb[0], reads=self.HTb, writes=[self.dbufs["htd"]])
                S.barrier()


    def p6_up(self):
        nc, S = self.nc, self.S
        if "htd" in self.ext_in:
            S.dma(S.sp, self.HT[:], self.htd, owner=self.HTb[0], reads=[self.dbufs["htd"]], writes=self.HTb)
            S.barrier()
        with ExitStack() as st:
            sb = self.sb
            wt = [sb(st, f"u_wt{i}", [128, KC, 256], BF16) for i in range(2)]
            wtb = [Buf(f"u_wt{i}") for i in range(2)]
            ga = [sb(st, f"u_ga{i}", [128, 2, 512], BF16) for i in range(2)]; gab = [Buf(f"u_ga{i}") for i in range(2)]
            t1 = [sb(st, f"u_t1{i}", [128, 512], F32) for i in range(2)]; t1b = [Buf(f"u_t1{i}") for i in range(2)]
            t2 = [sb(st, f"u_t2{i}", [128, 512], F32) for i in range(2)]; t2b = [Buf(f"u_t2{i}") for i in range(2)]
            mo = [sb(st, f"u_mo{i}", [128, 512], BF16) for i in range(2)]; mob = [Buf(f"u_mo{i}") for i in range(2)]
            wa = self.w_ua.rearrange("(kc p) n -> p kc n", p=128)
            wr = self.w_ur.rearrange("(kc p) n -> p kc n", p=128)
            dd = self.dbufs
            u = 0
            for fb2 in range(16):
                b = fb2 % 2
                c0 = fb2 * 256
                S.dma(S.pool, wt[b][:, 0:16, :], wa[:, :, c0:c0 + 256], owner=wtb[b], writes=[wtb[b]])
                S.dma(S.pool, wt[b][:, 16:32, :], wr[:, :, c0:c0 + 256], owner=wtb[b], writes=[wtb[b]])
                for sub in range(2):
                    f0 = c0 + sub * 128
                    for tb_ in range(4):
                        i2 = u % 2
                        t0 = tb_ * 512
                        S.dma(S.sp, ga[i2][:, 0, :], self.sga[f0:f0 + 128, t0:t0 + 512], owner=gab[i2], reads=[dd["sga"]], writes=[gab[i2]])
                        S.dma(S.sp, ga[i2][:, 1, :], self.sgr[f0:f0 + 128, t0:t0 + 512], owner=gab[i2], reads=[dd["sgr"]], writes=[gab[i2]])
                        pa, pr = (0, 1) if i2 == 0 else (2, 3)
                        self.mm(self.ps[pa][:], self.psb[pa],
                                [(wt[b][:, kc, sub * 128:(sub + 1) * 128], self.HT[:, kc, t0:t0 + 512]) for kc in range(16)],
                                reads=[wtb[b]] + self.HTb[0:16])
                        self.mm(self.ps[pr][:], self.psb[pr],
                                [(wt[b][:, kc, sub * 128:(sub + 1) * 128], self.HT[:, kc, t0:t0 + 512]) for kc in range(16, 32)],
                                reads=[wtb[b]] + self.HTb[16:32])
                        S.op(S.dve, lambda: nc.vector.tensor_tensor(out=t1[i2][:], in0=self.ps[pa][:], in1=ga[i2][:, 0, :], op=ALU.mult),
                             reads=[self.psb[pa], gab[i2]], writes=[t1b[i2]])
                        S.op(S.dve, lambda: nc.vector.tensor_tensor(out=t2[i2][:], in0=self.ps[pr][:], in1=ga[i2][:, 1, :], op=ALU.mult),
                             reads=[self.psb[pr], gab[i2]], writes=[t2b[i2]])
                        S.op(S.dve, lambda: nc.vector.tensor_tensor(out=mo[i2][:], in0=t1[i2][:], in1=t2[i2][:], op=ALU.add),
                             reads=[t1b[i2], t2b[i2]], writes=[mob[i2]])
                        S.dma(S.sp, self.mrg[f0:f0 + 128, t0:t0 + 512], mo[i2][:], owner=mob[i2], reads=[mob[i2]], writes=[dd["mrg"]])
                        u += 1
            S.barrier()
        for kc in range(KC):
            S.dma(S.sp, self.HT[:, kc, :], self.mrg[kc * 128:(kc + 1) * 128, :], owner=self.HTb[kc],
                  reads=[self.dbufs["mrg"]], writes=[self.HTb[kc]])
        S.barrier()

    def p7_out(self):
        nc, S = self.nc, self.S
        with ExitStack() as st:
            sb = self.sb
            wt = [sb(st, f"o_wt{i}", [128, KC, 256], BF16) for i in range(2)]
            wtb = [Buf(f"o_wt{i}") for i in range(2)]
            gm = [sb(st, f"o_gm{i}", [128, 256], F32) for i in range(2)]; gmb = [Buf(f"o_gm{i}") for i in range(2)]
            xt = [sb(st, f"o_x{i}", [128, 256], F32) for i in range(3)]; xtb = [Buf(f"o_x{i}") for i in range(3)]
            tt = [sb(st, f"o_t{i}", [128, 256], F32) for i in range(2)]; ttb = [Buf(f"o_t{i}") for i in range(2)]
            wv = self.w_out.rearrange("(kc p) n -> p kc n", p=128)
            dd = self.dbufs
            u = 0
            for nb in range(16):
                b = nb % 2
                c0 = nb * 256
                S.dma(S.pool, wt[b][:], wv[:, :, c0:c0 + 256], owner=wtb[b], writes=[wtb[b]])
                S.dma(S.sp, gm[b][:], self.modd[2 * D + c0:2 * D + c0 + 256].partition_broadcast(128), owner=gmb[b],
                      reads=[dd["modd"]], writes=[gmb[b]])
                for t in range(16):
                    i3 = u % 3
                    i2 = u % 2
                    S.dma(S.sp, xt[i3][:], self.x_own[t * 128:(t + 1) * 128, c0:c0 + 256], owner=xtb[i3], writes=[xtb[i3]])
                    pi = u % 4
                    self.mm(self.ps[pi][:, 0:256], self.psb[pi],
                            [(self.HT[:, kc, t * 128:(t + 1) * 128], wt[b][:, kc, :]) for kc in range(KC)],
                            reads=[wtb[b]] + self.HTb)
                    S.op(S.dve, lambda: nc.vector.tensor_tensor(out=tt[i2][:], in0=self.ps[pi][:, 0:256], in1=gm[b][:], op=ALU.mult),
                         reads=[self.psb[pi], gmb[b]], writes=[ttb[i2]])
                    S.op(S.dve, lambda: nc.vector.tensor_tensor(out=xt[i3][:], in0=xt[i3][:], in1=tt[i2][:], op=ALU.add),
                         reads=[ttb[i2], xtb[i3]], writes=[xtb[i3]])
                    S.dma(S.sp, self.x1[t * 128:(t + 1) * 128, c0:c0 + 256], xt[i3][:], owner=xtb[i3], reads=[xtb[i3]], writes=[dd["x1"]])
                    u += 1
            S.barrier()

    def p8_norm2(self):
        nc, S = self.nc, self.S
        self.norm_T(self.x1, "x1", 16, self.af, self.bf)
        with ExitStack() as st:
            sb = self.sb
            rw = sb(st, "g_rw", [128, KC, NEXP], BF16); rwb = Buf("g_rw")
            rbt = sb(st, "g_rb", [128, NEXP], F32)
            lg = [sb(st, f"g_lg{i}", [128, NEXP], F32) for i in range(2)]; lgb = [Buf(f"g_lg{i}") for i in range(2)]
            m8 = [sb(st, f"g_m8{i}", [128, 12], F32) for i in range(2)]; m8b = [Buf(f"g_m8{i}") for i in range(2)]
            mk = [sb(st, f"g_mk{i}", [128, NEXP], F32) for i in range(2)]; mkb = [Buf(f"g_mk{i}") for i in range(2)]
            ex = [sb(st, f"g_ex{i}", [128, NEXP], F32) for i in range(2)]; exb = [Buf(f"g_ex{i}") for i in range(2)]
            cm = [sb(st, f"g_cm{i}", [128, NEXP], F32) for i in range(2)]; cmb = [Buf(f"g_cm{i}") for i in range(2)]
            cmh = [sb(st, f"g_cmh{i}", [128, NEXP], BF16) for i in range(2)]; cmhb = [Buf(f"g_cmh{i}") for i in range(2)]
            S.dma(S.pool, rw[:], self.router_w.rearrange("(kc p) e -> p kc e", p=128), owner=rwb, writes=[rwb])
            S.dma(S.sp, rbt[:], self.rb, owner=rwb, writes=[rwb])
            dd = self.dbufs
            for t in range(16):
                b = t % 2
                pi = t % 2
                self.mm(self.ps[pi][:, 0:NEXP], self.psb[pi],
                        [(self.HT[:, kc, t * 128:(t + 1) * 128], rw[:, kc, :]) for kc in range(KC)], reads=[rwb] + self.HTb)
                S.op(S.dve, lambda: nc.vector.tensor_tensor(out=lg[b][:], in0=self.ps[pi][:, 0:NEXP], in1=rbt[:], op=ALU.add),
                     reads=[self.psb[pi], rwb], writes=[lgb[b]])
                S.op(S.dve, lambda: nc.vector.max(out=m8[b][:, 0:8], in_=lg[b][:]), reads=[lgb[b]], writes=[m8b[b]])
                S.op(S.dve, lambda: nc.vector.tensor_scalar(out=m8[b][:, 8:9], in0=m8[b][:, 0:1], scalar1=-1.0, scalar2=None, op0=ALU.mult),
                     reads=[m8b[b]], writes=[m8b[b]])
                S.op(S.dve, lambda: nc.vector.tensor_scalar(out=mk[b][:], in0=lg[b][:], scalar1=m8[b][:, 3:4], scalar2=None, op0=ALU.is_ge),
                     reads=[lgb[b], m8b[b]], writes=[mkb[b]])
                S.op(S.act, lambda: nc.scalar.activation(out=ex[b][:], in_=lg[b][:], func=AF.Exp, bias=m8[b][:, 8:9], scale=1.0),
                     reads=[lgb[b], m8b[b]], writes=[exb[b]])
                S.op(S.dve, lambda: nc.vector.tensor_tensor(out=ex[b][:], in0=ex[b][:], in1=mk[b][:], op=ALU.mult),
                     reads=[exb[b], mkb[b]], writes=[exb[b]])
                S.op(S.dve, lambda: nc.vector.reduce_sum(out=m8[b][:, 9:10], in_=ex[b][:], axis=AX.X), reads=[exb[b]], writes=[m8b[b]])
                S.op(S.dve, lambda: nc.vector.reciprocal(out=m8[b][:, 10:11], in_=m8[b][:, 9:10]), reads=[m8b[b]], writes=[m8b[b]])
                S.op(S.dve, lambda: nc.vector.tensor_scalar(out=cm[b][:], in0=ex[b][:], scalar1=m8[b][:, 10:11], scalar2=None, op0=ALU.mult),
                     reads=[exb[b], m8b[b]], writes=[cmb[b]])
                S.op(S.act, lambda: nc.scalar.activation(out=cmh[b][:], in_=cm[b][:], func=AF.Copy), reads=[cmb[b]], writes=[cmhb[b]])
                if "combd" in self.debug:
                    S.dma(S.sp, self.combd[t * 128:(t + 1) * 128, :], cm[b][:], owner=cmb[b], reads=[cmb[b]], writes=[dd["combd"]])
                p_t = 2 + b
                pv = self.ps[p_t][:].bitcast(BF16)
                S.op(S.pe, lambda: nc.tensor.transpose(out=pv[0:NEXP, 0:128], in_=cmh[b][:], identity=self.ident_bf[:]),
                     reads=[cmhb[b], self.cb], writes=[self.psb[p_t]])
                S.op(S.act, lambda: nc.scalar.activation(out=self.combT[:, t * 128:(t + 1) * 128], in_=pv[0:NEXP, 0:128], func=AF.Copy),
                     reads=[self.psb[p_t]], writes=[self.combTb])
            S.dma(S.sp, self.combTd, self.combT[:], owner=self.combTb, reads=[self.combTb], writes=[dd["combTd"]])
            S.dma(S.sp, self.h2d, self.HT[:, :, 1024:2048], owner=self.HTb[0], reads=self.HTb, writes=[dd["h2d"]])
            S.barrier()

    def p9_moe(self):
        nc, S = self.nc, self.S
        NE = self.n_exp
        with ExitStack() as st:
            sb = self.sb
            dd = self.dbufs
            w1t = [sb(st, f"m_w1{i}", [128, KC, 2, 128], BF16) for i in range(2)]; w1b = [Buf(f"m_w1{i}") for i in range(2)]
            selb = Buf("m_sel")
            b1t = sb(st, "m_b1", [128, NE * 16], F32)
            cbe = [sb(st, f"m_cbe{i}", [128, 1024], BF16) for i in range(2)]; cbeb = [Buf(f"m_cbe{i}") for i in range(2)]
            gt = [sb(st, f"m_g{i}", [128, 512], F32) for i in range(2)]; gtb = [Buf(f"m_g{i}") for i in range(2)]
            sg = [sb(st, f"m_s{i}", [128, 512], F32) for i in range(2)]; sgb = [Buf(f"m_s{i}") for i in range(2)]
            ut = [sb(st, f"m_u{i}", [128, 512], F32) for i in range(2)]; utb = [Buf(f"m_u{i}") for i in range(2)]
            yt = [sb(st, f"m_y{i}", [128, 512], F32) for i in range(3)]; ytb = [Buf(f"m_y{i}") for i in range(3)]
            xr = [sb(st, f"m_x{i}", [128, 512], F32) for i in range(2)]; xrb = [Buf(f"m_x{i}") for i in range(2)]
            gf = [sb(st, f"m_gf{i}", [128, 512], F32) for i in range(2)]; gfb = [Buf(f"m_gf{i}") for i in range(2)]
            b2t = [sb(st, f"m_b2{i}", [32, 512], BF16) for i in range(2)]; b2b = [Buf(f"m_b2{i}") for i in range(2)]
            S.dma(S.sp, b1t[:], self.b1T, owner=selb, writes=[selb])
            actT = self.HT[:, 0:16, 1024:2048]
            actb = [Buf("m_act0"), Buf("m_act1")]
            w2t = [self.HT[:, 16:32, 1024:1536], self.HT[:, 16:32, 1536:2048]]
            w2b = [Buf("m_w20"), Buf("m_w21")]
            hb = [Buf("m_h2")]
            ngrp = NE // 2
            cnt = dict(w1=0, blk=0, w2=0, y=0)
            for half in range(2):
                tk0 = half * 1024
                if half == 1:
                    S.dma(S.sp, self.HT[:, :, 0:1024], self.h2d, owner=hb[0], reads=[dd["h2d"]], writes=hb)
                for grp in range(ngrp):
                    for ei in range(2):
                        e = grp * 2 + ei
                        S.dma(S.sp, cbe[ei][:], self.combTd[e, tk0:tk0 + 1024].partition_broadcast(128), owner=cbeb[ei],
                              reads=[dd["combTd"]], writes=[cbeb[ei]])
                        for fb in range(8):
                            wb_ = cnt["w1"] % 2; cnt["w1"] += 1
                            w1v = self.w1[e].rearrange("(kc p) n -> p kc n", p=128)
                            S.dma(S.pool, w1t[wb_][:, :, 0, :], w1v[:, :, fb * 128:(fb + 1) * 128], owner=w1b[wb_], writes=[w1b[wb_]])
                            S.dma(S.pool, w1t[wb_][:, :, 1, :], w1v[:, :, DFF + fb * 128:DFF + (fb + 1) * 128], owner=w1b[wb_], writes=[w1b[wb_]])
                            for tb_ in range(2):
                                i2 = cnt["blk"] % 2; cnt["blk"] += 1
                                pg, pu = (0, 1) if i2 == 0 else (2, 3)
                                rhs_t = slice(tb_ * 512, (tb_ + 1) * 512)
                                self.mm(self.ps[pg][:], self.psb[pg],
                                        [(w1t[wb_][:, kc, 0, :], self.HT[:, kc, rhs_t]) for kc in range(KC)], reads=[w1b[wb_]] + hb)
                                self.mm(self.ps[pu][:], self.psb[pu],
                                        [(w1t[wb_][:, kc, 1, :], self.HT[:, kc, rhs_t]) for kc in range(KC)], reads=[w1b[wb_]] + hb)
                                bg = b1t[:, e * 16 + fb:e * 16 + fb + 1]
                                bu = b1t[:, e * 16 + 8 + fb:e * 16 + 8 + fb + 1]
                                S.op(S.dve, lambda: nc.vector.tensor_scalar(out=gt[i2][:], in0=self.ps[pg][:], scalar1=bg, scalar2=7.0,
                                                                            op0=ALU.add, op1=ALU.min), reads=[self.psb[pg], selb], writes=[gtb[i2]])
                                S.op(S.act, lambda: nc.scalar.activation(out=sg[i2][:], in_=gt[i2][:], func=AF.Sigmoid, scale=1.702),
                                     reads=[gtb[i2]], writes=[sgb[i2]])
                                S.op(S.dve, lambda: nc.vector.tensor_scalar(out=ut[i2][:], in0=self.ps[pu][:], scalar1=bu, scalar2=7.0,
                                                                            op0=ALU.add, op1=ALU.min), reads=[self.psb[pu], selb], writes=[utb[i2]])
                                S.op(S.dve, lambda: nc.vector.tensor_scalar(out=ut[i2][:], in0=ut[i2][:], scalar1=-7.0, scalar2=1.0,
                                                                            op0=ALU.max, op1=ALU.add), reads=[utb[i2]], writes=[utb[i2]])
                                S.op(S.dve, lambda: nc.vector.tensor_tensor(out=gt[i2][:], in0=gt[i2][:], in1=sg[i2][:], op=ALU.mult),
                                     reads=[gtb[i2], sgb[i2]], writes=[gtb[i2]])
                                S.op(S.dve, lambda: nc.vector.tensor_tensor(out=gt[i2][:], in0=gt[i2][:], in1=ut[i2][:], op=ALU.mult),
                                     reads=[gtb[i2], utb[i2]], writes=[gtb[i2]])
                                S.op(S.dve, lambda: nc.vector.tensor_tensor(out=actT[:, ei * 8 + fb, rhs_t], in0=gt[i2][:],
                                                                            in1=cbe[ei][:, rhs_t], op=ALU.mult),
                                     reads=[gtb[i2], cbeb[ei]], writes=[actb[ei]])
                    first, last = (grp == 0), (grp == ngrp - 1)
                    for nb in range(8):
                        wb_ = cnt["w2"] % 2; cnt["w2"] += 1
                        c0 = nb * 512
                        for ei in range(2):
                            e = grp * 2 + ei
                            S.dma(S.pool, w2t[wb_][:, ei * 8:(ei + 1) * 8, :], self.w2[e].rearrange("(kc p) n -> p kc n", p=128)[:, :, c0:c0 + 512],
                                  owner=w2b[wb_], writes=[w2b[wb_]])
                        if first:
                            S.dma(S.pool, b2t[wb_][:], self.b2[:, c0:c0 + 512], owner=b2b[wb_], writes=[b2b[wb_]])
                        if last:
                            S.dma(S.sp, gf[wb_][:], self.modd[5 * D + c0:5 * D + c0 + 512].partition_broadcast(128), owner=gfb[wb_],
                                  reads=[dd["modd"]], writes=[gfb[wb_]])
                        for t in range(8):
                            r0 = tk0 + t * 128
                            i3 = cnt["y"] % 3; i2 = cnt["y"] % 2; cnt["y"] += 1
                            pi = 6 + i2
                            if not first:
                                S.dma(S.sp, yt[i3][:], self.yd[r0:r0 + 128, c0:c0 + 512], owner=ytb[i3], reads=[dd["yd"]], writes=[ytb[i3]])
                            if last:
                                S.dma(S.sp, xr[i2][:], self.x1[r0:r0 + 128, c0:c0 + 512], owner=xrb[i2], reads=[dd["x1"]], writes=[xrb[i2]])
                            pairs = [(actT[:, kc, t * 128:(t + 1) * 128], w2t[wb_][:, kc, :]) for kc in range(16)]
                            rd = actb + [w2b[wb_]]
                            if first:
                                pairs.append((self.combT[:, r0:r0 + 128], b2t[wb_][:]))
                                rd = rd + [self.combTb, b2b[wb_]]
                            self.mm(self.ps[pi][:], self.psb[pi], pairs, reads=rd)
                            if first:
                                S.op(S.act, lambda: nc.scalar.activation(out=yt[i3][:], in_=self.ps[pi][:], func=AF.Copy),
                                     reads=[self.psb[pi]], writes=[ytb[i3]])
                            else:
                                S.op(S.dve, lambda: nc.vector.tensor_tensor(out=yt[i3][:], in0=self.ps[pi][:], in1=yt[i3][:], op=ALU.add),
                                     reads=[self.psb[pi], ytb[i3]], writes=[ytb[i3]])
                            if not last:
                                S.dma(S.sp, self.yd[r0:r0 + 128, c0:c0 + 512], yt[i3][:], owner=ytb[i3], reads=[ytb[i3]], writes=[dd["yd"]])
                            else:
                                S.op(S.dve, lambda: nc.vector.tensor_tensor(out=yt[i3][:], in0=yt[i3][:], in1=gf[wb_][:], op=ALU.mult),
                                     reads=[ytb[i3], gfb[wb_]], writes=[ytb[i3]])
                                S.op(S.dve, lambda: nc.vector.tensor_tensor(out=yt[i3][:], in0=yt[i3][:], in1=xr[i2][:], op=ALU.add),
                                     reads=[ytb[i3], xrb[i2]], writes=[ytb[i3]])
                                S.dma(S.sp, self.out[r0:r0 + 128, c0:c0 + 512], yt[i3][:], owner=ytb[i3], reads=[ytb[i3]], writes=[dd["out"]])
            S.barrier()

    def finish(self):
        pass


def _rope_tables(hf):
    l = np.arange(SEQ, dtype=np.float32)
    pos = l if hf == 0 else (np.float32(SEQ - 1) - l)
    inv = (np.float32(10000.0) ** (-np.arange(0, 128, 2, dtype=np.float32) / np.float32(128))).astype(np.float32)
    ang = pos[:, None] * inv[None, :]
    cos = np.cos(ang).astype(np.float32).T
    sin = np.sin(ang).astype(np.float32).T
    cosT = np.concatenate([cos, cos], 0)
    sinT = np.concatenate([sin, sin], 0)
    return np.ascontiguousarray(cosT), np.ascontiguousarray(sinT)


def _consts():
    r = np.zeros((128, 128), np.float32)
    for m in range(64):
        r[m + 64, m] = -1.0
    for m in range(64, 128):
        r[m - 64, m] = 1.0
    return r, np.eye(128, dtype=np.float32)


def prepare(inputs, names):
    g = lambda k: np.asarray(inputs[k])
    x = g("x") if "x" in inputs else None
    c = g("c") if "c" in inputs else None
    rperm, ident = _consts()
    tabs = [_rope_tables(0), _rope_tables(1)]
    ks = np.float32(128.0 ** -0.5)
    shared = {}
    lazy = {
        "ada_w": lambda: g("ada_w")[0], "ada_b": lambda: g("ada_b"), "w_in": lambda: g("w_in")[0],
        "nw_m": lambda: np.ascontiguousarray(g("norm_mix_w")[0].reshape(KC, 128).T),
        "nw_f": lambda: np.ascontiguousarray(g("norm_ffn_w")[0].reshape(KC, 128).T),
        "qnw": lambda: np.ascontiguousarray(g("q_norm_w")[0].reshape(128, 1)),
        "knw": lambda: np.ascontiguousarray(g("k_norm_w")[0].reshape(128, 1)),
        "rperm": lambda: rperm, "ident": lambda: ident,
        "w_ua": lambda: g("w_up_attn")[0], "w_ur": lambda: g("w_up_ret")[0], "w_out": lambda: g("w_out")[0],
        "router_w": lambda: g("router_w")[0],
        "rb": lambda: np.ascontiguousarray(np.tile(g("router_b")[0][None, :], (128, 1))),
        "w1": lambda: g("expert_w1")[0], "w2": lambda: g("expert_w2")[0], "b2": lambda: g("expert_b2")[0],
        "b1T": lambda: np.ascontiguousarray(g("expert_b1")[0].reshape(-1, 16, 128).transpose(2, 0, 1).reshape(128, -1)),
    }
    for k_, f_ in lazy.items():
        if k_ in names:
            shared[k_] = f_()
    def t5_bucket(rel):
        nb, me = 16, 8
        base = np.where(rel > 0, nb, 0)
        n = np.abs(rel)
        nf = np.maximum(n, 1).astype(np.float32)
        large = me + (np.log(nf / me) / math.log(128 / me) * (nb - me)).astype(np.int32)
        large = np.minimum(large, nb - 1)
        return base + np.where(n < me, n, large)
    rb = g("rel_bias") if "rel_bias" in inputs else None
    kk = np.arange(128)[:, None, None]
    kb_ = np.arange(3)[None, :, None]
    qq = np.arange(128)[None, None, :]
    rel_loc = (kb_ - 1) * 128 + kk - qq
    valid = np.abs(rel_loc) <= 128
    jj = np.arange(128, dtype=np.float32)
    cpos = np.maximum(jj[None, :] - jj[:, None], 0).astype(np.float32)
    cneg = np.maximum(jj[:, None] - jj[None, :], 0).astype(np.float32)
    ccol = np.stack([127 - jj, jj], 1).astype(np.float32)
    crow = np.tile(np.concatenate([jj + 1, 128 - jj])[None, :], (128, 1)).astype(np.float32)
    if "gnw" in names:
        shared.update({
            "cpos": cpos, "cneg": cneg, "ccol": ccol, "crow": crow,
            "gnw": np.ascontiguousarray(np.tile(g("ret_gn_w")[0][None, :], (128, 1))),
            "gnb": np.ascontiguousarray(np.tile(g("ret_gn_b")[0][None, :], (128, 1))),
            "sinkr": np.ascontiguousarray(np.tile(g("attn_sink")[0][None, :], (128, 1))),
        })
    maps = []
    for b in range(4):
        m = dict(shared)
        if "cT" in names:
            m["cT"] = np.ascontiguousarray(c[b].reshape(KC, 128).T)
        for hf in (0, 1):
            sfx = str(hf)
            xl = None if x is None else (x[b] if hf == 0 else x[b][::-1])
            cosT, sinT = tabs[hf]
            if "x_own" + sfx in names:
                m["x_own" + sfx] = np.ascontiguousarray(xl[:TOK])
            if "x_oth" + sfx in names:
                m["x_oth" + sfx] = np.ascontiguousarray(xl[TOK:])
            if "cosq" + sfx in names:
                m["cosq" + sfx] = np.ascontiguousarray(cosT[:, :TOK])
                m["sinq" + sfx] = np.ascontiguousarray(sinT[:, :TOK])
            if "cosk" + sfx in names:
                m["cosk" + sfx] = cosT * ks
                m["sink" + sfx] = sinT * ks
            if "biasT" + sfx in names:
                rel_o = rel_loc if hf == 0 else -rel_loc
                bt = rb[t5_bucket(rel_o)]
                bt = np.where(valid[..., None], bt, np.float32(-1e30)).astype(np.float32)
                bt = bt.reshape(128, 3, 128, 4, 4).transpose(3, 0, 1, 4, 2)
                m["biasT" + sfx] = np.ascontiguousarray(bt.reshape(4, 128, 1536))
                df, db = g("ret_decay_fwd")[0], g("ret_decay_bwd")[0]
                if hf == 1:
                    df, db = db, df
                m["dec_f" + sfx] = np.ascontiguousarray(np.tile(df[None, :], (128, 1)))
                m["dec_b" + sfx] = np.ascontiguousarray(np.tile(db[None, :], (128, 1)))
        for k in names:
            if k in inputs and k not in m:
                m[k] = inputs[k][b] if isinstance(inputs[k], (list, tuple)) else inputs[k]
        maps.append({k: m[k] for k in names})
    return maps


_NC_CACHE = {}


def kernel(**inputs):
    if "k" not in _NC_CACHE:
        k = K(halves=(0,))
        k.build()
        _NC_CACHE["k"] = k
    k = _NC_CACHE["k"]
    names0 = list(k.inputs.keys())
    per_half = [n[:-1] for n in names0 if n.endswith("0") and n[:-1] in K.USED]
    names_all = [n for n in names0 if not (n.endswith("0") and n[:-1] in K.USED)]
    names_all += [p + s for p in per_half for s in ("0", "1")]
    bmaps = prepare(inputs, names_all)
    maps = []
    for i in range(8):
        b, hf = i // 2, i % 2
        m = {}
        for n in names0:
            if n.endswith("0") and n[:-1] in K.USED:
                m[n] = bmaps[b][n[:-1] + str(hf)]
            else:
                m[n] = bmaps[b][n]
        maps.append(m)
    del bmaps
    res = run_bass_kernel_spmd(k.nc, maps, core_ids=list(range(8)))
    del maps
    out = np.empty((4, SEQ, D), np.float32)
    for i in range(8):
        b, hf = i // 2, i % 2
        o = np.asarray(res.results[i]["out0"])
        if hf == 0:
            out[b, :TOK] = o
        else:
            out[b, TOK:] = o[::-1]
    return out
```

```python
import math
from contextlib import ExitStack
import numpy as np
import concourse.bass as bass
import concourse.mybir as mybir
from concourse.bass_utils import run_bass_kernel_spmd

F32 = mybir.dt.float32
BF16 = mybir.dt.bfloat16
AF = mybir.ActivationFunctionType
ALU = mybir.AluOpType
AX = mybir.AxisListType

D = 4096
SEQ = 4096
TOK = 2048
KC = 32
NEXP = 32
DFF = 1024
EPS = 1e-6
PW = (2048, 512, 512, 1024, 1024, 2048, 2048, 4096, 4096)
OFF = [0]
for _w in PW:
    OFF.append(OFF[-1] + _w)
(O_AQ, O_AK, O_AV, O_RQ, O_RK, O_RV, O_RG, O_GA, O_GR, O_END) = OFF


class Buf:
    __slots__ = ("name", "writers", "readers", "dsems")

    def __init__(self, name):
        self.name = name
        self.writers = {}
        self.readers = {}
        self.dsems = {}


class Eng:
    def __init__(self, name, eng, sem, inorder, self_sync):
        self.name = name
        self.eng = eng
        self.sem = sem
        self.count = 0
        self.seen = {}
        self.inorder = inorder
        self.self_sync = self_sync
        self.pending = False


class Sched:
    def __init__(self, nc, stack):
        self.nc = nc
        self.stack = stack
        mk = lambda n: stack.enter_context(nc.semaphore(n))
        self.pe = Eng("pe", nc.tensor, mk("s_pe"), True, False)
        self.act = Eng("act", nc.scalar, mk("s_act"), True, True)
        self.dve = Eng("dve", nc.vector, mk("s_dve"), True, True)
        self.pool = Eng("pool", nc.gpsimd, mk("s_pool"), True, True)
        self.sp = Eng("sp", nc.sync, None, False, False)
        self.engs = [self.pe, self.act, self.dve, self.pool, self.sp]
        self.dma_bufs = []
        self.free_dsems = {}
        self.dma_owners = []
        self.nwaits = 0
        self.nins = 0

    def _wait(self, E, need):
        for sem, val in need.items():
            if E.seen.get(sem, 0) < val:
                E.eng.wait_ge(sem, val)
                E.seen[sem] = val
                self.nwaits += 1

    def _need(self, E, reads, writes):
        need = {}

        def merge(d, skip_self):
            for s, v in d.items():
                if skip_self and s is E.sem:
                    continue
                if need.get(s, 0) < v:
                    need[s] = v
        for b in reads:
            merge(b.writers, not E.self_sync)
        for b in writes:
            merge(b.writers, not E.self_sync)
            merge(b.readers, E.inorder)
        return need

    def _record(self, tok, reads, writes):
        s, v = tok
        for b in reads:
            if b.readers.get(s, 0) < v:
                b.readers[s] = v
        for b in writes:
            if b.writers.get(s, 0) < v:
                b.writers[s] = v
            b.readers = {}

    def op(self, E, emit, reads=(), writes=(), signal=True):
        self._wait(E, self._need(E, reads, writes))
        ins = emit()
        self.nins += 1
        if signal:
            E.count += 1
            ins.then_inc(E.sem, 1)
            E.pending = False
            tok = (E.sem, E.count)
        else:
            E.pending = True
            tok = (E.sem, E.count + 1)
        self._record(tok, reads, writes)
        return ins

    def dma(self, E, out, in_, owner, reads=(), writes=(), **kw):
        self._wait(E, self._need(E, reads, writes))
        ent = owner.dsems.get(E.name)
        if ent is None:
            fl = self.free_dsems.setdefault(E.name, [])
            if fl:
                ent = fl.pop()
            else:
                ent = [self.stack.enter_context(self.nc.semaphore(f"d{len(self.dma_bufs)}_{E.name}")), 0]
                self.dma_bufs.append(ent)
            owner.dsems[E.name] = ent
            self.dma_owners.append(owner)
        ins = E.eng.dma_start(out=out, in_=in_, **kw)
        ins.then_inc(ent[0], 16)
        ent[1] += 16
        self.nins += 1
        self._record((ent[0], ent[1]), reads, writes)
        return ins

    def barrier(self):
        assert not self.pe.pending
        need = {}
        for E in self.engs:
            if E.sem is not None and E.count:
                need[E.sem] = E.count
        for ent in self.dma_bufs:
            if ent[1]:
                need[ent[0]] = ent[1]
        for E in self.engs:
            self._wait(E, need)
        for o in self.dma_owners:
            for qn, ent in o.dsems.items():
                self.free_dsems.setdefault(qn, []).append(ent)
            o.dsems = {}
        self.dma_owners = []


class K:
    def set_half(self, hf):
        self.hf = hf
        h = self.hin[hf]
        self.x_own, self.x_oth = h["x_own"], h["x_oth"]
        self.cosq, self.sinq, self.cosk, self.sink_ = h["cosq"], h["sinq"], h["cosk"], h["sink"]
        self.biasT, self.dec_f, self.dec_b, self.out = h["biasT"], h["dec_f"], h["dec_b"], h["out"]
        for nm in ("x_own", "x_oth", "out"):
            self.dbufs[nm] = self.dbufs[f"{nm}{hf}"]

    def __init__(self, stop_after=99, debug=(), from_phase=1, ext_in=(), halves=(0, 1), skip=(), n_exp=NEXP):
        self.halves = tuple(halves)
        self.skip = set(skip)
        self._n_exp = n_exp
        self.from_phase = from_phase
        self.ext_in = set(ext_in)
        self.n_exp = self._n_exp
        self.stop_after = stop_after
        self.debug = set(debug)
        self.nc = bass.Bass("TRN2", target_bir_lowering=False)
        self.inputs = {}
        self.dbufs = {}

    USED = {"x_own": (3, 7), "x_oth": (2, 2), "cosq": (3, 3), "sinq": (3, 3), "cosk": (2, 3), "sink": (2, 3),
            "biasT": (4, 4), "dec_f": (5, 5), "dec_b": (5, 5), "cT": (1, 1), "ada_w": (1, 1), "ada_b": (1, 1),
            "nw_m": (1, 1), "nw_f": (1, 1), "w_in": (2, 3), "sinkr": (4, 4), "cpos": (5, 5), "cneg": (5, 5),
            "ccol": (5, 5), "crow": (5, 5), "gnw": (5, 5), "gnb": (5, 5), "w_ua": (6, 6), "w_ur": (6, 6),
            "w_out": (7, 7), "router_w": (8, 8), "rb": (8, 8), "w1": (9, 9), "b1T": (9, 9), "w2": (9, 9), "b2": (9, 9)}

    def inp(self, name, shape, dt=F32):
        base = name.rstrip("01") if name[:-1] in self.USED else name
        lo, hi = self.USED.get(base, (0, 99))
        if hi < self.from_phase or lo > self.stop_after:
            self.dbufs[name] = Buf(name)
            return self.nc.dram_tensor(name, [1] * len(shape), dt, kind="Internal").ap()
        t = self.nc.dram_tensor(name, list(shape), dt, kind="ExternalInput").ap()
        self.inputs[name] = (tuple(shape), dt)
        self.dbufs[name] = Buf(name)
        return t

    def scratch(self, name, shape, dt):
        kind = "ExternalOutput" if name in self.debug else "Internal"
        if name in self.ext_in:
            kind = "ExternalInput"
            self.inputs[name] = (tuple(shape), dt)
        t = self.nc.dram_tensor(name, list(shape), dt, kind=kind).ap()
        self.dbufs[name] = Buf(name)
        return t

    def sb(self, st, name, shape, dt):
        self._uid = getattr(self, "_uid", 0) + 1
        return st.enter_context(self.nc.sbuf_tensor(f"{name}_{self._uid}", list(shape), dt))

    def mm(self, ps_ap, psb, pairs, reads):
        S, nc = self.S, self.nc
        n = len(pairs)
        for i, (l, r) in enumerate(pairs):
            S.op(S.pe, lambda l=l, r=r, i=i: nc.tensor.matmul(ps_ap, lhsT=l, rhs=r, start=(i == 0), stop=(i == n - 1)),
                 reads=reads, writes=[psb], signal=(i == n - 1))

    def build(self):
        nc = self.nc
        with ExitStack() as gst:
            self.S = S = Sched(nc, gst)
            self.gst = gst
            self.declare()
            self.consts()
            phases = [self.p1_mod, self.p2_other, self.p3_own, self.p4_attn, self.p5_ret,
                      self.p6_up, self.p7_out, self.p8_norm2, self.p9_moe]
            for hi, hf in enumerate(self.halves):
                self.set_half(hf)
                for i, ph in enumerate(phases):
                    if i + 1 > self.stop_after:
                        break
                    if i + 1 < self.from_phase or (i == 0 and hi > 0) or (i + 1) in self.skip:
                        continue
                    ph()
                    S.barrier()
            self.finish()
        return nc

    def declare(self):
        nc = self.nc
        I = self.inp
        self.hin = [{}, {}]
        for hf in self.halves:
            for nm, shp in (("x_own", [TOK, D]), ("x_oth", [TOK, D]), ("cosq", [128, TOK]), ("sinq", [128, TOK]),
                            ("cosk", [128, SEQ]), ("sink", [128, SEQ]), ("biasT", [4, 128, 1536]),
                            ("dec_f", [128, 8]), ("dec_b", [128, 8])):
                self.hin[hf][nm] = I(f"{nm}{hf}", shp)
            self.hin[hf]["out"] = self.nc.dram_tensor(f"out{hf}", [TOK, D], F32, kind="ExternalOutput").ap()
            self.dbufs[f"out{hf}"] = Buf(f"out{hf}")
        self.cT = I("cT", [128, KC])
        self.ada_w = I("ada_w", [D, 6 * D])
        self.ada_b = I("ada_b", [1, 6 * D])
        self.nw_m = I("nw_m", [128, KC])
        self.nw_f = I("nw_f", [128, KC])
        self.w_in = I("w_in", [D, O_END])
        self.qnw = I("qnw", [128, 1])
        self.knw = I("knw", [128, 1])
        self.rperm = I("rperm", [128, 128])
        self.ident = I("ident", [128, 128])
        self.sinkr = I("sinkr", [128, 16])
        self.cpos = I("cpos", [128, 128])
        self.cneg = I("cneg", [128, 128])
        self.ccol = I("ccol", [128, 2])
        self.crow = I("crow", [128, 256])
        self.gnw = I("gnw", [128, 2048])
        self.gnb = I("gnb", [128, 2048])
        self.w_ua = I("w_ua", [2048, D])
        self.w_ur = I("w_ur", [2048, D])
        self.w_out = I("w_out", [D, D])
        self.router_w = I("router_w", [D, NEXP])
        self.rb = I("rb", [128, NEXP])
        self.w1 = I("w1", [self.n_exp, 8, 128, KC * 2 * 128])
        self.b1T = I("b1T", [128, self.n_exp * 16])
        self.w2 = I("w2", [self.n_exp, 8, 128, 8 * 512])
        self.b2 = I("b2", [NEXP, D])
        Sc = self.scratch
        self.mrg = Sc("mrg", [D, TOK], BF16)
        self.x1 = Sc("x1", [TOK, D], F32)
        self.h2d = Sc("h2d", [128, KC, 1024], BF16)
        self.yd = Sc("yd", [TOK, D], F32)
        self.combd = Sc("combd", [TOK, NEXP], F32)
        self.combTd = Sc("combTd", [NEXP, TOK], BF16)
        self.htd = Sc("htd", [128, KC, TOK], BF16)
        self.modd = Sc("modd", [6 * D], F32)
        self.qT = Sc("qT", [16, 128, TOK], BF16)
        self.kT = Sc("kT", [4, 128, TOK + 128], BF16)
        self.av = Sc("av", [TOK + 128, 512], BF16)
        self.rqT = Sc("rqT", [8, 128, TOK], BF16)
        self.rkT = Sc("rkT", [8, 128, SEQ], BF16)
        self.rkt = Sc("rkt", [SEQ, 1024], BF16)
        self.rv = Sc("rv", [SEQ, 2048], BF16)
        self.rg = Sc("rg", [TOK, 2048], BF16)
        self.sga = Sc("sga", [D, TOK], BF16)
        self.sgr = Sc("sgr", [D, TOK], BF16)
        self.HT = self.sb(self.gst, "HT", [128, KC, TOK], BF16)
        self.HTb = [Buf(f"HT{k}") for k in range(KC)]
        self.ps = [self.gst.enter_context(nc.psum_tensor(f"ps{i}", [128, 512], F32)) for i in range(8)]
        self.psb = [Buf(f"ps{i}") for i in range(8)]

    def consts(self):
        nc, S, st = self.nc, self.S, self.gst
        sb = self.sb
        self.ones_bf = sb(st, "ones_bf", [128, 128], BF16)
        self.ident_bf = sb(st, "ident_bf", [128, 128], BF16)
        self.rperm_bf = sb(st, "rperm_bf", [128, 128], BF16)
        self.eps_t = sb(st, "eps_t", [128, 1], F32)
        self.am = sb(st, "am", [128, KC], F32)
        self.bm = sb(st, "bm", [128, KC], F32)
        self.af = sb(st, "af", [128, KC], F32)
        self.bf = sb(st, "bf", [128, KC], F32)
        self.qnw_t = sb(st, "qnw_t", [128, 1], F32)
        self.knw_t = sb(st, "knw_t", [128, 1], F32)
        self.combT = sb(st, "combT", [32, TOK], BF16)
        self.combTb = Buf("combT")
        self.cb = Buf("consts")
        cb = self.cb
        S.op(S.dve, lambda: nc.vector.memset(self.ones_bf[:], 1.0), writes=[cb])
        S.op(S.dve, lambda: nc.vector.memset(self.eps_t[:], EPS), writes=[cb])
        S.dma(S.pool, self.ident_bf[:], self.ident, owner=cb, writes=[cb])
        S.dma(S.pool, self.rperm_bf[:], self.rperm, owner=cb, writes=[cb])
        S.dma(S.sp, self.qnw_t[:], self.qnw, owner=cb, writes=[cb])
        S.dma(S.sp, self.knw_t[:], self.knw, owner=cb, writes=[cb])
        import os
        if os.environ.get("K_INIT_AB"):
            for t_ in (self.am, self.af):
                S.op(S.dve, lambda: nc.vector.memset(t_[:], 1.0), writes=[cb])
            for t_ in (self.bm, self.bf):
                S.op(S.dve, lambda: nc.vector.memset(t_[:], 0.0), writes=[cb])
        S.op(S.dve, lambda: nc.vector.tensor_scalar(out=self.qnw_t[:], in0=self.qnw_t[:], scalar1=128.0 ** -0.5,
                                                     scalar2=None, op0=ALU.mult), reads=[cb], writes=[cb])

    def p1_mod(self):
        nc, S = self.nc, self.S
        with ExitStack() as st:
            sb = self.sb
            NB = 256
            wt = [sb(st, f"p1w{i}", [128, KC, NB], BF16) for i in range(2)]
            wb = [Buf(f"p1w{i}") for i in range(2)]
            cs = sb(st, "p1cs", [128, KC], F32)
            csb = sb(st, "p1csb", [128, KC], BF16)
            cbuf = Buf("p1cs")
            row = [sb(st, f"p1row{i}", [1, NB], F32) for i in range(2)]
            rowb = [Buf(f"p1row{i}") for i in range(2)]
            adab = [sb(st, f"p1adab{i}", [1, NB], F32) for i in range(2)]
            adabb = [Buf(f"p1adab{i}") for i in range(2)]
            S.dma(S.sp, cs[:], self.cT, owner=cbuf, writes=[cbuf])
            S.op(S.act, lambda: nc.scalar.activation(out=csb[:], in_=cs[:], func=AF.Silu), reads=[cbuf], writes=[cbuf])
            wv = self.ada_w.rearrange("(kc p) n -> p kc n", p=128)
            md = self.dbufs["modd"]
            moddv = self.modd.rearrange("(a n) -> a n", a=1)
            nblk = 6 * D // NB
            for nb in range(nblk):
                b = nb % 2
                S.dma(S.pool, wt[b][:], wv[:, :, nb * NB:(nb + 1) * NB], owner=wb[b], writes=[wb[b]])
                S.dma(S.sp, adab[b][:], self.ada_b[0:1, nb * NB:(nb + 1) * NB], owner=adabb[b], writes=[adabb[b]])
                pi = nb % 2
                self.mm(self.ps[pi][0:1, 0:NB], self.psb[pi],
                        [(csb[:, kc:kc + 1], wt[b][:, kc, :]) for kc in range(KC)], reads=[cbuf, wb[b]])
                S.op(S.dve, lambda: nc.vector.tensor_tensor(out=row[b][:], in0=self.ps[pi][0:1, 0:NB],
                                                            in1=adab[b][:], op=ALU.add),
                     reads=[self.psb[pi], adabb[b]], writes=[rowb[b]])
                S.dma(S.sp, moddv[0:1, nb * NB:(nb + 1) * NB], row[b][:], owner=rowb[b], reads=[rowb[b]], writes=[md])
            tmp = sb(st, "p1tmp", [128, 4, KC], F32)
            tb = Buf("p1tmp")
            nwm = sb(st, "p1nwm", [128, KC], F32)
            nwf = sb(st, "p1nwf", [128, KC], F32)
            S.dma(S.sp, nwm[:], self.nw_m, owner=tb, writes=[tb])
            S.dma(S.sp, nwf[:], self.nw_f, owner=tb, writes=[tb])
            for j, ci in enumerate((0, 1, 3, 4)):
                S.dma(S.sp, tmp[:, j, :], self.modd[ci * D:(ci + 1) * D].rearrange("(j p) -> p j", p=128),
                      owner=tb, reads=[md], writes=[tb], allow_slow_non_contiguous=True)
            cb = self.cb
            S.op(S.dve, lambda: nc.vector.scalar_tensor_tensor(out=self.am[:], in0=tmp[:, 1, :], scalar=1.0, in1=nwm[:],
                                                               op0=ALU.add, op1=ALU.mult), reads=[tb], writes=[cb])
            S.op(S.dve, lambda: nc.vector.tensor_copy(out=self.bm[:], in_=tmp[:, 0, :]), reads=[tb], writes=[cb])
            S.op(S.dve, lambda: nc.vector.scalar_tensor_tensor(out=self.af[:], in0=tmp[:, 3, :], scalar=1.0, in1=nwf[:],
                                                               op0=ALU.add, op1=ALU.mult), reads=[tb], writes=[cb])
            S.op(S.dve, lambda: nc.vector.tensor_copy(out=self.bf[:], in_=tmp[:, 2, :]), reads=[tb], writes=[cb])
            S.barrier()

    def norm_T(self, xsrc, xname, ntiles, a_t, b_t):
        nc, S = self.nc, self.S
        with ExitStack() as st:
            sb = self.sb
            xt = [sb(st, f"nx{i}", [128, D], F32) for i in range(2)]
            xb = [Buf(f"nx{i}") for i in range(2)]
            xn = [sb(st, f"nxn{i}", [128, D], BF16) for i in range(2)]
            xnb = [Buf(f"nxn{i}") for i in range(2)]
            junk = sb(st, "njunk", [128, D], BF16)
            jb = Buf("njunk")
            stt = [sb(st, f"nst{i}", [128, 4], F32) for i in range(2)]
            stb = [Buf(f"nst{i}") for i in range(2)]
            xd = self.dbufs[xname]
            import os
            NTS = int(os.environ.get("NT_STOP", "9"))
            ntiles = int(os.environ.get("NT_TILES", ntiles))
            for t in range(ntiles):
                b = t % 2
                S.dma(S.sp, xt[b][:], xsrc[t * 128:(t + 1) * 128, :], owner=xb[b], reads=[xd], writes=[xb[b]])
                s_ = stt[b]
                if NTS < 1:
                    continue
                S.op(S.act, lambda: nc.scalar.activation(out=junk[:], in_=xt[b][:], func=AF.Square, accum_out=s_[:, 0:1]),
                     reads=[xb[b]], writes=[jb, stb[b]])
                if NTS < 2:
                    continue
                S.op(S.act, lambda: nc.scalar.activation(out=s_[:, 1:2], in_=s_[:, 0:1], func=AF.Sqrt,
                                                         bias=self.eps_t[:], scale=1.0 / D),
                     reads=[stb[b], self.cb], writes=[stb[b]])
                S.op(S.dve, lambda: nc.vector.reciprocal(out=s_[:, 2:3], in_=s_[:, 1:2]), reads=[stb[b]], writes=[stb[b]])
                if NTS < 3:
                    continue
                S.op(S.dve, lambda: nc.vector.tensor_scalar(out=xn[b][:], in0=xt[b][:], scalar1=s_[:, 2:3], scalar2=None,
                                                            op0=ALU.mult), reads=[xb[b], stb[b]], writes=[xnb[b]])
                if NTS < 4:
                    continue
                for g in range(8):
                    pi = g % 4
                    pv = self.ps[pi][:].bitcast(BF16)
                    for j in range(4):
                        kc = g * 4 + j
                        S.op(S.pe, lambda kc=kc, j=j: nc.tensor.transpose(out=pv[:, j * 128:(j + 1) * 128],
                                                                         in_=xn[b][:, kc * 128:(kc + 1) * 128],
                                                                         identity=self.ident_bf[:]),
                             reads=[xnb[b], self.cb], writes=[self.psb[pi]], signal=(j == 3))
                    if NTS < 5:
                        continue
                    for j in range(4):
                        kc = g * 4 + j
                        dst = self.HT[:, kc, t * 128:(t + 1) * 128]
                        ev = os.environ.get("NT_EV", "alt")
                        if (g % 2 == 0 and ev == "alt") or ev == "act":
                            S.op(S.act, lambda kc=kc, j=j, dst=dst: nc.scalar.activation(
                                out=dst, in_=pv[:, j * 128:(j + 1) * 128], func=AF.Identity,
                                bias=b_t[:, kc:kc + 1], scale=a_t[:, kc:kc + 1]),
                                reads=[self.psb[pi], self.cb], writes=[self.HTb[kc]])
                        else:
                            S.op(S.dve, lambda kc=kc, j=j, dst=dst: nc.vector.tensor_scalar(
                                out=dst, in0=pv[:, j * 128:(j + 1) * 128], scalar1=a_t[:, kc:kc + 1],
                                scalar2=b_t[:, kc:kc + 1], op0=ALU.mult, op1=ALU.add),
                                reads=[self.psb[pi], self.cb], writes=[self.HTb[kc]])
            S.barrier()

    def project(self, w_ap, wname, jobs, tag):
        nc, S = self.nc, self.S
        wv = w_ap.rearrange("(kc p) n -> p kc n", p=128)
        wd = self.dbufs[wname]
        for i, (c0, fn) in enumerate(jobs):
            b = i % 2
            S.dma(S.pool, self.wt[b][:], wv[:, :, c0:c0 + 256], owner=self.wtb[b], reads=[wd], writes=[self.wtb[b]])
            fn(self.wt[b], self.wtb[b], c0)

    def _psrot(self):
        self._pr = (self._pr + 1) % 4
        return self._pr

    def fm_block(self, wt, wb, sub, t0, n):
        pi = self._psrot()
        self.mm(self.ps[pi][:, 0:n], self.psb[pi],
                [(wt[:, kc, sub * 128:(sub + 1) * 128], self.HT[:, kc, t0:t0 + n]) for kc in range(KC)],
                reads=[wb] + self.HTb)
        return pi

    def tm_block(self, wt, wb, t0):
        pi = self._psrot()
        self.mm(self.ps[pi][:, 0:256], self.psb[pi],
                [(self.HT[:, kc, t0:t0 + 128], wt[:, kc, :]) for kc in range(KC)],
                reads=[wb] + self.HTb)
        return pi

    def ep_alloc(self, st):
        sb = self.sb
        self.e_bf = [sb(st, f"e_bf{i}", [128, 512], BF16) for i in range(2)]
        self.e_bfb = [Buf(f"e_bf{i}") for i in range(2)]
        self.e_f = [sb(st, f"e_f{i}", [128, 512], F32) for i in range(4)]
        self.e_fb = [Buf(f"e_f{i}") for i in range(4)]
        self.e_o = [sb(st, f"e_o{i}", [128, 512], BF16) for i in range(3)]
        self.e_ob = [Buf(f"e_o{i}") for i in range(3)]
        self.e_tab = [sb(st, f"e_tab{i}", [128, 2, 512], F32) for i in range(2)]
        self.e_tabb = [Buf(f"e_tab{i}") for i in range(2)]
        self.e_kt = [sb(st, f"e_kt{i}", [128, 512], BF16) for i in range(2)]
        self.e_ktb = [Buf(f"e_kt{i}") for i in range(2)]
        self._eo = 0
        self._ebf = 0
        self._ef = 0
        self._etab = 0
        self._ekt = 0

    def _rot(self, attr, n):
        v = getattr(self, attr)
        setattr(self, attr, (v + 1) % n)
        return v

    def ep_qknorm(self, pi, n, wcol, dst_ap, dname):
        nc, S = self.nc, self.S
        P1 = self.ps[pi][:, 0:n]
        bi = self._rot("_ebf", 2)
        sq, sqb = self.e_bf[bi], self.e_bfb[bi]
        S.op(S.act, lambda: nc.scalar.activation(out=sq[:, 0:n], in_=P1, func=AF.Square), reads=[self.psb[pi]], writes=[sqb])
        p2 = 4 + (pi % 2)
        self.mm(self.ps[p2][:, 0:n], self.psb[p2], [(self.ones_bf[:], sq[:, 0:n])], reads=[sqb, self.cb])
        fi = self._rot("_ef", 4)
        rt, rtb = self.e_f[fi], self.e_fb[fi]
        S.op(S.act, lambda: nc.scalar.activation(out=rt[:, 0:n], in_=self.ps[p2][:, 0:n], func=AF.Sqrt,
                                                 bias=self.eps_t[:], scale=1.0 / 128),
             reads=[self.psb[p2], self.cb], writes=[rtb])
        S.op(S.dve, lambda: nc.vector.reciprocal(out=rt[:, 0:n], in_=rt[:, 0:n]), reads=[rtb], writes=[rtb])
        oi = self._rot("_eo", 3)
        o, ob = self.e_o[oi], self.e_ob[oi]
        S.op(S.dve, lambda: nc.vector.scalar_tensor_tensor(out=o[:, 0:n], in0=P1, scalar=wcol[:, 0:1], in1=rt[:, 0:n],
                                                           op0=ALU.mult, op1=ALU.mult),
             reads=[self.psb[pi], rtb, self.cb], writes=[ob])
        S.dma(S.sp, dst_ap, o[:, 0:n], owner=ob, reads=[ob], writes=[self.dbufs[dname]])

    def ep_rope(self, pi, n, cos_ap, sin_ap, tabkey, dst_ap, dname, tok_dst=None):
        nc, S = self.nc, self.S
        P1 = self.ps[pi][:, 0:n]
        if self._tabkey != tabkey:
            ti = self._rot("_etab", 2)
            tab, tabb = self.e_tab[ti], self.e_tabb[ti]
            S.dma(S.sp, tab[:, 0, 0:n], cos_ap, owner=tabb, writes=[tabb])
            S.dma(S.sp, tab[:, 1, 0:n], sin_ap, owner=tabb, writes=[tabb])
            self._tabkey = tabkey
            self._tab = (tab, tabb)
        tab, tabb = self._tab
        bi = self._rot("_ebf", 2)
        xb, xbb = self.e_bf[bi], self.e_bfb[bi]
        S.op(S.act, lambda: nc.scalar.activation(out=xb[:, 0:n], in_=P1, func=AF.Copy), reads=[self.psb[pi]], writes=[xbb])
        p2 = 4 + (pi % 2)
        self.mm(self.ps[p2][:, 0:n], self.psb[p2], [(self.rperm_bf[:], xb[:, 0:n])], reads=[xbb, self.cb])
        f1 = self._rot("_ef", 4)
        t1, t1b = self.e_f[f1], self.e_fb[f1]
        S.op(S.dve, lambda: nc.vector.tensor_tensor(out=t1[:, 0:n], in0=P1, in1=tab[:, 0, 0:n], op=ALU.mult),
             reads=[self.psb[pi], tabb, xbb], writes=[t1b])
        f2 = self._rot("_ef", 4)
        t2, t2b = self.e_f[f2], self.e_fb[f2]
        S.op(S.dve, lambda: nc.vector.tensor_tensor(out=t2[:, 0:n], in0=self.ps[p2][:, 0:n], in1=tab[:, 1, 0:n], op=ALU.mult),
             reads=[self.psb[p2], tabb], writes=[t2b])
        oi = self._rot("_eo", 3)
        o, ob = self.e_o[oi], self.e_ob[oi]
        S.op(S.dve, lambda: nc.vector.tensor_tensor(out=o[:, 0:n], in0=t1[:, 0:n], in1=t2[:, 0:n], op=ALU.add),
             reads=[t1b, t2b], writes=[ob])
        S.dma(S.sp, dst_ap, o[:, 0:n], owner=ob, reads=[ob], writes=[self.dbufs[dname]])
        if tok_dst is not None:
            p3 = 6 + (pi % 2)
            pv = self.ps[p3][:].bitcast(BF16)
            nb = n // 128
            for j in range(nb):
                S.op(S.pe, lambda j=j: nc.tensor.transpose(out=pv[:, j * 128:(j + 1) * 128], in_=o[:, j * 128:(j + 1) * 128],
                                                           identity=self.ident_bf[:]),
                     reads=[ob, self.cb], writes=[self.psb[p3]], signal=(j == nb - 1))
            ki = self._rot("_ekt", 2)
            kt, ktb = self.e_kt[ki], self.e_ktb[ki]
            S.op(S.act, lambda: nc.scalar.activation(out=kt[:, 0:n], in_=pv[:, 0:n], func=AF.Copy),
                 reads=[self.psb[p3]], writes=[ktb])
            dst, dn = tok_dst
            S.dma(S.sp, dst, kt[:, 0:n].rearrange("p (j d) -> p j d", d=128), owner=ktb, reads=[ktb], writes=[self.dbufs[dn]])

    def ep_act(self, pi, n, func, dst_ap, dname):
        nc, S = self.nc, self.S
        oi = self._rot("_eo", 3)
        o, ob = self.e_o[oi], self.e_ob[oi]
        S.op(S.act, lambda: nc.scalar.activation(out=o[:, 0:n], in_=self.ps[pi][:, 0:n], func=func),
             reads=[self.psb[pi]], writes=[ob])
        S.dma(S.sp, dst_ap, o[:, 0:n], owner=ob, reads=[ob], writes=[self.dbufs[dname]])

    def proj_phase(self, own):
        nc, S = self.nc, self.S
        with ExitStack() as st:
            self.wt = [self.sb(st, f"wt{i}", [128, KC, 256], BF16) for i in range(2)]
            self.wtb = [Buf(f"wt{i}") for i in range(2)]
            self.ep_alloc(st)
            self._pr = 0
            self._tabkey = None
            base = 0 if own else TOK
            jobs = []

            def fm_job(c0, ep):
                def fn(wt, wb, c0_, ep=ep):
                    for sub in range(2):
                        ep(wt, wb, sub, c0_ + sub * 128)
                jobs.append((c0, fn))

            def tm_job(c0, ep, ntile):
                def fn(wt, wb, c0_, ep=ep):
                    for t in range(ntile):
                        pi = self.tm_block(wt, wb, t * 128)
                        ep(pi, t, c0_)
                jobs.append((c0, fn))

            def ep_rk(wt, wb, sub, col):
                h = (col - O_RK) // 128
                for tb in range(4):
                    pi = self.fm_block(wt, wb, sub, tb * 512, 512)
                    l0 = base + tb * 512
                    self.ep_rope(pi, 512, self.cosk[:, l0:l0 + 512], self.sink_[:, l0:l0 + 512], ("k", l0),
                                 self.rkT[h, :, l0:l0 + 512], "rkT",
                                 tok_dst=(self.rkt[l0:l0 + 512, h * 128:(h + 1) * 128].rearrange("(j p) d -> p j d", p=128), "rkt"))
            for c0 in range(O_RK, O_RV, 256):
                fm_job(c0, ep_rk)

            def ep_rv(pi, t, c0_):
                l0 = base + t * 128
                self.ep_act(pi, 256, AF.Copy, self.rv[l0:l0 + 128, c0_ - O_RV:c0_ - O_RV + 256], "rv")
            for c0 in range(O_RV, O_RG, 256):
                tm_job(c0, ep_rv, 16)

            def ep_ak(wt, wb, sub, col):
                g = (col - O_AK) // 128
                if own:
                    for tb in range(4):
                        pi = self.fm_block(wt, wb, sub, tb * 512, 512)
                        self.ep_qknorm(pi, 512, self.knw_t, self.kT[g, :, tb * 512:(tb + 1) * 512], "kT")
                else:
                    pi = self.fm_block(wt, wb, sub, 0, 128)
                    self.ep_qknorm(pi, 128, self.knw_t, self.kT[g, :, TOK:TOK + 128], "kT")
            for c0 in range(O_AK, O_AV, 256):
                fm_job(c0, ep_ak)

            def ep_av(pi, t, c0_):
                l0 = base + t * 128
                self.ep_act(pi, 256, AF.Copy, self.av[l0:l0 + 128, c0_ - O_AV:c0_ - O_AV + 256], "av")
            for c0 in range(O_AV, O_RQ, 256):
                tm_job(c0, ep_av, 16 if own else 1)

            if own:
                def ep_aq(wt, wb, sub, col):
                    h = (col - O_AQ) // 128
                    for tb in range(4):
                        pi = self.fm_block(wt, wb, sub, tb * 512, 512)
                        self.ep_qknorm(pi, 512, self.qnw_t, self.qT[h, :, tb * 512:(tb + 1) * 512], "qT")
                for c0 in range(O_AQ, O_AK, 256):
                    fm_job(c0, ep_aq)

                def ep_rq(wt, wb, sub, col):
                    h = (col - O_RQ) // 128
                    for tb in range(4):
                        pi = self.fm_block(wt, wb, sub, tb * 512, 512)
                        l0 = tb * 512
                        self.ep_rope(pi, 512, self.cosq[:, l0:l0 + 512], self.sinq[:, l0:l0 + 512], ("q", l0),
                                     self.rqT[h, :, l0:l0 + 512], "rqT")
                for c0 in range(O_RQ, O_RK, 256):
                    fm_job(c0, ep_rq)

                def ep_rg(pi, t, c0_):
                    self.ep_act(pi, 256, AF.Silu, self.rg[t * 128:(t + 1) * 128, c0_ - O_RG:c0_ - O_RG + 256], "rg")
                for c0 in range(O_RG, O_GA, 256):
                    tm_job(c0, ep_rg, 16)

                def ep_g(wt, wb, sub, col):
                    if col < O_GR:
                        dst, dn, f0 = self.sga, "sga", col - O_GA
                    else:
                        dst, dn, f0 = self.sgr, "sgr", col - O_GR
                    for tb in range(4):
                        pi = self.fm_block(wt, wb, sub, tb * 512, 512)
                        self.ep_act(pi, 512, AF.Sigmoid, dst[f0:f0 + 128, tb * 512:(tb + 1) * 512], dn)
                for c0 in range(O_GA, O_END, 256):
                    fm_job(c0, ep_g)

            self.project(self.w_in, "w_in", jobs, "in")
            S.barrier()

    def p2_other(self):
        import os
        sk = os.environ.get("KSKIP", "")
        if "normT" not in sk:
            self.norm_T(self.x_oth, "x_oth", 16, self.am, self.bm)
        if "proj" not in sk:
            self.proj_phase(False)

    def p3_own(self):
        self.norm_T(self.x_own, "x_own", 16, self.am, self.bm)
        self.proj_phase(True)


    def p4_attn(self):
        nc, S = self.nc, self.S
        with ExitStack() as st:
            sb = self.sb
            qg = sb(st, "a_q", [128, 4, TOK], BF16); qgb = Buf("a_q")
            kg = sb(st, "a_k", [128, TOK + 128], BF16); kgb = Buf("a_k")
            vg = sb(st, "a_v", [128, 17, 128], BF16); vgb = Buf("a_v")
            bg = sb(st, "a_b", [128, 1536], F32); bgb = Buf("a_b")
            es = sb(st, "a_es", [128, 16], F32); esb = Buf("a_es")
            lg = [sb(st, f"a_lg{i}", [128, 512], F32) for i in range(3)]
            lgb = [Buf(f"a_lg{i}") for i in range(3)]
            pt = [sb(st, f"a_pt{i}", [128, 512], BF16) for i in range(6)]
            ptb = [Buf(f"a_pt{i}") for i in range(6)]
            dn = [sb(st, f"a_dn{i}", [128, 512], F32) for i in range(2)]
            dnb = [Buf(f"a_dn{i}") for i in range(2)]
            S.dma(S.sp, es[:], self.sinkr, owner=esb, writes=[esb])
            S.op(S.act, lambda: nc.scalar.activation(out=es[:], in_=es[:], func=AF.Exp), reads=[esb], writes=[esb])
            u = 0
            for g in range(4):
                S.dma(S.sp, qg[:], self.qT[4 * g:4 * g + 4].rearrange("h d t -> d h t"), owner=qgb,
                      reads=[self.dbufs["qT"]], writes=[qgb])
                S.dma(S.sp, kg[:], self.kT[g], owner=kgb, reads=[self.dbufs["kT"]], writes=[kgb])
                S.dma(S.sp, vg[:], self.av[:, g * 128:(g + 1) * 128].rearrange("(n p) d -> p n d", p=128), owner=vgb,
                      reads=[self.dbufs["av"]], writes=[vgb])
                S.dma(S.sp, bg[:], self.biasT[g], owner=bgb, writes=[bgb])
                for qb in range(16):
                    kbs = [kb for kb in range(3) if qb + kb - 1 >= 0]
                    sbank = [0, 1, 2] if u % 2 == 0 else [5, 6, 7]
                    pts = []
                    for kb in kbs:
                        blk = qb + kb - 1
                        pi = sbank[kb]
                        self.mm(self.ps[pi][:].rearrange("p (h q) -> p h q", h=4), self.psb[pi],
                                [(kg[:, blk * 128:(blk + 1) * 128], qg[:, :, qb * 128:(qb + 1) * 128])],
                                reads=[kgb, qgb])
                        S.op(S.dve, lambda: nc.vector.tensor_tensor(out=lg[kb][:], in0=self.ps[pi][:],
                                                                    in1=bg[:, kb * 512:(kb + 1) * 512], op=ALU.add),
                             reads=[self.psb[pi], bgb], writes=[lgb[kb]])
                        pj = (u % 2) * 3 + kb
                        S.op(S.act, lambda: nc.scalar.activation(out=pt[pj][:], in_=lg[kb][:], func=AF.Exp),
                             reads=[lgb[kb]], writes=[ptb[pj]])
                        pts.append((blk, pj))
                    self.mm(self.ps[3][:], self.psb[3], [(vg[:, blk, :], pt[pj][:]) for blk, pj in pts],
                            reads=[vgb] + [ptb[pj] for _, pj in pts])
                    self.mm(self.ps[4][:], self.psb[4], [(self.ones_bf[:], pt[pj][:]) for blk, pj in pts],
                            reads=[self.cb] + [ptb[pj] for _, pj in pts])
                    d_ = dn[u % 2]; db_ = dnb[u % 2]
                    for hh in range(4):
                        h = 4 * g + hh
                        S.op(S.dve, lambda: nc.vector.tensor_scalar(out=d_[:, hh * 128:(hh + 1) * 128],
                                                                    in0=self.ps[4][:, hh * 128:(hh + 1) * 128],
                                                                    scalar1=es[:, h:h + 1], scalar2=None, op0=ALU.add),
                             reads=[self.psb[4], esb], writes=[db_])
                    S.op(S.dve, lambda: nc.vector.reciprocal(out=d_[:], in_=d_[:]), reads=[db_], writes=[db_])
                    S.op(S.dve, lambda: nc.vector.tensor_tensor(
                        out=self.HT[:, 4 * g:4 * g + 4, qb * 128:(qb + 1) * 128],
                        in0=self.ps[3][:].rearrange("p (h q) -> p h q", h=4),
                        in1=d_[:].rearrange("p (h q) -> p h q", h=4), op=ALU.mult),
                        reads=[self.psb[3], db_], writes=self.HTb[4 * g:4 * g + 4])
                    u += 1
            S.barrier()

    def p5_ret(self):
        nc, S = self.nc, self.S
        with ExitStack() as st:
            sb = self.sb
            tb = Buf("r_tabs")
            lgf = sb(st, "r_lgf", [128, 8], F32)
            lgb_ = sb(st, "r_lgb", [128, 8], F32)
            cpos = sb(st, "r_cpos", [128, 128], F32)
            cneg = sb(st, "r_cneg", [128, 128], F32)
            ccol = sb(st, "r_ccol", [128, 2], F32)
            crow = sb(st, "r_crow", [128, 256], F32)
            cdf = sb(st, "r_cdf", [128, 8], F32)
            cdb = sb(st, "r_cdb", [128, 8], F32)
            tmp = sb(st, "r_tmp", [128, 128], F32)
            for dst, src_ in ((lgf, self.dec_f), (lgb_, self.dec_b), (cpos, self.cpos), (cneg, self.cneg),
                              (ccol, self.ccol), (crow, self.crow)):
                S.dma(S.sp, dst[:], src_, owner=tb, writes=[tb])
            for t_ in (lgf, lgb_):
                S.op(S.act, lambda: nc.scalar.activation(out=t_[:], in_=t_[:], func=AF.Exp), reads=[tb], writes=[tb])
                S.op(S.dve, lambda: nc.vector.tensor_scalar(out=t_[:], in0=t_[:], scalar1=-1.0, scalar2=None, op0=ALU.mult),
                     reads=[tb], writes=[tb])
            S.op(S.act, lambda: nc.scalar.activation(out=cdf[:], in_=lgf[:], func=AF.Exp, scale=128.0), reads=[tb], writes=[tb])
            S.op(S.act, lambda: nc.scalar.activation(out=cdb[:], in_=lgb_[:], func=AF.Exp, scale=128.0), reads=[tb], writes=[tb])
            hb = Buf("r_htab")
            DT = sb(st, "r_DT", [128, 128], F32)
            kdf = sb(st, "r_kdf", [128, 1], F32)
            kdb = sb(st, "r_kdb", [128, 1], F32)
            qdf = sb(st, "r_qdf", [128, 128], F32)
            qdb = sb(st, "r_qdb", [128, 128], F32)
            gnw = sb(st, "r_gnw", [128, 256], F32)
            gnb = sb(st, "r_gnb", [128, 256], F32)

            kt = sb(st, "r_kt", [128, 32, 128], BF16); ktb = Buf("r_kt")
            vv = sb(st, "r_v", [128, 32, 256], BF16); vvb = Buf("r_v")
            qT = sb(st, "r_qT", [128, TOK], BF16); qTb = Buf("r_qT")
            kT = sb(st, "r_kT", [128, TOK], BF16); kTb = Buf("r_kT")
            rgt = [sb(st, f"r_rg{i}", [128, 256], BF16) for i in range(2)]; rgb = [Buf(f"r_rg{i}") for i in range(2)]
            kdec = sb(st, "r_kdec", [128, 8, 128], BF16); kdecb = [Buf(f"r_kdec{i}") for i in range(8)]
            KV = sb(st, "r_KV", [128, 8, 256], F32); KVb = [Buf(f"r_KV{i}") for i in range(8)]
            Sf = sb(st, "r_Sf", [128, 16, 256], BF16); Sfb = [Buf(f"r_Sf{i}") for i in range(16)]
            Sb_ = sb(st, "r_Sb", [128, 16, 256], BF16); Sbb = [Buf(f"r_Sb{i}") for i in range(16)]
            run = sb(st, "r_run", [128, 2, 256], F32); runb = [Buf("r_runf"), Buf("r_runb")]
            scd = [sb(st, f"r_scd{i}", [128, 128], BF16) for i in range(2)]; scdb = [Buf(f"r_scd{i}") for i in range(2)]
            qfb = [sb(st, f"r_qfb{i}", [128, 2, 128], BF16) for i in range(2)]; qfbb = [Buf(f"r_qfb{i}") for i in range(2)]
            stt = [sb(st, f"r_st{i}", [128, 12], F32) for i in range(2)]; sttb = [Buf(f"r_st{i}") for i in range(2)]
            yn = [sb(st, f"r_yn{i}", [128, 256], F32) for i in range(2)]; ynb = [Buf(f"r_yn{i}") for i in range(2)]
            yo = [sb(st, f"r_yo{i}", [128, 256], BF16) for i in range(2)]; yob = [Buf(f"r_yo{i}") for i in range(2)]
            dd = self.dbufs

            def states(h, chunks, kd, cd, ri, store):
                for g0 in range(0, len(chunks), 8):
                    grp = chunks[g0:g0 + 8]
                    for i, n in enumerate(grp):
                        eng, ns = (S.dve, nc.vector) if i % 2 else (S.pool, nc.gpsimd)
                        S.op(eng, lambda: ns.tensor_scalar(out=kdec[:, i, :], in0=kt[:, n, :], scalar1=kd[:, 0:1], scalar2=None,
                                                           op0=ALU.mult), reads=[ktb, hb], writes=[kdecb[i]])
                    for i, n in enumerate(grp):
                        pi = i % 4
                        half = i // 4
                        pa = self.ps[pi][:, half * 256:(half + 1) * 256]
                        self.mm(pa, self.psb[pi], [(kdec[:, i, :], vv[:, n, :])], reads=[kdecb[i], vvb])
                        S.op(S.act, lambda: nc.scalar.activation(out=KV[:, i, :], in_=pa, func=AF.Copy),
                             reads=[self.psb[pi]], writes=[KVb[i]])
                    for i, n in enumerate(grp):
                        S.op(S.dve, lambda: nc.vector.scalar_tensor_tensor(out=run[:, ri, :], in0=run[:, ri, :], scalar=cd,
                                                                           in1=KV[:, i, :], op0=ALU.mult, op1=ALU.add),
                             reads=[KVb[i], tb, runb[ri]], writes=[runb[ri]])
                        store(n)

            for h in range(8):
                S.dma(S.sp, kt[:], self.rkt[:, h * 128:(h + 1) * 128].rearrange("(n p) d -> p n d", p=128), owner=ktb,
                      reads=[dd["rkt"]], writes=[ktb])
                S.dma(S.sp, vv[:], self.rv[:, h * 256:(h + 1) * 256].rearrange("(n p) e -> p n e", p=128), owner=vvb,
                      reads=[dd["rv"]], writes=[vvb])
                S.dma(S.sp, qT[:], self.rqT[h], owner=qTb, reads=[dd["rqT"]], writes=[qTb])
                S.dma(S.sp, kT[:], self.rkT[h, :, 0:TOK], owner=kTb, reads=[dd["rkT"]], writes=[kTb])
                S.dma(S.sp, gnw[:], self.gnw[:, h * 256:(h + 1) * 256], owner=hb, writes=[hb])
                S.dma(S.sp, gnb[:], self.gnb[:, h * 256:(h + 1) * 256], owner=hb, writes=[hb])
                S.op(S.dve, lambda: nc.vector.tensor_scalar(out=tmp[:], in0=cpos[:], scalar1=lgf[:, h:h + 1], scalar2=None,
                                                            op0=ALU.mult), reads=[tb], writes=[hb])
                S.op(S.dve, lambda: nc.vector.scalar_tensor_tensor(out=tmp[:], in0=cneg[:], scalar=lgb_[:, h:h + 1], in1=tmp[:],
                                                                   op0=ALU.mult, op1=ALU.add), reads=[tb, hb], writes=[hb])
                S.op(S.act, lambda: nc.scalar.activation(out=DT[:], in_=tmp[:], func=AF.Exp), reads=[hb], writes=[hb])
                S.op(S.act, lambda: nc.scalar.activation(out=kdf[:], in_=ccol[:, 0:1], func=AF.Exp, scale=lgf[:, h:h + 1]),
                     reads=[tb], writes=[hb])
                S.op(S.act, lambda: nc.scalar.activation(out=kdb[:], in_=ccol[:, 1:2], func=AF.Exp, scale=lgb_[:, h:h + 1]),
                     reads=[tb], writes=[hb])
                S.op(S.act, lambda: nc.scalar.activation(out=qdf[:], in_=crow[:, 0:128], func=AF.Exp, scale=lgf[:, h:h + 1]),
                     reads=[tb], writes=[hb])
                S.op(S.act, lambda: nc.scalar.activation(out=qdb[:], in_=crow[:, 128:256], func=AF.Exp, scale=lgb_[:, h:h + 1]),
                     reads=[tb], writes=[hb])
                S.op(S.dve, lambda: nc.vector.memset(run[:, 1, :], 0.0), writes=[runb[1]])

                def store_b(n):
                    if n - 1 <= 15:
                        S.op(S.act, lambda: nc.scalar.activation(out=Sb_[:, n - 1, :], in_=run[:, 1, :], func=AF.Copy),
                             reads=[runb[1]], writes=[Sbb[n - 1]])
                states(h, list(range(31, 0, -1)), kdb, cdb[:, h:h + 1], 1, store_b)
                S.op(S.dve, lambda: nc.vector.memset(run[:, 0, :], 0.0), writes=[runb[0]])
                S.op(S.act, lambda: nc.scalar.activation(out=Sf[:, 0, :], in_=run[:, 0, :], func=AF.Copy),
                     reads=[runb[0]], writes=[Sfb[0]])

                def store_f(n):
                    S.op(S.act, lambda: nc.scalar.activation(out=Sf[:, n + 1, :], in_=run[:, 0, :], func=AF.Copy),
                         reads=[runb[0]], writes=[Sfb[n + 1]])
                states(h, list(range(0, 15)), kdf, cdf[:, h:h + 1], 0, store_f)
                for n in range(16):
                    b = n % 2
                    c0, c1 = n * 128, (n + 1) * 128
                    S.dma(S.sp, rgt[b][:], self.rg[c0:c1, h * 256:(h + 1) * 256], owner=rgb[b], reads=[dd["rg"]], writes=[rgb[b]])
                    p_s = 4 + b
                    self.mm(self.ps[p_s][:, 0:128], self.psb[p_s], [(kT[:, c0:c1], qT[:, c0:c1])], reads=[kTb, qTb])
                    S.op(S.dve, lambda: nc.vector.tensor_tensor(out=scd[b][:], in0=self.ps[p_s][:, 0:128], in1=DT[:], op=ALU.mult),
                         reads=[self.psb[p_s], hb], writes=[scdb[b]])
                    S.op(S.pool, lambda: nc.gpsimd.tensor_tensor(out=qfb[b][:, 0, :], in0=qT[:, c0:c1], in1=qdf[:], op=ALU.mult),
                         reads=[qTb, hb], writes=[qfbb[b]])
                    S.op(S.pool, lambda: nc.gpsimd.tensor_tensor(out=qfb[b][:, 1, :], in0=qT[:, c0:c1], in1=qdb[:], op=ALU.mult),
                         reads=[qTb, hb], writes=[qfbb[b]])
                    p_y = 6 + b
                    self.mm(self.ps[p_y][:, 0:256], self.psb[p_y],
                            [(scd[b][:], vv[:, n, :]), (qfb[b][:, 0, :], Sf[:, n, :]), (qfb[b][:, 1, :], Sb_[:, n, :])],
                            reads=[scdb[b], vvb, qfbb[b], Sfb[n], Sbb[n]])
                    y = self.ps[p_y][:, 0:256]
                    s_ = stt[b]
                    S.op(S.dve, lambda: nc.vector.bn_stats(out=s_[:, 0:6], in_=y), reads=[self.psb[p_y]], writes=[sttb[b]])
                    S.op(S.dve, lambda: nc.vector.bn_aggr(out=s_[:, 6:8], in_=s_[:, 0:6]), reads=[sttb[b]], writes=[sttb[b]])
                    S.op(S.act, lambda: nc.scalar.activation(out=s_[:, 8:9], in_=s_[:, 7:8], func=AF.Sqrt, bias=self.eps_t[:], scale=1.0),
                         reads=[sttb[b], self.cb], writes=[sttb[b]])
                    S.op(S.dve, lambda: nc.vector.reciprocal(out=s_[:, 9:10], in_=s_[:, 8:9]), reads=[sttb[b]], writes=[sttb[b]])
                    S.op(S.dve, lambda: nc.vector.tensor_scalar(out=yn[b][:], in0=y, scalar1=s_[:, 6:7], scalar2=s_[:, 9:10],
                                                                op0=ALU.subtract, op1=ALU.mult),
                         reads=[self.psb[p_y], sttb[b]], writes=[ynb[b]])
                    S.op(S.pool, lambda: nc.gpsimd.tensor_tensor(out=yn[b][:], in0=yn[b][:], in1=gnw[:], op=ALU.mult),
                         reads=[ynb[b], hb], writes=[ynb[b]])
                    S.op(S.pool, lambda: nc.gpsimd.tensor_tensor(out=yn[b][:], in0=yn[b][:], in1=gnb[:], op=ALU.add),
                         reads=[ynb[b], hb], writes=[ynb[b]])
                    S.op(S.pool, lambda: nc.gpsimd.tensor_tensor(out=yo[b][:], in0=yn[b][:], in1=rgt[b][:], op=ALU.mult),
                         reads=[ynb[b], rgb[b]], writes=[yob[b]])
                    p_t = 2 + b
                    pv = self.ps[p_t][:].bitcast(BF16)
                    for eb in range(2):
                        S.op(S.pe, lambda: nc.tensor.transpose(out=pv[:, eb * 128:(eb + 1) * 128], in_=yo[b][:, eb * 128:(eb + 1) * 128],
                                                               identity=self.ident_bf[:]),
                             reads=[yob[b], self.cb], writes=[self.psb[p_t]], signal=(eb == 1))
                    S.op(S.act, lambda: nc.scalar.activation(out=self.HT[:, 16 + 2 * h:18 + 2 * h, c0:c1],
                                                             in_=pv[:, 0:256].rearrange("p (e i) -> p e i", e=2), func=AF.Copy),
                         reads=[self.psb[p_t]], writes=self.HTb[16 + 2 * h:18 + 2 * h])
            S.barrier()
            if "htd" in self.debug:
                S.dma(S.sp, self.htd, self.HT[:], owner=self.HTb[0], reads=self.HTb, writes=[self.dbufs["htd"]])
                S.barrier()


    def p6_up(self):
        nc, S = self.nc, self.S
        if "htd" in self.ext_in:
            S.dma(S.sp, self.HT[:], self.htd, owner=self.HTb[0], reads=[self.dbufs["htd"]], writes=self.HTb)
            S.barrier()
        with ExitStack() as st:
            sb = self.sb
            wt = [sb(st, f"u_wt{i}", [128, KC, 256], BF16) for i in range(2)]
            wtb = [Buf(f"u_wt{i}") for i in range(2)]
            ga = [sb(st, f"u_ga{i}", [128, 2, 512], BF16) for i in range(2)]; gab = [Buf(f"u_ga{i}") for i in range(2)]
            t1 = [sb(st, f"u_t1{i}", [128, 512], F32) for i in range(2)]; t1b = [Buf(f"u_t1{i}") for i in range(2)]
            t2 = [sb(st, f"u_t2{i}", [128, 512], F32) for i in range(2)]; t2b = [Buf(f"u_t2{i}") for i in range(2)]
            mo = [sb(st, f"u_mo{i}", [128, 512], BF16) for i in range(2)]; mob = [Buf(f"u_mo{i}") for i in range(2)]
            wa = self.w_ua.rearrange("(kc p) n -> p kc n", p=128)
            wr = self.w_ur.rearrange("(kc p) n -> p kc n", p=128)
            dd = self.dbufs
            u = 0
            for fb2 in range(16):
                b = fb2 % 2
                c0 = fb2 * 256
                S.dma(S.pool, wt[b][:, 0:16, :], wa[:, :, c0:c0 + 256], owner=wtb[b], writes=[wtb[b]])
                S.dma(S.pool, wt[b][:, 16:32, :], wr[:, :, c0:c0 + 256], owner=wtb[b], writes=[wtb[b]])
                for sub in range(2):
                    f0 = c0 + sub * 128
                    for tb_ in range(4):
                        i2 = u % 2
                        t0 = tb_ * 512
                        S.dma(S.sp, ga[i2][:, 0, :], self.sga[f0:f0 + 128, t0:t0 + 512], owner=gab[i2], reads=[dd["sga"]], writes=[gab[i2]])
                        S.dma(S.sp, ga[i2][:, 1, :], self.sgr[f0:f0 + 128, t0:t0 + 512], owner=gab[i2], reads=[dd["sgr"]], writes=[gab[i2]])
                        pa, pr = (0, 1) if i2 == 0 else (2, 3)
                        self.mm(self.ps[pa][:], self.psb[pa],
                                [(wt[b][:, kc, sub * 128:(sub + 1) * 128], self.HT[:, kc, t0:t0 + 512]) for kc in range(16)],
                                reads=[wtb[b]] + self.HTb[0:16])
                        self.mm(self.ps[pr][:], self.psb[pr],
                                [(wt[b][:, kc, sub * 128:(sub + 1) * 128], self.HT[:, kc, t0:t0 + 512]) for kc in range(16, 32)],
                                reads=[wtb[b]] + self.HTb[16:32])
                        S.op(S.dve, lambda: nc.vector.tensor_tensor(out=t1[i2][:], in0=self.ps[pa][:], in1=ga[i2][:, 0, :], op=ALU.mult),
                             reads=[self.psb[pa], gab[i2]], writes=[t1b[i2]])
                        S.op(S.dve, lambda: nc.vector.tensor_tensor(out=t2[i2][:], in0=self.ps[pr][:], in1=ga[i2][:, 1, :], op=ALU.mult),
                             reads=[self.psb[pr], gab[i2]], writes=[t2b[i2]])
                        S.op(S.dve, lambda: nc.vector.tensor_tensor(out=mo[i2][:], in0=t1[i2][:], in1=t2[i2][:], op=ALU.add),
                             reads=[t1b[i2], t2b[i2]], writes=[mob[i2]])
                        S.dma(S.sp, self.mrg[f0:f0 + 128, t0:t0 + 512], mo[i2][:], owner=mob[i2], reads=[mob[i2]], writes=[dd["mrg"]])
                        u += 1
            S.barrier()
        for kc in range(KC):
            S.dma(S.sp, self.HT[:, kc, :], self.mrg[kc * 128:(kc + 1) * 128, :], owner=self.HTb[kc],
                  reads=[self.dbufs["mrg"]], writes=[self.HTb[kc]])
        S.barrier()

    def p7_out(self):
        nc, S = self.nc, self.S
        with ExitStack() as st:
            sb = self.sb
            wt = [sb(st, f"o_wt{i}", [128, KC, 256], BF16) for i in range(2)]
            wtb = [Buf(f"o_wt{i}") for i in range(2)]
            gm = [sb(st, f"o_gm{i}", [128, 256], F32) for i in range(2)]; gmb = [Buf(f"o_gm{i}") for i in range(2)]
            xt = [sb(st, f"o_x{i}", [128, 256], F32) for i in range(3)]; xtb = [Buf(f"o_x{i}") for i in range(3)]
            tt = [sb(st, f"o_t{i}", [128, 256], F32) for i in range(2)]; ttb = [Buf(f"o_t{i}") for i in range(2)]
            wv = self.w_out.rearrange("(kc p) n -> p kc n", p=128)
            dd = self.dbufs
            u = 0
            for nb in range(16):
                b = nb % 2
                c0 = nb * 256
                S.dma(S.pool, wt[b][:], wv[:, :, c0:c0 + 256], owner=wtb[b], writes=[wtb[b]])
                S.dma(S.sp, gm[b][:], self.modd[2 * D + c0:2 * D + c0 + 256].partition_broadcast(128), owner=gmb[b],
                      reads=[dd["modd"]], writes=[gmb[b]])
                for t in range(16):
                    i3 = u % 3
                    i2 = u % 2
                    S.dma(S.sp, xt[i3][:], self.x_own[t * 128:(t + 1) * 128, c0:c0 + 256], owner=xtb[i3], writes=[xtb[i3]])
                    pi = u % 4
                    self.mm(self.ps[pi][:, 0:256], self.psb[pi],
                            [(self.HT[:, kc, t * 128:(t + 1) * 128], wt[b][:, kc, :]) for kc in range(KC)],
                            reads=[wtb[b]] + self.HTb)
                    S.op(S.dve, lambda: nc.vector.tensor_tensor(out=tt[i2][:], in0=self.ps[pi][:, 0:256], in1=gm[b][:], op=ALU.mult),
                         reads=[self.psb[pi], gmb[b]], writes=[ttb[i2]])
                    S.op(S.dve, lambda: nc.vector.tensor_tensor(out=xt[i3][:], in0=xt[i3][:], in1=tt[i2][:], op=ALU.add),
                         reads=[ttb[i2], xtb[i3]], writes=[xtb[i3]])
                    S.dma(S.sp, self.x1[t * 128:(t + 1) * 128, c0:c0 + 256], xt[i3][:], owner=xtb[i3], reads=[xtb[i3]], writes=[dd["x1"]])
                    u += 1
            S.barrier()

    def p8_norm2(self):
        nc, S = self.nc, self.S
        self.norm_T(self.x1, "x1", 16, self.af, self.bf)
        with ExitStack() as st:
            sb = self.sb
            rw = sb(st, "g_rw", [128, KC, NEXP], BF16); rwb = Buf("g_rw")
            rbt = sb(st, "g_rb", [128, NEXP], F32)
            lg = [sb(st, f"g_lg{i}", [128, NEXP], F32) for i in range(2)]; lgb = [Buf(f"g_lg{i}") for i in range(2)]
            m8 = [sb(st, f"g_m8{i}", [128, 12], F32) for i in range(2)]; m8b = [Buf(f"g_m8{i}") for i in range(2)]
            mk = [sb(st, f"g_mk{i}", [128, NEXP], F32) for i in range(2)]; mkb = [Buf(f"g_mk{i}") for i in range(2)]
            ex = [sb(st, f"g_ex{i}", [128, NEXP], F32) for i in range(2)]; exb = [Buf(f"g_ex{i}") for i in range(2)]
            cm = [sb(st, f"g_cm{i}", [128, NEXP], F32) for i in range(2)]; cmb = [Buf(f"g_cm{i}") for i in range(2)]
            cmh = [sb(st, f"g_cmh{i}", [128, NEXP], BF16) for i in range(2)]; cmhb = [Buf(f"g_cmh{i}") for i in range(2)]
            S.dma(S.pool, rw[:], self.router_w.rearrange("(kc p) e -> p kc e", p=128), owner=rwb, writes=[rwb])
            S.dma(S.sp, rbt[:], self.rb, owner=rwb, writes=[rwb])
            dd = self.dbufs
            for t in range(16):
                b = t % 2
                pi = t % 2
                self.mm(self.ps[pi][:, 0:NEXP], self.psb[pi],
                        [(self.HT[:, kc, t * 128:(t + 1) * 128], rw[:, kc, :]) for kc in range(KC)], reads=[rwb] + self.HTb)
                S.op(S.dve, lambda: nc.vector.tensor_tensor(out=lg[b][:], in0=self.ps[pi][:, 0:NEXP], in1=rbt[:], op=ALU.add),
                     reads=[self.psb[pi], rwb], writes=[lgb[b]])
                S.op(S.dve, lambda: nc.vector.max(out=m8[b][:, 0:8], in_=lg[b][:]), reads=[lgb[b]], writes=[m8b[b]])
                S.op(S.dve, lambda: nc.vector.tensor_scalar(out=m8[b][:, 8:9], in0=m8[b][:, 0:1], scalar1=-1.0, scalar2=None, op0=ALU.mult),
                     reads=[m8b[b]], writes=[m8b[b]])
                S.op(S.dve, lambda: nc.vector.tensor_scalar(out=mk[b][:], in0=lg[b][:], scalar1=m8[b][:, 3:4], scalar2=None, op0=ALU.is_ge),
                     reads=[lgb[b], m8b[b]], writes=[mkb[b]])
                S.op(S.act, lambda: nc.scalar.activation(out=ex[b][:], in_=lg[b][:], func=AF.Exp, bias=m8[b][:, 8:9], scale=1.0),
                     reads=[lgb[b], m8b[b]], writes=[exb[b]])
                S.op(S.dve, lambda: nc.vector.tensor_tensor(out=ex[b][:], in0=ex[b][:], in1=mk[b][:], op=ALU.mult),
                     reads=[exb[b], mkb[b]], writes=[exb[b]])
                S.op(S.dve, lambda: nc.vector.reduce_sum(out=m8[b][:, 9:10], in_=ex[b][:], axis=AX.X), reads=[exb[b]], writes=[m8b[b]])
                S.op(S.dve, lambda: nc.vector.reciprocal(out=m8[b][:, 10:11], in_=m8[b][:, 9:10]), reads=[m8b[b]], writes=[m8b[b]])
                S.op(S.dve, lambda: nc.vector.tensor_scalar(out=cm[b][:], in0=ex[b][:], scalar1=m8[b][:, 10:11], scalar2=None, op0=ALU.mult),
                     reads=[exb[b], m8b[b]], writes=[cmb[b]])
                S.op(S.act, lambda: nc.scalar.activation(out=cmh[b][:], in_=cm[b][:], func=AF.Copy), reads=[cmb[b]], writes=[cmhb[b]])
                if "combd" in self.debug:
                    S.dma(S.sp, self.combd[t * 128:(t + 1) * 128, :], cm[b][:], owner=cmb[b], reads=[cmb[b]], writes=[dd["combd"]])
                p_t = 2 + b
                pv = self.ps[p_t][:].bitcast(BF16)
                S.op(S.pe, lambda: nc.tensor.transpose(out=pv[0:NEXP, 0:128], in_=cmh[b][:], identity=self.ident_bf[:]),
                     reads=[cmhb[b], self.cb], writes=[self.psb[p_t]])
                S.op(S.act, lambda: nc.scalar.activation(out=self.combT[:, t * 128:(t + 1) * 128], in_=pv[0:NEXP, 0:128], func=AF.Copy),
                     reads=[self.psb[p_t]], writes=[self.combTb])
            S.dma(S.sp, self.combTd, self.combT[:], owner=self.combTb, reads=[self.combTb], writes=[dd["combTd"]])
            S.dma(S.sp, self.h2d, self.HT[:, :, 1024:2048], owner=self.HTb[0], reads=self.HTb, writes=[dd["h2d"]])
            S.barrier()

    def p9_moe(self):
        nc, S = self.nc, self.S
        NE = self.n_exp
        with ExitStack() as st:
            sb = self.sb
            dd = self.dbufs
            w1t = [sb(st, f"m_w1{i}", [128, KC, 2, 128], BF16) for i in range(2)]; w1b = [Buf(f"m_w1{i}") for i in range(2)]
            selb = Buf("m_sel")
            b1t = sb(st, "m_b1", [128, NE * 16], F32)
            cbe = [sb(st, f"m_cbe{i}", [128, 1024], BF16) for i in range(2)]; cbeb = [Buf(f"m_cbe{i}") for i in range(2)]
            gt = [sb(st, f"m_g{i}", [128, 512], F32) for i in range(2)]; gtb = [Buf(f"m_g{i}") for i in range(2)]
            sg = [sb(st, f"m_s{i}", [128, 512], F32) for i in range(2)]; sgb = [Buf(f"m_s{i}") for i in range(2)]
            ut = [sb(st, f"m_u{i}", [128, 512], F32) for i in range(2)]; utb = [Buf(f"m_u{i}") for i in range(2)]
            yt = [sb(st, f"m_y{i}", [128, 512], F32) for i in range(3)]; ytb = [Buf(f"m_y{i}") for i in range(3)]
            xr = [sb(st, f"m_x{i}", [128, 512], F32) for i in range(2)]; xrb = [Buf(f"m_x{i}") for i in range(2)]
            gf = [sb(st, f"m_gf{i}", [128, 512], F32) for i in range(2)]; gfb = [Buf(f"m_gf{i}") for i in range(2)]
            b2t = [sb(st, f"m_b2{i}", [32, 512], BF16) for i in range(2)]; b2b = [Buf(f"m_b2{i}") for i in range(2)]
            S.dma(S.sp, b1t[:], self.b1T, owner=selb, writes=[selb])
            actT = self.HT[:, 0:16, 1024:2048]
            actb = [Buf("m_act0"), Buf("m_act1")]
            w2t = [self.HT[:, 16:32, 1024:1536], self.HT[:, 16:32, 1536:2048]]
            w2b = [Buf("m_w20"), Buf("m_w21")]
            hb = [Buf("m_h2")]
            ngrp = NE // 2
            cnt = dict(w1=0, blk=0, w2=0, y=0)
            for half in range(2):
                tk0 = half * 1024
                if half == 1:
                    S.dma(S.sp, self.HT[:, :, 0:1024], self.h2d, owner=hb[0], reads=[dd["h2d"]], writes=hb)
                for grp in range(ngrp):
                    for ei in range(2):
                        e = grp * 2 + ei
                        S.dma(S.sp, cbe[ei][:], self.combTd[e, tk0:tk0 + 1024].partition_broadcast(128), owner=cbeb[ei],
                              reads=[dd["combTd"]], writes=[cbeb[ei]])
                        for fb in range(8):
                            wb_ = cnt["w1"] % 2; cnt["w1"] += 1
                            S.dma(S.pool, w1t[wb_][:].rearrange("p k g c -> p (k g c)"), self.w1[e, fb], owner=w1b[wb_], writes=[w1b[wb_]])
                            for tb_ in range(2):
                                i2 = cnt["blk"] % 2; cnt["blk"] += 1
                                pg, pu = (0, 1) if i2 == 0 else (2, 3)
                                rhs_t = slice(tb_ * 512, (tb_ + 1) * 512)
                                self.mm(self.ps[pg][:], self.psb[pg],
                                        [(w1t[wb_][:, kc, 0, :], self.HT[:, kc, rhs_t]) for kc in range(KC)], reads=[w1b[wb_]] + hb)
                                self.mm(self.ps[pu][:], self.psb[pu],
                                        [(w1t[wb_][:, kc, 1, :], self.HT[:, kc, rhs_t]) for kc in range(KC)], reads=[w1b[wb_]] + hb)
                                bg = b1t[:, e * 16 + fb:e * 16 + fb + 1]
                                bu = b1t[:, e * 16 + 8 + fb:e * 16 + 8 + fb + 1]
                                S.op(S.dve, lambda: nc.vector.tensor_scalar(out=gt[i2][:], in0=self.ps[pg][:], scalar1=bg, scalar2=7.0,
                                                                            op0=ALU.add, op1=ALU.min), reads=[self.psb[pg], selb], writes=[gtb[i2]])
                                S.op(S.act, lambda: nc.scalar.activation(out=sg[i2][:], in_=gt[i2][:], func=AF.Sigmoid, scale=1.702),
                                     reads=[gtb[i2]], writes=[sgb[i2]])
                                S.op(S.dve, lambda: nc.vector.tensor_scalar(out=ut[i2][:], in0=self.ps[pu][:], scalar1=bu, scalar2=7.0,
                                                                            op0=ALU.add, op1=ALU.min), reads=[self.psb[pu], selb], writes=[utb[i2]])
                                S.op(S.dve, lambda: nc.vector.tensor_scalar(out=ut[i2][:], in0=ut[i2][:], scalar1=-7.0, scalar2=1.0,
                                                                            op0=ALU.max, op1=ALU.add), reads=[utb[i2]], writes=[utb[i2]])
                                S.op(S.dve, lambda: nc.vector.tensor_tensor(out=gt[i2][:], in0=gt[i2][:], in1=sg[i2][:], op=ALU.mult),
                                     reads=[gtb[i2], sgb[i2]], writes=[gtb[i2]])
                                S.op(S.dve, lambda: nc.vector.tensor_tensor(out=gt[i2][:], in0=gt[i2][:], in1=ut[i2][:], op=ALU.mult),
                                     reads=[gtb[i2], utb[i2]], writes=[gtb[i2]])
                                S.op(S.dve, lambda: nc.vector.tensor_tensor(out=actT[:, ei * 8 + fb, rhs_t], in0=gt[i2][:],
                                                                            in1=cbe[ei][:, rhs_t], op=ALU.mult),
                                     reads=[gtb[i2], cbeb[ei]], writes=[actb[ei]])
                    first, last = (grp == 0), (grp == ngrp - 1)
                    for nb in range(8):
                        wb_ = cnt["w2"] % 2; cnt["w2"] += 1
                        c0 = nb * 512
                        for ei in range(2):
                            e = grp * 2 + ei
                            S.dma(S.pool, w2t[wb_][:, ei * 8:(ei + 1) * 8, :], self.w2[e, nb].rearrange("p (k c) -> p k c", k=8),
                                  owner=w2b[wb_], writes=[w2b[wb_]])
                        if first:
                            S.dma(S.pool, b2t[wb_][:], self.b2[:, c0:c0 + 512], owner=b2b[wb_], writes=[b2b[wb_]])
                        if last:
                            S.dma(S.sp, gf[wb_][:], self.modd[5 * D + c0:5 * D + c0 + 512].partition_broadcast(128), owner=gfb[wb_],
                                  reads=[dd["modd"]], writes=[gfb[wb_]])
                        for t in range(8):
                            r0 = tk0 + t * 128
                            i3 = cnt["y"] % 3; i2 = cnt["y"] % 2; cnt["y"] += 1
                            pi = 6 + i2
                            if not first:
                                S.dma(S.sp, yt[i3][:], self.yd[r0:r0 + 128, c0:c0 + 512], owner=ytb[i3], reads=[dd["yd"]], writes=[ytb[i3]])
                            if last:
                                S.dma(S.sp, xr[i2][:], self.x1[r0:r0 + 128, c0:c0 + 512], owner=xrb[i2], reads=[dd["x1"]], writes=[xrb[i2]])
                            pairs = [(actT[:, kc, t * 128:(t + 1) * 128], w2t[wb_][:, kc, :]) for kc in range(16)]
                            rd = actb + [w2b[wb_]]
                            if first:
                                pairs.append((self.combT[:, r0:r0 + 128], b2t[wb_][:]))
                                rd = rd + [self.combTb, b2b[wb_]]
                            self.mm(self.ps[pi][:], self.psb[pi], pairs, reads=rd)
                            if first:
                                S.op(S.act, lambda: nc.scalar.activation(out=yt[i3][:], in_=self.ps[pi][:], func=AF.Copy),
                                     reads=[self.psb[pi]], writes=[ytb[i3]])
                            else:
                                S.op(S.dve, lambda: nc.vector.tensor_tensor(out=yt[i3][:], in0=self.ps[pi][:], in1=yt[i3][:], op=ALU.add),
                                     reads=[self.psb[pi], ytb[i3]], writes=[ytb[i3]])
                            if not last:
                                S.dma(S.sp, self.yd[r0:r0 + 128, c0:c0 + 512], yt[i3][:], owner=ytb[i3], reads=[ytb[i3]], writes=[dd["yd"]])
                            else:
                                S.op(S.dve, lambda: nc.vector.tensor_tensor(out=yt[i3][:], in0=yt[i3][:], in1=gf[wb_][:], op=ALU.mult),
                                     reads=[ytb[i3], gfb[wb_]], writes=[ytb[i3]])
                                S.op(S.dve, lambda: nc.vector.tensor_tensor(out=yt[i3][:], in0=yt[i3][:], in1=xr[i2][:], op=ALU.add),
                                     reads=[ytb[i3], xrb[i2]], writes=[ytb[i3]])
                                S.dma(S.sp, self.out[r0:r0 + 128, c0:c0 + 512], yt[i3][:], owner=ytb[i3], reads=[ytb[i3]], writes=[dd["out"]])
            S.barrier()

    def finish(self):
        pass


def _rope_tables(hf):
    l = np.arange(SEQ, dtype=np.float32)
    pos = l if hf == 0 else (np.float32(SEQ - 1) - l)
    inv = (np.float32(10000.0) ** (-np.arange(0, 128, 2, dtype=np.float32) / np.float32(128))).astype(np.float32)
    ang = pos[:, None] * inv[None, :]
    cos = np.cos(ang).astype(np.float32).T
    sin = np.sin(ang).astype(np.float32).T
    cosT = np.concatenate([cos, cos], 0)
    sinT = np.concatenate([sin, sin], 0)
    return np.ascontiguousarray(cosT), np.ascontiguousarray(sinT)


def _consts():
    r = np.zeros((128, 128), np.float32)
    for m in range(64):
        r[m + 64, m] = -1.0
    for m in range(64, 128):
        r[m - 64, m] = 1.0
    return r, np.eye(128, dtype=np.float32)


def prepare(inputs, names):
    g = lambda k: np.asarray(inputs[k])
    x = g("x") if "x" in inputs else None
    c = g("c") if "c" in inputs else None
    rperm, ident = _consts()
    tabs = [_rope_tables(0), _rope_tables(1)]
    ks = np.float32(128.0 ** -0.5)
    shared = {}
    lazy = {
        "ada_w": lambda: g("ada_w")[0], "ada_b": lambda: g("ada_b"), "w_in": lambda: g("w_in")[0],
        "nw_m": lambda: np.ascontiguousarray(g("norm_mix_w")[0].reshape(KC, 128).T),
        "nw_f": lambda: np.ascontiguousarray(g("norm_ffn_w")[0].reshape(KC, 128).T),
        "qnw": lambda: np.ascontiguousarray(g("q_norm_w")[0].reshape(128, 1)),
        "knw": lambda: np.ascontiguousarray(g("k_norm_w")[0].reshape(128, 1)),
        "rperm": lambda: rperm, "ident": lambda: ident,
        "w_ua": lambda: g("w_up_attn")[0], "w_ur": lambda: g("w_up_ret")[0], "w_out": lambda: g("w_out")[0],
        "router_w": lambda: g("router_w")[0],
        "rb": lambda: np.ascontiguousarray(np.tile(g("router_b")[0][None, :], (128, 1))),
        "w1": lambda: np.ascontiguousarray(g("expert_w1")[0].reshape(-1, KC, 128, 2, 8, 128).transpose(0, 4, 2, 1, 3, 5)
                                           ).reshape(-1, 8, 128, KC * 2 * 128),
        "w2": lambda: np.ascontiguousarray(g("expert_w2")[0].reshape(-1, 8, 128, 8, 512).transpose(0, 3, 2, 1, 4)
                                           ).reshape(-1, 8, 128, 8 * 512),
        "b2": lambda: g("expert_b2")[0],
        "b1T": lambda: np.ascontiguousarray(g("expert_b1")[0].reshape(-1, 16, 128).transpose(2, 0, 1).reshape(128, -1)),
    }
    for k_, f_ in lazy.items():
        if k_ in names:
            shared[k_] = f_()
    def t5_bucket(rel):
        nb, me = 16, 8
        base = np.where(rel > 0, nb, 0)
        n = np.abs(rel)
        nf = np.maximum(n, 1).astype(np.float32)
        large = me + (np.log(nf / me) / math.log(128 / me) * (nb - me)).astype(np.int32)
        large = np.minimum(large, nb - 1)
        return base + np.where(n < me, n, large)
    rb = g("rel_bias") if "rel_bias" in inputs else None
    kk = np.arange(128)[:, None, None]
    kb_ = np.arange(3)[None, :, None]
    qq = np.arange(128)[None, None, :]
    rel_loc = (kb_ - 1) * 128 + kk - qq
    valid = np.abs(rel_loc) <= 128
    jj = np.arange(128, dtype=np.float32)
    cpos = np.maximum(jj[None, :] - jj[:, None], 0).astype(np.float32)
    cneg = np.maximum(jj[:, None] - jj[None, :], 0).astype(np.float32)
    ccol = np.stack([127 - jj, jj], 1).astype(np.float32)
    crow = np.tile(np.concatenate([jj + 1, 128 - jj])[None, :], (128, 1)).astype(np.float32)
    if "gnw" in names:
        shared.update({
            "cpos": cpos, "cneg": cneg, "ccol": ccol, "crow": crow,
            "gnw": np.ascontiguousarray(np.tile(g("ret_gn_w")[0][None, :], (128, 1))),
            "gnb": np.ascontiguousarray(np.tile(g("ret_gn_b")[0][None, :], (128, 1))),
            "sinkr": np.ascontiguousarray(np.tile(g("attn_sink")[0][None, :], (128, 1))),
        })
    maps = []
    for b in range(4):
        m = dict(shared)
        if "cT" in names:
            m["cT"] = np.ascontiguousarray(c[b].reshape(KC, 128).T)
        for hf in (0, 1):
            sfx = str(hf)
            xl = None if x is None else (x[b] if hf == 0 else x[b][::-1])
            cosT, sinT = tabs[hf]
            if "x_own" + sfx in names:
                m["x_own" + sfx] = np.ascontiguousarray(xl[:TOK])
            if "x_oth" + sfx in names:
                m["x_oth" + sfx] = np.ascontiguousarray(xl[TOK:])
            if "cosq" + sfx in names:
                m["cosq" + sfx] = np.ascontiguousarray(cosT[:, :TOK])
                m["sinq" + sfx] = np.ascontiguousarray(sinT[:, :TOK])
            if "cosk" + sfx in names:
                m["cosk" + sfx] = cosT * ks
                m["sink" + sfx] = sinT * ks
            if "biasT" + sfx in names:
                rel_o = rel_loc if hf == 0 else -rel_loc
                bt = rb[t5_bucket(rel_o)]
                bt = np.where(valid[..., None], bt, np.float32(-1e30)).astype(np.float32)
                bt = bt.reshape(128, 3, 128, 4, 4).transpose(3, 0, 1, 4, 2)
                m["biasT" + sfx] = np.ascontiguousarray(bt.reshape(4, 128, 1536))
                df, db = g("ret_decay_fwd")[0], g("ret_decay_bwd")[0]
                if hf == 1:
                    df, db = db, df
                m["dec_f" + sfx] = np.ascontiguousarray(np.tile(df[None, :], (128, 1)))
                m["dec_b" + sfx] = np.ascontiguousarray(np.tile(db[None, :], (128, 1)))
        for k in names:
            if k in inputs and k not in m:
                m[k] = inputs[k][b] if isinstance(inputs[k], (list, tuple)) else inputs[k]
        maps.append({k: m[k] for k in names})
    return maps


_NC_CACHE = {}


def kernel(**inputs):
    if "k" not in _NC_CACHE:
        k = K(halves=(0,))
        k.build()
        _NC_CACHE["k"] = k
    k = _NC_CACHE["k"]
    names0 = list(k.inputs.keys())
    per_half = [n[:-1] for n in names0 if n.endswith("0") and n[:-1] in K.USED]
    names_all = [n for n in names0 if not (n.endswith("0") and n[:-1] in K.USED)]
    names_all += [p + s for p in per_half for s in ("0", "1")]
    bmaps = prepare(inputs, names_all)
    maps = []
    for i in range(8):
        b, hf = i // 2, i % 2
        m = {}
        for n in names0:
            if n.endswith("0") and n[:-1] in K.USED:
                m[n] = bmaps[b][n[:-1] + str(hf)]
            else:
                m[n] = bmaps[b][n]
        maps.append(m)
    del bmaps
    res = run_bass_kernel_spmd(k.nc, maps, core_ids=list(range(8)))
    del maps
    out = np.empty((4, SEQ, D), np.float32)
    for i in range(8):
        b, hf = i // 2, i % 2
        o = np.asarray(res.results[i]["out0"])
        if hf == 0:
            out[b, :TOK] = o
        else:
            out[b, TOK:] = o[::-1]
    return out
```

```python
import math
from contextlib import ExitStack
import numpy as np
import concourse.bass as bass
import concourse.mybir as mybir
from concourse.bass_utils import run_bass_kernel_spmd

F32 = mybir.dt.float32
BF16 = mybir.dt.bfloat16
AF = mybir.ActivationFunctionType
ALU = mybir.AluOpType
AX = mybir.AxisListType

D = 4096
SEQ = 4096
TOK = 2048
KC = 32
NEXP = 32
DFF = 1024
EPS = 1e-6
PW = (2048, 512, 512, 1024, 1024, 2048, 2048, 4096, 4096)
OFF = [0]
for _w in PW:
    OFF.append(OFF[-1] + _w)
(O_AQ, O_AK, O_AV, O_RQ, O_RK, O_RV, O_RG, O_GA, O_GR, O_END) = OFF


class Buf:
    __slots__ = ("name", "writers", "readers", "dsems")

    def __init__(self, name):
        self.name = name
        self.writers = {}
        self.readers = {}
        self.dsems = {}


class Eng:
    def __init__(self, name, eng, sem, inorder, self_sync):
        self.name = name
        self.eng = eng
        self.sem = sem
        self.count = 0
        self.seen = {}
        self.inorder = inorder
        self.self_sync = self_sync
        self.pending = False


class Sched:
    def __init__(self, nc, stack):
        self.nc = nc
        self.stack = stack
        mk = lambda n: stack.enter_context(nc.semaphore(n))
        self.pe = Eng("pe", nc.tensor, mk("s_pe"), True, False)
        self.act = Eng("act", nc.scalar, mk("s_act"), True, True)
        self.dve = Eng("dve", nc.vector, mk("s_dve"), True, True)
        self.pool = Eng("pool", nc.gpsimd, mk("s_pool"), True, True)
        self.sp = Eng("sp", nc.sync, None, False, False)
        self.engs = [self.pe, self.act, self.dve, self.pool, self.sp]
        self.dma_bufs = []
        self.free_dsems = {}
        self.dma_owners = []
        self.nwaits = 0
        self.nins = 0

    def _wait(self, E, need):
        for sem, val in need.items():
            if E.seen.get(sem, 0) < val:
                E.eng.wait_ge(sem, val)
                E.seen[sem] = val
                self.nwaits += 1

    def _need(self, E, reads, writes):
        need = {}

        def merge(d, skip_self):
            for s, v in d.items():
                if skip_self and s is E.sem:
                    continue
                if need.get(s, 0) < v:
                    need[s] = v
        for b in reads:
            merge(b.writers, not E.self_sync)
        for b in writes:
            merge(b.writers, not E.self_sync)
            merge(b.readers, E.inorder)
        return need

    def _record(self, tok, reads, writes):
        s, v = tok
        for b in reads:
            if b.readers.get(s, 0) < v:
                b.readers[s] = v
        for b in writes:
            if b.writers.get(s, 0) < v:
                b.writers[s] = v
            b.readers = {}

    def op(self, E, emit, reads=(), writes=(), signal=True):
        self._wait(E, self._need(E, reads, writes))
        ins = emit()
        self.nins += 1
        if signal:
            E.count += 1
            ins.then_inc(E.sem, 1)
            E.pending = False
            tok = (E.sem, E.count)
        else:
            E.pending = True
            tok = (E.sem, E.count + 1)
        self._record(tok, reads, writes)
        return ins

    def dma(self, E, out, in_, owner, reads=(), writes=(), **kw):
        self._wait(E, self._need(E, reads, writes))
        ent = owner.dsems.get(E.name)
        if ent is None:
            fl = self.free_dsems.setdefault(E.name, [])
            if fl:
                ent = fl.pop()
            else:
                ent = [self.stack.enter_context(self.nc.semaphore(f"d{len(self.dma_bufs)}_{E.name}")), 0]
                self.dma_bufs.append(ent)
            owner.dsems[E.name] = ent
            self.dma_owners.append(owner)
        ins = E.eng.dma_start(out=out, in_=in_, **kw)
        ins.then_inc(ent[0], 16)
        ent[1] += 16
        self.nins += 1
        self._record((ent[0], ent[1]), reads, writes)
        return ins

    def barrier(self):
        assert not self.pe.pending
        need = {}
        for E in self.engs:
            if E.sem is not None and E.count:
                need[E.sem] = E.count
        for ent in self.dma_bufs:
            if ent[1]:
                need[ent[0]] = ent[1]
        for E in self.engs:
            self._wait(E, need)
        for o in self.dma_owners:
            for qn, ent in o.dsems.items():
                self.free_dsems.setdefault(qn, []).append(ent)
            o.dsems = {}
        self.dma_owners = []


class K:
    def set_half(self, hf):
        self.hf = hf
        h = self.hin[hf]
        self.x_own, self.x_oth = h["x_own"], h["x_oth"]
        self.cosq, self.sinq, self.cosk, self.sink_ = h["cosq"], h["sinq"], h["cosk"], h["sink"]
        self.biasT, self.dec_f, self.dec_b, self.out = h["biasT"], h["dec_f"], h["dec_b"], h["out"]
        for nm in ("x_own", "x_oth", "out"):
            self.dbufs[nm] = self.dbufs[f"{nm}{hf}"]

    def __init__(self, stop_after=99, debug=(), from_phase=1, ext_in=(), halves=(0, 1), skip=(), n_exp=NEXP):
        self.halves = tuple(halves)
        self.skip = set(skip)
        self._n_exp = n_exp
        self.from_phase = from_phase
        self.ext_in = set(ext_in)
        self.n_exp = self._n_exp
        self.stop_after = stop_after
        self.debug = set(debug)
        self.nc = bass.Bass("TRN2", target_bir_lowering=False)
        self.inputs = {}
        self.dbufs = {}

    USED = {"x_own": (3, 7), "x_oth": (2, 2), "cosq": (3, 3), "sinq": (3, 3), "cosk": (2, 3), "sink": (2, 3),
            "biasT": (4, 4), "dec_f": (5, 5), "dec_b": (5, 5), "cT": (1, 1), "ada_w": (1, 1), "ada_b": (1, 1),
            "nw_m": (1, 1), "nw_f": (1, 1), "w_in": (2, 3), "sinkr": (4, 4), "cpos": (5, 5), "cneg": (5, 5),
            "ccol": (5, 5), "crow": (5, 5), "gnw": (5, 5), "gnb": (5, 5), "w_ua": (6, 6), "w_ur": (6, 6),
            "w_out": (7, 7), "router_w": (8, 8), "rb": (8, 8), "w1": (9, 9), "b1T": (9, 9), "w2": (9, 9), "b2": (9, 9)}

    def inp(self, name, shape, dt=F32):
        base = name.rstrip("01") if name[:-1] in self.USED else name
        lo, hi = self.USED.get(base, (0, 99))
        if hi < self.from_phase or lo > self.stop_after:
            self.dbufs[name] = Buf(name)
            return self.nc.dram_tensor(name, [1] * len(shape), dt, kind="Internal").ap()
        t = self.nc.dram_tensor(name, list(shape), dt, kind="ExternalInput").ap()
        self.inputs[name] = (tuple(shape), dt)
        self.dbufs[name] = Buf(name)
        return t

    def scratch(self, name, shape, dt):
        kind = "ExternalOutput" if name in self.debug else "Internal"
        if name in self.ext_in:
            kind = "ExternalInput"
            self.inputs[name] = (tuple(shape), dt)
        t = self.nc.dram_tensor(name, list(shape), dt, kind=kind).ap()
        self.dbufs[name] = Buf(name)
        return t

    def sb(self, st, name, shape, dt):
        self._uid = getattr(self, "_uid", 0) + 1
        return st.enter_context(self.nc.sbuf_tensor(f"{name}_{self._uid}", list(shape), dt))

    def mm(self, ps_ap, psb, pairs, reads):
        S, nc = self.S, self.nc
        n = len(pairs)
        for i, (l, r) in enumerate(pairs):
            S.op(S.pe, lambda l=l, r=r, i=i: nc.tensor.matmul(ps_ap, lhsT=l, rhs=r, start=(i == 0), stop=(i == n - 1)),
                 reads=reads, writes=[psb], signal=(i == n - 1))

    def build(self):
        nc = self.nc
        with ExitStack() as gst:
            self.S = S = Sched(nc, gst)
            self.gst = gst
            self.declare()
            self.consts()
            phases = [self.p1_mod, self.p2_other, self.p3_own, self.p4_attn, self.p5_ret,
                      self.p6_up, self.p7_out, self.p8_norm2, self.p9_moe]
            for hi, hf in enumerate(self.halves):
                self.set_half(hf)
                for i, ph in enumerate(phases):
                    if i + 1 > self.stop_after:
                        break
                    if i + 1 < self.from_phase or (i == 0 and hi > 0) or (i + 1) in self.skip:
                        continue
                    ph()
                    S.barrier()
            self.finish()
        return nc

    def declare(self):
        nc = self.nc
        I = self.inp
        self.hin = [{}, {}]
        for hf in self.halves:
            for nm, shp in (("x_own", [TOK, D]), ("x_oth", [TOK, D]), ("cosq", [128, TOK]), ("sinq", [128, TOK]),
                            ("cosk", [128, SEQ]), ("sink", [128, SEQ]), ("biasT", [4, 128, 1536]),
                            ("dec_f", [128, 8]), ("dec_b", [128, 8])):
                self.hin[hf][nm] = I(f"{nm}{hf}", shp)
            self.hin[hf]["out"] = self.nc.dram_tensor(f"out{hf}", [TOK, D], F32, kind="ExternalOutput").ap()
            self.dbufs[f"out{hf}"] = Buf(f"out{hf}")
        self.cT = I("cT", [128, KC])
        self.ada_w = I("ada_w", [D, 6 * D])
        self.ada_b = I("ada_b", [1, 6 * D])
        self.nw_m = I("nw_m", [128, KC])
        self.nw_f = I("nw_f", [128, KC])
        self.w_in = I("w_in", [D, O_END])
        self.qnw = I("qnw", [128, 1])
        self.knw = I("knw", [128, 1])
        self.rperm = I("rperm", [128, 128])
        self.ident = I("ident", [128, 128])
        self.sinkr = I("sinkr", [128, 16])
        self.cpos = I("cpos", [128, 128])
        self.cneg = I("cneg", [128, 128])
        self.ccol = I("ccol", [128, 2])
        self.crow = I("crow", [128, 256])
        self.gnw = I("gnw", [128, 2048])
        self.gnb = I("gnb", [128, 2048])
        self.w_ua = I("w_ua", [2048, D])
        self.w_ur = I("w_ur", [2048, D])
        self.w_out = I("w_out", [D, D])
        self.router_w = I("router_w", [D, NEXP])
        self.rb = I("rb", [128, NEXP])
        self.w1 = I("w1", [self.n_exp, 8, 128, KC * 2 * 128])
        self.b1T = I("b1T", [128, self.n_exp * 16])
        self.w2 = I("w2", [self.n_exp, 8, 128, 8 * 512])
        self.b2 = I("b2", [NEXP, D])
        Sc = self.scratch
        self.mrg = Sc("mrg", [D, TOK], BF16)
        self.x1 = Sc("x1", [TOK, D], F32)
        self.h2d = Sc("h2d", [128, KC, 1024], BF16)
        self.yd = Sc("yd", [TOK, D], F32)
        self.combd = Sc("combd", [TOK, NEXP], F32)
        self.combTd = Sc("combTd", [NEXP, TOK], BF16)
        self.htd = Sc("htd", [128, KC, TOK], BF16)
        self.modd = Sc("modd", [6 * D], F32)
        self.qT = Sc("qT", [16, 128, TOK], BF16)
        self.kT = Sc("kT", [4, 128, TOK + 128], BF16)
        self.av = Sc("av", [TOK + 128, 512], BF16)
        self.rqT = Sc("rqT", [8, 128, TOK], BF16)
        self.rkT = Sc("rkT", [8, 128, SEQ], BF16)
        self.rkt = Sc("rkt", [SEQ, 1024], BF16)
        self.rv = Sc("rv", [SEQ, 2048], BF16)
        self.rg = Sc("rg", [TOK, 2048], BF16)
        self.sga = Sc("sga", [D, TOK], BF16)
        self.sgr = Sc("sgr", [D, TOK], BF16)
        self.HT = self.sb(self.gst, "HT", [128, KC, TOK], BF16)
        self.HTb = [Buf(f"HT{k}") for k in range(KC)]
        self.ps = [self.gst.enter_context(nc.psum_tensor(f"ps{i}", [128, 512], F32)) for i in range(8)]
        self.psb = [Buf(f"ps{i}") for i in range(8)]

    def consts(self):
        nc, S, st = self.nc, self.S, self.gst
        sb = self.sb
        self.ones_bf = sb(st, "ones_bf", [128, 128], BF16)
        self.ident_bf = sb(st, "ident_bf", [128, 128], BF16)
        self.rperm_bf = sb(st, "rperm_bf", [128, 128], BF16)
        self.eps_t = sb(st, "eps_t", [128, 1], F32)
        self.am = sb(st, "am", [128, KC], F32)
        self.bm = sb(st, "bm", [128, KC], F32)
        self.af = sb(st, "af", [128, KC], F32)
        self.bf = sb(st, "bf", [128, KC], F32)
        self.qnw_t = sb(st, "qnw_t", [128, 1], F32)
        self.knw_t = sb(st, "knw_t", [128, 1], F32)
        self.combT = sb(st, "combT", [32, TOK], BF16)
        self.combTb = Buf("combT")
        self.cb = Buf("consts")
        cb = self.cb
        S.op(S.dve, lambda: nc.vector.memset(self.ones_bf[:], 1.0), writes=[cb])
        S.op(S.dve, lambda: nc.vector.memset(self.eps_t[:], EPS), writes=[cb])
        S.dma(S.pool, self.ident_bf[:], self.ident, owner=cb, writes=[cb])
        S.dma(S.pool, self.rperm_bf[:], self.rperm, owner=cb, writes=[cb])
        S.dma(S.sp, self.qnw_t[:], self.qnw, owner=cb, writes=[cb])
        S.dma(S.sp, self.knw_t[:], self.knw, owner=cb, writes=[cb])
        import os
        if os.environ.get("K_INIT_AB"):
            for t_ in (self.am, self.af):
                S.op(S.dve, lambda: nc.vector.memset(t_[:], 1.0), writes=[cb])
            for t_ in (self.bm, self.bf):
                S.op(S.dve, lambda: nc.vector.memset(t_[:], 0.0), writes=[cb])
        S.op(S.dve, lambda: nc.vector.tensor_scalar(out=self.qnw_t[:], in0=self.qnw_t[:], scalar1=128.0 ** -0.5,
                                                     scalar2=None, op0=ALU.mult), reads=[cb], writes=[cb])

    def p1_mod(self):
        nc, S = self.nc, self.S
        with ExitStack() as st:
            sb = self.sb
            NB = 256
            wt = [sb(st, f"p1w{i}", [128, KC, NB], BF16) for i in range(2)]
            wb = [Buf(f"p1w{i}") for i in range(2)]
            cs = sb(st, "p1cs", [128, KC], F32)
            csb = sb(st, "p1csb", [128, KC], BF16)
            cbuf = Buf("p1cs")
            row = [sb(st, f"p1row{i}", [1, NB], F32) for i in range(2)]
            rowb = [Buf(f"p1row{i}") for i in range(2)]
            adab = [sb(st, f"p1adab{i}", [1, NB], F32) for i in range(2)]
            adabb = [Buf(f"p1adab{i}") for i in range(2)]
            S.dma(S.sp, cs[:], self.cT, owner=cbuf, writes=[cbuf])
            S.op(S.act, lambda: nc.scalar.activation(out=csb[:], in_=cs[:], func=AF.Silu), reads=[cbuf], writes=[cbuf])
            wv = self.ada_w.rearrange("(kc p) n -> p kc n", p=128)
            md = self.dbufs["modd"]
            moddv = self.modd.rearrange("(a n) -> a n", a=1)
            nblk = 6 * D // NB
            for nb in range(nblk):
                b = nb % 2
                S.dma(S.pool, wt[b][:], wv[:, :, nb * NB:(nb + 1) * NB], owner=wb[b], writes=[wb[b]])
                S.dma(S.sp, adab[b][:], self.ada_b[0:1, nb * NB:(nb + 1) * NB], owner=adabb[b], writes=[adabb[b]])
                pi = nb % 2
                self.mm(self.ps[pi][0:1, 0:NB], self.psb[pi],
                        [(csb[:, kc:kc + 1], wt[b][:, kc, :]) for kc in range(KC)], reads=[cbuf, wb[b]])
                S.op(S.dve, lambda: nc.vector.tensor_tensor(out=row[b][:], in0=self.ps[pi][0:1, 0:NB],
                                                            in1=adab[b][:], op=ALU.add),
                     reads=[self.psb[pi], adabb[b]], writes=[rowb[b]])
                S.dma(S.sp, moddv[0:1, nb * NB:(nb + 1) * NB], row[b][:], owner=rowb[b], reads=[rowb[b]], writes=[md])
            tmp = sb(st, "p1tmp", [128, 4, KC], F32)
            tb = Buf("p1tmp")
            nwm = sb(st, "p1nwm", [128, KC], F32)
            nwf = sb(st, "p1nwf", [128, KC], F32)
            S.dma(S.sp, nwm[:], self.nw_m, owner=tb, writes=[tb])
            S.dma(S.sp, nwf[:], self.nw_f, owner=tb, writes=[tb])
            for j, ci in enumerate((0, 1, 3, 4)):
                S.dma(S.sp, tmp[:, j, :], self.modd[ci * D:(ci + 1) * D].rearrange("(j p) -> p j", p=128),
                      owner=tb, reads=[md], writes=[tb], allow_slow_non_contiguous=True)
            cb = self.cb
            S.op(S.dve, lambda: nc.vector.scalar_tensor_tensor(out=self.am[:], in0=tmp[:, 1, :], scalar=1.0, in1=nwm[:],
                                                               op0=ALU.add, op1=ALU.mult), reads=[tb], writes=[cb])
            S.op(S.dve, lambda: nc.vector.tensor_copy(out=self.bm[:], in_=tmp[:, 0, :]), reads=[tb], writes=[cb])
            S.op(S.dve, lambda: nc.vector.scalar_tensor_tensor(out=self.af[:], in0=tmp[:, 3, :], scalar=1.0, in1=nwf[:],
                                                               op0=ALU.add, op1=ALU.mult), reads=[tb], writes=[cb])
            S.op(S.dve, lambda: nc.vector.tensor_copy(out=self.bf[:], in_=tmp[:, 2, :]), reads=[tb], writes=[cb])
            S.barrier()

    def norm_T(self, xsrc, xname, ntiles, a_t, b_t):
        nc, S = self.nc, self.S
        with ExitStack() as st:
            sb = self.sb
            xt = [sb(st, f"nx{i}", [128, D], F32) for i in range(2)]
            xb = [Buf(f"nx{i}") for i in range(2)]
            xn = [sb(st, f"nxn{i}", [128, D], BF16) for i in range(2)]
            xnb = [Buf(f"nxn{i}") for i in range(2)]
            junk = sb(st, "njunk", [128, D], BF16)
            jb = Buf("njunk")
            stt = [sb(st, f"nst{i}", [128, 4], F32) for i in range(2)]
            stb = [Buf(f"nst{i}") for i in range(2)]
            xd = self.dbufs[xname]
            import os
            NTS = int(os.environ.get("NT_STOP", "9"))
            ntiles = int(os.environ.get("NT_TILES", ntiles))
            for t in range(ntiles):
                b = t % 2
                S.dma(S.sp, xt[b][:], xsrc[t * 128:(t + 1) * 128, :], owner=xb[b], reads=[xd], writes=[xb[b]])
                s_ = stt[b]
                if NTS < 1:
                    continue
                S.op(S.act, lambda: nc.scalar.activation(out=junk[:], in_=xt[b][:], func=AF.Square, accum_out=s_[:, 0:1]),
                     reads=[xb[b]], writes=[jb, stb[b]])
                if NTS < 2:
                    continue
                S.op(S.act, lambda: nc.scalar.activation(out=s_[:, 1:2], in_=s_[:, 0:1], func=AF.Sqrt,
                                                         bias=self.eps_t[:], scale=1.0 / D),
                     reads=[stb[b], self.cb], writes=[stb[b]])
                S.op(S.dve, lambda: nc.vector.reciprocal(out=s_[:, 2:3], in_=s_[:, 1:2]), reads=[stb[b]], writes=[stb[b]])
                if NTS < 3:
                    continue
                S.op(S.dve, lambda: nc.vector.tensor_scalar(out=xn[b][:], in0=xt[b][:], scalar1=s_[:, 2:3], scalar2=None,
                                                            op0=ALU.mult), reads=[xb[b], stb[b]], writes=[xnb[b]])
                if NTS < 4:
                    continue
                for g in range(8):
                    pi = g % 4
                    pv = self.ps[pi][:].bitcast(BF16)
                    for j in range(4):
                        kc = g * 4 + j
                        S.op(S.pe, lambda kc=kc, j=j: nc.tensor.transpose(out=pv[:, j * 128:(j + 1) * 128],
                                                                         in_=xn[b][:, kc * 128:(kc + 1) * 128],
                                                                         identity=self.ident_bf[:]),
                             reads=[xnb[b], self.cb], writes=[self.psb[pi]], signal=(j == 3))
                    if NTS < 5:
                        continue
                    for j in range(4):
                        kc = g * 4 + j
                        dst = self.HT[:, kc, t * 128:(t + 1) * 128]
                        ev = os.environ.get("NT_EV", "alt")
                        if (g % 2 == 0 and ev == "alt") or ev == "act":
                            S.op(S.act, lambda kc=kc, j=j, dst=dst: nc.scalar.activation(
                                out=dst, in_=pv[:, j * 128:(j + 1) * 128], func=AF.Identity,
                                bias=b_t[:, kc:kc + 1], scale=a_t[:, kc:kc + 1]),
                                reads=[self.psb[pi], self.cb], writes=[self.HTb[kc]])
                        else:
                            S.op(S.dve, lambda kc=kc, j=j, dst=dst: nc.vector.tensor_scalar(
                                out=dst, in0=pv[:, j * 128:(j + 1) * 128], scalar1=a_t[:, kc:kc + 1],
                                scalar2=b_t[:, kc:kc + 1], op0=ALU.mult, op1=ALU.add),
                                reads=[self.psb[pi], self.cb], writes=[self.HTb[kc]])
            S.barrier()

    def project(self, w_ap, wname, jobs, tag):
        nc, S = self.nc, self.S
        wv = w_ap.rearrange("(kc p) n -> p kc n", p=128)
        wd = self.dbufs[wname]
        for i, (c0, fn) in enumerate(jobs):
            b = i % 2
            S.dma(S.pool, self.wt[b][:], wv[:, :, c0:c0 + 256], owner=self.wtb[b], reads=[wd], writes=[self.wtb[b]])
            fn(self.wt[b], self.wtb[b], c0)

    def _psrot(self):
        self._pr = (self._pr + 1) % 4
        return self._pr

    def fm_block(self, wt, wb, sub, t0, n):
        pi = self._psrot()
        self.mm(self.ps[pi][:, 0:n], self.psb[pi],
                [(wt[:, kc, sub * 128:(sub + 1) * 128], self.HT[:, kc, t0:t0 + n]) for kc in range(KC)],
                reads=[wb] + self.HTb)
        return pi

    def tm_block(self, wt, wb, t0):
        pi = self._psrot()
        self.mm(self.ps[pi][:, 0:256], self.psb[pi],
                [(self.HT[:, kc, t0:t0 + 128], wt[:, kc, :]) for kc in range(KC)],
                reads=[wb] + self.HTb)
        return pi

    def ep_alloc(self, st):
        sb = self.sb
        self.e_bf = [sb(st, f"e_bf{i}", [128, 512], BF16) for i in range(2)]
        self.e_bfb = [Buf(f"e_bf{i}") for i in range(2)]
        self.e_f = [sb(st, f"e_f{i}", [128, 512], F32) for i in range(4)]
        self.e_fb = [Buf(f"e_f{i}") for i in range(4)]
        self.e_o = [sb(st, f"e_o{i}", [128, 512], BF16) for i in range(3)]
        self.e_ob = [Buf(f"e_o{i}") for i in range(3)]
        self.e_tab = [sb(st, f"e_tab{i}", [128, 2, 512], F32) for i in range(2)]
        self.e_tabb = [Buf(f"e_tab{i}") for i in range(2)]
        self.e_kt = [sb(st, f"e_kt{i}", [128, 512], BF16) for i in range(2)]
        self.e_ktb = [Buf(f"e_kt{i}") for i in range(2)]
        self._eo = 0
        self._ebf = 0
        self._ef = 0
        self._etab = 0
        self._ekt = 0

    def _rot(self, attr, n):
        v = getattr(self, attr)
        setattr(self, attr, (v + 1) % n)
        return v

    def ep_qknorm(self, pi, n, wcol, dst_ap, dname):
        nc, S = self.nc, self.S
        P1 = self.ps[pi][:, 0:n]
        bi = self._rot("_ebf", 2)
        sq, sqb = self.e_bf[bi], self.e_bfb[bi]
        S.op(S.act, lambda: nc.scalar.activation(out=sq[:, 0:n], in_=P1, func=AF.Square), reads=[self.psb[pi]], writes=[sqb])
        p2 = 4 + (pi % 2)
        self.mm(self.ps[p2][:, 0:n], self.psb[p2], [(self.ones_bf[:], sq[:, 0:n])], reads=[sqb, self.cb])
        fi = self._rot("_ef", 4)
        rt, rtb = self.e_f[fi], self.e_fb[fi]
        S.op(S.act, lambda: nc.scalar.activation(out=rt[:, 0:n], in_=self.ps[p2][:, 0:n], func=AF.Sqrt,
                                                 bias=self.eps_t[:], scale=1.0 / 128),
             reads=[self.psb[p2], self.cb], writes=[rtb])
        S.op(S.dve, lambda: nc.vector.reciprocal(out=rt[:, 0:n], in_=rt[:, 0:n]), reads=[rtb], writes=[rtb])
        oi = self._rot("_eo", 3)
        o, ob = self.e_o[oi], self.e_ob[oi]
        S.op(S.dve, lambda: nc.vector.scalar_tensor_tensor(out=o[:, 0:n], in0=P1, scalar=wcol[:, 0:1], in1=rt[:, 0:n],
                                                           op0=ALU.mult, op1=ALU.mult),
             reads=[self.psb[pi], rtb, self.cb], writes=[ob])
        S.dma(S.sp, dst_ap, o[:, 0:n], owner=ob, reads=[ob], writes=[self.dbufs[dname]])

    def ep_rope(self, pi, n, cos_ap, sin_ap, tabkey, dst_ap, dname, tok_dst=None):
        nc, S = self.nc, self.S
        P1 = self.ps[pi][:, 0:n]
        if self._tabkey != tabkey:
            ti = self._rot("_etab", 2)
            tab, tabb = self.e_tab[ti], self.e_tabb[ti]
            S.dma(S.sp, tab[:, 0, 0:n], cos_ap, owner=tabb, writes=[tabb])
            S.dma(S.sp, tab[:, 1, 0:n], sin_ap, owner=tabb, writes=[tabb])
            self._tabkey = tabkey
            self._tab = (tab, tabb)
        tab, tabb = self._tab
        bi = self._rot("_ebf", 2)
        xb, xbb = self.e_bf[bi], self.e_bfb[bi]
        S.op(S.act, lambda: nc.scalar.activation(out=xb[:, 0:n], in_=P1, func=AF.Copy), reads=[self.psb[pi]], writes=[xbb])
        p2 = 4 + (pi % 2)
        self.mm(self.ps[p2][:, 0:n], self.psb[p2], [(self.rperm_bf[:], xb[:, 0:n])], reads=[xbb, self.cb])
        f1 = self._rot("_ef", 4)
        t1, t1b = self.e_f[f1], self.e_fb[f1]
        S.op(S.dve, lambda: nc.vector.tensor_tensor(out=t1[:, 0:n], in0=P1, in1=tab[:, 0, 0:n], op=ALU.mult),
             reads=[self.psb[pi], tabb, xbb], writes=[t1b])
        f2 = self._rot("_ef", 4)
        t2, t2b = self.e_f[f2], self.e_fb[f2]
        S.op(S.dve, lambda: nc.vector.tensor_tensor(out=t2[:, 0:n], in0=self.ps[p2][:, 0:n], in1=tab[:, 1, 0:n], op=ALU.mult),
             reads=[self.psb[p2], tabb], writes=[t2b])
        oi = self._rot("_eo", 3)
        o, ob = self.e_o[oi], self.e_ob[oi]
        S.op(S.dve, lambda: nc.vector.tensor_tensor(out=o[:, 0:n], in0=t1[:, 0:n], in1=t2[:, 0:n], op=ALU.add),
             reads=[t1b, t2b], writes=[ob])
        S.dma(S.sp, dst_ap, o[:, 0:n], owner=ob, reads=[ob], writes=[self.dbufs[dname]])
        if tok_dst is not None:
            p3 = 6 + (pi % 2)
            pv = self.ps[p3][:].bitcast(BF16)
            nb = n // 128
            for j in range(nb):
                S.op(S.pe, lambda j=j: nc.tensor.transpose(out=pv[:, j * 128:(j + 1) * 128], in_=o[:, j * 128:(j + 1) * 128],
                                                           identity=self.ident_bf[:]),
                     reads=[ob, self.cb], writes=[self.psb[p3]], signal=(j == nb - 1))
            ki = self._rot("_ekt", 2)
            kt, ktb = self.e_kt[ki], self.e_ktb[ki]
            S.op(S.act, lambda: nc.scalar.activation(out=kt[:, 0:n], in_=pv[:, 0:n], func=AF.Copy),
                 reads=[self.psb[p3]], writes=[ktb])
            dst, dn = tok_dst
            S.dma(S.sp, dst, kt[:, 0:n].rearrange("p (j d) -> p j d", d=128), owner=ktb, reads=[ktb], writes=[self.dbufs[dn]])

    def ep_act(self, pi, n, func, dst_ap, dname):
        nc, S = self.nc, self.S
        oi = self._rot("_eo", 3)
        o, ob = self.e_o[oi], self.e_ob[oi]
        S.op(S.act, lambda: nc.scalar.activation(out=o[:, 0:n], in_=self.ps[pi][:, 0:n], func=func),
             reads=[self.psb[pi]], writes=[ob])
        S.dma(S.sp, dst_ap, o[:, 0:n], owner=ob, reads=[ob], writes=[self.dbufs[dname]])

    def proj_phase(self, own):
        nc, S = self.nc, self.S
        with ExitStack() as st:
            self.wt = [self.sb(st, f"wt{i}", [128, KC, 256], BF16) for i in range(2)]
            self.wtb = [Buf(f"wt{i}") for i in range(2)]
            self.ep_alloc(st)
            self._pr = 0
            self._tabkey = None
            base = 0 if own else TOK
            jobs = []

            def fm_job(c0, ep):
                def fn(wt, wb, c0_, ep=ep):
                    for sub in range(2):
                        ep(wt, wb, sub, c0_ + sub * 128)
                jobs.append((c0, fn))

            def tm_job(c0, ep, ntile):
                def fn(wt, wb, c0_, ep=ep):
                    for t in range(ntile):
                        pi = self.tm_block(wt, wb, t * 128)
                        ep(pi, t, c0_)
                jobs.append((c0, fn))

            def ep_rk(wt, wb, sub, col):
                h = (col - O_RK) // 128
                for tb in range(4):
                    pi = self.fm_block(wt, wb, sub, tb * 512, 512)
                    l0 = base + tb * 512
                    self.ep_rope(pi, 512, self.cosk[:, l0:l0 + 512], self.sink_[:, l0:l0 + 512], ("k", l0),
                                 self.rkT[h, :, l0:l0 + 512], "rkT",
                                 tok_dst=(self.rkt[l0:l0 + 512, h * 128:(h + 1) * 128].rearrange("(j p) d -> p j d", p=128), "rkt"))
            for c0 in range(O_RK, O_RV, 256):
                fm_job(c0, ep_rk)

            def ep_rv(pi, t, c0_):
                l0 = base + t * 128
                self.ep_act(pi, 256, AF.Copy, self.rv[l0:l0 + 128, c0_ - O_RV:c0_ - O_RV + 256], "rv")
            for c0 in range(O_RV, O_RG, 256):
                tm_job(c0, ep_rv, 16)

            def ep_ak(wt, wb, sub, col):
                g = (col - O_AK) // 128
                if own:
                    for tb in range(4):
                        pi = self.fm_block(wt, wb, sub, tb * 512, 512)
                        self.ep_qknorm(pi, 512, self.knw_t, self.kT[g, :, tb * 512:(tb + 1) * 512], "kT")
                else:
                    pi = self.fm_block(wt, wb, sub, 0, 128)
                    self.ep_qknorm(pi, 128, self.knw_t, self.kT[g, :, TOK:TOK + 128], "kT")
            for c0 in range(O_AK, O_AV, 256):
                fm_job(c0, ep_ak)

            def ep_av(pi, t, c0_):
                l0 = base + t * 128
                self.ep_act(pi, 256, AF.Copy, self.av[l0:l0 + 128, c0_ - O_AV:c0_ - O_AV + 256], "av")
            for c0 in range(O_AV, O_RQ, 256):
                tm_job(c0, ep_av, 16 if own else 1)

            if own:
                def ep_aq(wt, wb, sub, col):
                    h = (col - O_AQ) // 128
                    for tb in range(4):
                        pi = self.fm_block(wt, wb, sub, tb * 512, 512)
                        self.ep_qknorm(pi, 512, self.qnw_t, self.qT[h, :, tb * 512:(tb + 1) * 512], "qT")
                for c0 in range(O_AQ, O_AK, 256):
                    fm_job(c0, ep_aq)

                def ep_rq(wt, wb, sub, col):
                    h = (col - O_RQ) // 128
                    for tb in range(4):
                        pi = self.fm_block(wt, wb, sub, tb * 512, 512)
                        l0 = tb * 512
                        self.ep_rope(pi, 512, self.cosq[:, l0:l0 + 512], self.sinq[:, l0:l0 + 512], ("q", l0),
                                     self.rqT[h, :, l0:l0 + 512], "rqT")
                for c0 in range(O_RQ, O_RK, 256):
                    fm_job(c0, ep_rq)

                def ep_rg(pi, t, c0_):
                    self.ep_act(pi, 256, AF.Silu, self.rg[t * 128:(t + 1) * 128, c0_ - O_RG:c0_ - O_RG + 256], "rg")
                for c0 in range(O_RG, O_GA, 256):
                    tm_job(c0, ep_rg, 16)

                def ep_g(wt, wb, sub, col):
                    if col < O_GR:
                        dst, dn, f0 = self.sga, "sga", col - O_GA
                    else:
                        dst, dn, f0 = self.sgr, "sgr", col - O_GR
                    for tb in range(4):
                        pi = self.fm_block(wt, wb, sub, tb * 512, 512)
                        self.ep_act(pi, 512, AF.Sigmoid, dst[f0:f0 + 128, tb * 512:(tb + 1) * 512], dn)
                for c0 in range(O_GA, O_END, 256):
                    fm_job(c0, ep_g)

            self.project(self.w_in, "w_in", jobs, "in")
            S.barrier()

    def p2_other(self):
        import os
        sk = os.environ.get("KSKIP", "")
        if "normT" not in sk:
            self.norm_T(self.x_oth, "x_oth", 16, self.am, self.bm)
        if "proj" not in sk:
            self.proj_phase(False)

    def p3_own(self):
        self.norm_T(self.x_own, "x_own", 16, self.am, self.bm)
        self.proj_phase(True)


    def p4_attn(self):
        nc, S = self.nc, self.S
        with ExitStack() as st:
            sb = self.sb
            qg = sb(st, "a_q", [128, 4, TOK], BF16); qgb = Buf("a_q")
            kg = sb(st, "a_k", [128, TOK + 128], BF16); kgb = Buf("a_k")
            vg = sb(st, "a_v", [128, 17, 128], BF16); vgb = Buf("a_v")
            bg = sb(st, "a_b", [128, 1536], F32); bgb = Buf("a_b")
            es = sb(st, "a_es", [128, 16], F32); esb = Buf("a_es")
            lg = [sb(st, f"a_lg{i}", [128, 512], F32) for i in range(3)]
            lgb = [Buf(f"a_lg{i}") for i in range(3)]
            pt = [sb(st, f"a_pt{i}", [128, 512], BF16) for i in range(6)]
            ptb = [Buf(f"a_pt{i}") for i in range(6)]
            dn = [sb(st, f"a_dn{i}", [128, 512], F32) for i in range(2)]
            dnb = [Buf(f"a_dn{i}") for i in range(2)]
            S.dma(S.sp, es[:], self.sinkr, owner=esb, writes=[esb])
            S.op(S.act, lambda: nc.scalar.activation(out=es[:], in_=es[:], func=AF.Exp), reads=[esb], writes=[esb])
            u = 0
            for g in range(4):
                S.dma(S.sp, qg[:], self.qT[4 * g:4 * g + 4].rearrange("h d t -> d h t"), owner=qgb,
                      reads=[self.dbufs["qT"]], writes=[qgb])
                S.dma(S.sp, kg[:], self.kT[g], owner=kgb, reads=[self.dbufs["kT"]], writes=[kgb])
                S.dma(S.sp, vg[:], self.av[:, g * 128:(g + 1) * 128].rearrange("(n p) d -> p n d", p=128), owner=vgb,
                      reads=[self.dbufs["av"]], writes=[vgb])
                S.dma(S.sp, bg[:], self.biasT[g], owner=bgb, writes=[bgb])
                for qb in range(16):
                    kbs = [kb for kb in range(3) if qb + kb - 1 >= 0]
                    sbank = [0, 1, 2] if u % 2 == 0 else [5, 6, 7]
                    pts = []
                    for kb in kbs:
                        blk = qb + kb - 1
                        pi = sbank[kb]
                        self.mm(self.ps[pi][:].rearrange("p (h q) -> p h q", h=4), self.psb[pi],
                                [(kg[:, blk * 128:(blk + 1) * 128], qg[:, :, qb * 128:(qb + 1) * 128])],
                                reads=[kgb, qgb])
                        S.op(S.dve, lambda: nc.vector.tensor_tensor(out=lg[kb][:], in0=self.ps[pi][:],
                                                                    in1=bg[:, kb * 512:(kb + 1) * 512], op=ALU.add),
                             reads=[self.psb[pi], bgb], writes=[lgb[kb]])
                        pj = (u % 2) * 3 + kb
                        S.op(S.act, lambda: nc.scalar.activation(out=pt[pj][:], in_=lg[kb][:], func=AF.Exp),
                             reads=[lgb[kb]], writes=[ptb[pj]])
                        pts.append((blk, pj))
                    self.mm(self.ps[3][:], self.psb[3], [(vg[:, blk, :], pt[pj][:]) for blk, pj in pts],
                            reads=[vgb] + [ptb[pj] for _, pj in pts])
                    self.mm(self.ps[4][:], self.psb[4], [(self.ones_bf[:], pt[pj][:]) for blk, pj in pts],
                            reads=[self.cb] + [ptb[pj] for _, pj in pts])
                    d_ = dn[u % 2]; db_ = dnb[u % 2]
                    for hh in range(4):
                        h = 4 * g + hh
                        S.op(S.dve, lambda: nc.vector.tensor_scalar(out=d_[:, hh * 128:(hh + 1) * 128],
                                                                    in0=self.ps[4][:, hh * 128:(hh + 1) * 128],
                                                                    scalar1=es[:, h:h + 1], scalar2=None, op0=ALU.add),
                             reads=[self.psb[4], esb], writes=[db_])
                    S.op(S.dve, lambda: nc.vector.reciprocal(out=d_[:], in_=d_[:]), reads=[db_], writes=[db_])
                    S.op(S.dve, lambda: nc.vector.tensor_tensor(
                        out=self.HT[:, 4 * g:4 * g + 4, qb * 128:(qb + 1) * 128],
                        in0=self.ps[3][:].rearrange("p (h q) -> p h q", h=4),
                        in1=d_[:].rearrange("p (h q) -> p h q", h=4), op=ALU.mult),
                        reads=[self.psb[3], db_], writes=self.HTb[4 * g:4 * g + 4])
                    u += 1
            S.barrier()

    def p5_ret(self):
        nc, S = self.nc, self.S
        with ExitStack() as st:
            sb = self.sb
            tb = Buf("r_tabs")
            lgf = sb(st, "r_lgf", [128, 8], F32)
            lgb_ = sb(st, "r_lgb", [128, 8], F32)
            cpos = sb(st, "r_cpos", [128, 128], F32)
            cneg = sb(st, "r_cneg", [128, 128], F32)
            ccol = sb(st, "r_ccol", [128, 2], F32)
            crow = sb(st, "r_crow", [128, 256], F32)
            cdf = sb(st, "r_cdf", [128, 8], F32)
            cdb = sb(st, "r_cdb", [128, 8], F32)
            tmp = sb(st, "r_tmp", [128, 128], F32)
            for dst, src_ in ((lgf, self.dec_f), (lgb_, self.dec_b), (cpos, self.cpos), (cneg, self.cneg),
                              (ccol, self.ccol), (crow, self.crow)):
                S.dma(S.sp, dst[:], src_, owner=tb, writes=[tb])
            for t_ in (lgf, lgb_):
                S.op(S.act, lambda: nc.scalar.activation(out=t_[:], in_=t_[:], func=AF.Exp), reads=[tb], writes=[tb])
                S.op(S.dve, lambda: nc.vector.tensor_scalar(out=t_[:], in0=t_[:], scalar1=-1.0, scalar2=None, op0=ALU.mult),
                     reads=[tb], writes=[tb])
            S.op(S.act, lambda: nc.scalar.activation(out=cdf[:], in_=lgf[:], func=AF.Exp, scale=128.0), reads=[tb], writes=[tb])
            S.op(S.act, lambda: nc.scalar.activation(out=cdb[:], in_=lgb_[:], func=AF.Exp, scale=128.0), reads=[tb], writes=[tb])
            hb = Buf("r_htab")
            DT = sb(st, "r_DT", [128, 128], F32)
            kdf = sb(st, "r_kdf", [128, 1], F32)
            kdb = sb(st, "r_kdb", [128, 1], F32)
            qdf = sb(st, "r_qdf", [128, 128], F32)
            qdb = sb(st, "r_qdb", [128, 128], F32)
            gnw = sb(st, "r_gnw", [128, 256], F32)
            gnb = sb(st, "r_gnb", [128, 256], F32)

            kt = sb(st, "r_kt", [128, 32, 128], BF16); ktb = Buf("r_kt")
            vv = sb(st, "r_v", [128, 32, 256], BF16); vvb = Buf("r_v")
            qT = sb(st, "r_qT", [128, TOK], BF16); qTb = Buf("r_qT")
            kT = sb(st, "r_kT", [128, TOK], BF16); kTb = Buf("r_kT")
            rgt = [sb(st, f"r_rg{i}", [128, 256], BF16) for i in range(2)]; rgb = [Buf(f"r_rg{i}") for i in range(2)]
            kdec = sb(st, "r_kdec", [128, 8, 128], BF16); kdecb = [Buf(f"r_kdec{i}") for i in range(8)]
            KV = sb(st, "r_KV", [128, 8, 256], F32); KVb = [Buf(f"r_KV{i}") for i in range(8)]
            Sf = sb(st, "r_Sf", [128, 16, 256], BF16); Sfb = [Buf(f"r_Sf{i}") for i in range(16)]
            Sb_ = sb(st, "r_Sb", [128, 16, 256], BF16); Sbb = [Buf(f"r_Sb{i}") for i in range(16)]
            run = sb(st, "r_run", [128, 2, 256], F32); runb = [Buf("r_runf"), Buf("r_runb")]
            scd = [sb(st, f"r_scd{i}", [128, 128], BF16) for i in range(2)]; scdb = [Buf(f"r_scd{i}") for i in range(2)]
            qfb = [sb(st, f"r_qfb{i}", [128, 2, 128], BF16) for i in range(2)]; qfbb = [Buf(f"r_qfb{i}") for i in range(2)]
            stt = [sb(st, f"r_st{i}", [128, 12], F32) for i in range(2)]; sttb = [Buf(f"r_st{i}") for i in range(2)]
            yn = [sb(st, f"r_yn{i}", [128, 256], F32) for i in range(2)]; ynb = [Buf(f"r_yn{i}") for i in range(2)]
            yo = [sb(st, f"r_yo{i}", [128, 256], BF16) for i in range(2)]; yob = [Buf(f"r_yo{i}") for i in range(2)]
            dd = self.dbufs

            def states(h, chunks, kd, cd, ri, store):
                for g0 in range(0, len(chunks), 8):
                    grp = chunks[g0:g0 + 8]
                    for i, n in enumerate(grp):
                        eng, ns = (S.dve, nc.vector) if i % 2 else (S.pool, nc.gpsimd)
                        S.op(eng, lambda: ns.tensor_scalar(out=kdec[:, i, :], in0=kt[:, n, :], scalar1=kd[:, 0:1], scalar2=None,
                                                           op0=ALU.mult), reads=[ktb, hb], writes=[kdecb[i]])
                    for i, n in enumerate(grp):
                        pi = i % 4
                        half = i // 4
                        pa = self.ps[pi][:, half * 256:(half + 1) * 256]
                        self.mm(pa, self.psb[pi], [(kdec[:, i, :], vv[:, n, :])], reads=[kdecb[i], vvb])
                        S.op(S.act, lambda: nc.scalar.activation(out=KV[:, i, :], in_=pa, func=AF.Copy),
                             reads=[self.psb[pi]], writes=[KVb[i]])
                    for i, n in enumerate(grp):
                        S.op(S.dve, lambda: nc.vector.scalar_tensor_tensor(out=run[:, ri, :], in0=run[:, ri, :], scalar=cd,
                                                                           in1=KV[:, i, :], op0=ALU.mult, op1=ALU.add),
                             reads=[KVb[i], tb, runb[ri]], writes=[runb[ri]])
                        store(n)

            for h in range(8):
                S.dma(S.sp, kt[:], self.rkt[:, h * 128:(h + 1) * 128].rearrange("(n p) d -> p n d", p=128), owner=ktb,
                      reads=[dd["rkt"]], writes=[ktb])
                S.dma(S.sp, vv[:], self.rv[:, h * 256:(h + 1) * 256].rearrange("(n p) e -> p n e", p=128), owner=vvb,
                      reads=[dd["rv"]], writes=[vvb])
                S.dma(S.sp, qT[:], self.rqT[h], owner=qTb, reads=[dd["rqT"]], writes=[qTb])
                S.dma(S.sp, kT[:], self.rkT[h, :, 0:TOK], owner=kTb, reads=[dd["rkT"]], writes=[kTb])
                S.dma(S.sp, gnw[:], self.gnw[:, h * 256:(h + 1) * 256], owner=hb, writes=[hb])
                S.dma(S.sp, gnb[:], self.gnb[:, h * 256:(h + 1) * 256], owner=hb, writes=[hb])
                S.op(S.dve, lambda: nc.vector.tensor_scalar(out=tmp[:], in0=cpos[:], scalar1=lgf[:, h:h + 1], scalar2=None,
                                                            op0=ALU.mult), reads=[tb], writes=[hb])
                S.op(S.dve, lambda: nc.vector.scalar_tensor_tensor(out=tmp[:], in0=cneg[:], scalar=lgb_[:, h:h + 1], in1=tmp[:],
                                                                   op0=ALU.mult, op1=ALU.add), reads=[tb, hb], writes=[hb])
                S.op(S.act, lambda: nc.scalar.activation(out=DT[:], in_=tmp[:], func=AF.Exp), reads=[hb], writes=[hb])
                S.op(S.act, lambda: nc.scalar.activation(out=kdf[:], in_=ccol[:, 0:1], func=AF.Exp, scale=lgf[:, h:h + 1]),
                     reads=[tb], writes=[hb])
                S.op(S.act, lambda: nc.scalar.activation(out=kdb[:], in_=ccol[:, 1:2], func=AF.Exp, scale=lgb_[:, h:h + 1]),
                     reads=[tb], writes=[hb])
                S.op(S.act, lambda: nc.scalar.activation(out=qdf[:], in_=crow[:, 0:128], func=AF.Exp, scale=lgf[:, h:h + 1]),
                     reads=[tb], writes=[hb])
                S.op(S.act, lambda: nc.scalar.activation(out=qdb[:], in_=crow[:, 128:256], func=AF.Exp, scale=lgb_[:, h:h + 1]),
                     reads=[tb], writes=[hb])
                S.op(S.dve, lambda: nc.vector.memset(run[:, 1, :], 0.0), writes=[runb[1]])

                def store_b(n):
                    if n - 1 <= 15:
                        S.op(S.act, lambda: nc.scalar.activation(out=Sb_[:, n - 1, :], in_=run[:, 1, :], func=AF.Copy),
                             reads=[runb[1]], writes=[Sbb[n - 1]])
                states(h, list(range(31, 0, -1)), kdb, cdb[:, h:h + 1], 1, store_b)
                S.op(S.dve, lambda: nc.vector.memset(run[:, 0, :], 0.0), writes=[runb[0]])
                S.op(S.act, lambda: nc.scalar.activation(out=Sf[:, 0, :], in_=run[:, 0, :], func=AF.Copy),
                     reads=[runb[0]], writes=[Sfb[0]])

                def store_f(n):
                    S.op(S.act, lambda: nc.scalar.activation(out=Sf[:, n + 1, :], in_=run[:, 0, :], func=AF.Copy),
                         reads=[runb[0]], writes=[Sfb[n + 1]])
                states(h, list(range(0, 15)), kdf, cdf[:, h:h + 1], 0, store_f)
                for n in range(16):
                    b = n % 2
                    c0, c1 = n * 128, (n + 1) * 128
                    S.dma(S.sp, rgt[b][:], self.rg[c0:c1, h * 256:(h + 1) * 256], owner=rgb[b], reads=[dd["rg"]], writes=[rgb[b]])
                    p_s = 4 + b
                    self.mm(self.ps[p_s][:, 0:128], self.psb[p_s], [(kT[:, c0:c1], qT[:, c0:c1])], reads=[kTb, qTb])
                    S.op(S.dve, lambda: nc.vector.tensor_tensor(out=scd[b][:], in0=self.ps[p_s][:, 0:128], in1=DT[:], op=ALU.mult),
                         reads=[self.psb[p_s], hb], writes=[scdb[b]])
                    S.op(S.pool, lambda: nc.gpsimd.tensor_tensor(out=qfb[b][:, 0, :], in0=qT[:, c0:c1], in1=qdf[:], op=ALU.mult),
                         reads=[qTb, hb], writes=[qfbb[b]])
                    S.op(S.pool, lambda: nc.gpsimd.tensor_tensor(out=qfb[b][:, 1, :], in0=qT[:, c0:c1], in1=qdb[:], op=ALU.mult),
                         reads=[qTb, hb], writes=[qfbb[b]])
                    p_y = 6 + b
                    self.mm(self.ps[p_y][:, 0:256], self.psb[p_y],
                            [(scd[b][:], vv[:, n, :]), (qfb[b][:, 0, :], Sf[:, n, :]), (qfb[b][:, 1, :], Sb_[:, n, :])],
                            reads=[scdb[b], vvb, qfbb[b], Sfb[n], Sbb[n]])
                    y = self.ps[p_y][:, 0:256]
                    s_ = stt[b]
                    S.op(S.dve, lambda: nc.vector.bn_stats(out=s_[:, 0:6], in_=y), reads=[self.psb[p_y]], writes=[sttb[b]])
                    S.op(S.dve, lambda: nc.vector.bn_aggr(out=s_[:, 6:8], in_=s_[:, 0:6]), reads=[sttb[b]], writes=[sttb[b]])
                    S.op(S.act, lambda: nc.scalar.activation(out=s_[:, 8:9], in_=s_[:, 7:8], func=AF.Sqrt, bias=self.eps_t[:], scale=1.0),
                         reads=[sttb[b], self.cb], writes=[sttb[b]])
                    S.op(S.dve, lambda: nc.vector.reciprocal(out=s_[:, 9:10], in_=s_[:, 8:9]), reads=[sttb[b]], writes=[sttb[b]])
                    S.op(S.dve, lambda: nc.vector.tensor_scalar(out=yn[b][:], in0=y, scalar1=s_[:, 6:7], scalar2=s_[:, 9:10],
                                                                op0=ALU.subtract, op1=ALU.mult),
                         reads=[self.psb[p_y], sttb[b]], writes=[ynb[b]])
                    S.op(S.pool, lambda: nc.gpsimd.tensor_tensor(out=yn[b][:], in0=yn[b][:], in1=gnw[:], op=ALU.mult),
                         reads=[ynb[b], hb], writes=[ynb[b]])
                    S.op(S.pool, lambda: nc.gpsimd.tensor_tensor(out=yn[b][:], in0=yn[b][:], in1=gnb[:], op=ALU.add),
                         reads=[ynb[b], hb], writes=[ynb[b]])
                    S.op(S.pool, lambda: nc.gpsimd.tensor_tensor(out=yo[b][:], in0=yn[b][:], in1=rgt[b][:], op=ALU.mult),
                         reads=[ynb[b], rgb[b]], writes=[yob[b]])
                    p_t = 2 + b
                    pv = self.ps[p_t][:].bitcast(BF16)
                    for eb in range(2):
                        S.op(S.pe, lambda: nc.tensor.transpose(out=pv[:, eb * 128:(eb + 1) * 128], in_=yo[b][:, eb * 128:(eb + 1) * 128],
                                                               identity=self.ident_bf[:]),
                             reads=[yob[b], self.cb], writes=[self.psb[p_t]], signal=(eb == 1))
                    S.op(S.act, lambda: nc.scalar.activation(out=self.HT[:, 16 + 2 * h:18 + 2 * h, c0:c1],
                                                             in_=pv[:, 0:256].rearrange("p (e i) -> p e i", e=2), func=AF.Copy),
                         reads=[self.psb[p_t]], writes=self.HTb[16 + 2 * h:18 + 2 * h])
            S.barrier()
            if "htd" in self.debug:
                S.dma(S.sp, self.htd, self.HT[:], owner=self.HTb[0], reads=self.HTb, writes=[self.dbufs["htd"]])
                S.barrier()


    def p6_up(self):
        nc, S = self.nc, self.S
        if "htd" in self.ext_in:
            S.dma(S.sp, self.HT[:], self.htd, owner=self.HTb[0], reads=[self.dbufs["htd"]], writes=self.HTb)
            S.barrier()
        with ExitStack() as st:
            sb = self.sb
            wt = [sb(st, f"u_wt{i}", [128, KC, 256], BF16) for i in range(2)]
            wtb = [Buf(f"u_wt{i}") for i in range(2)]
            ga = [sb(st, f"u_ga{i}", [128, 2, 512], BF16) for i in range(2)]; gab = [Buf(f"u_ga{i}") for i in range(2)]
            t1 = [sb(st, f"u_t1{i}", [128, 512], F32) for i in range(2)]; t1b = [Buf(f"u_t1{i}") for i in range(2)]
            t2 = [sb(st, f"u_t2{i}", [128, 512], F32) for i in range(2)]; t2b = [Buf(f"u_t2{i}") for i in range(2)]
            mo = [sb(st, f"u_mo{i}", [128, 512], BF16) for i in range(2)]; mob = [Buf(f"u_mo{i}") for i in range(2)]
            wa = self.w_ua.rearrange("(kc p) n -> p kc n", p=128)
            wr = self.w_ur.rearrange("(kc p) n -> p kc n", p=128)
            dd = self.dbufs
            u = 0
            for fb2 in range(16):
                b = fb2 % 2
                c0 = fb2 * 256
                S.dma(S.pool, wt[b][:, 0:16, :], wa[:, :, c0:c0 + 256], owner=wtb[b], writes=[wtb[b]])
                S.dma(S.pool, wt[b][:, 16:32, :], wr[:, :, c0:c0 + 256], owner=wtb[b], writes=[wtb[b]])
                for sub in range(2):
                    f0 = c0 + sub * 128
                    for tb_ in range(4):
                        i2 = u % 2
                        t0 = tb_ * 512
                        S.dma(S.act, ga[i2][:, 0, :], self.sga[f0:f0 + 128, t0:t0 + 512], owner=gab[i2], reads=[dd["sga"]], writes=[gab[i2]])
                        S.dma(S.act, ga[i2][:, 1, :], self.sgr[f0:f0 + 128, t0:t0 + 512], owner=gab[i2], reads=[dd["sgr"]], writes=[gab[i2]])
                        pa, pr = (0, 1) if i2 == 0 else (2, 3)
                        self.mm(self.ps[pa][:], self.psb[pa],
                                [(wt[b][:, kc, sub * 128:(sub + 1) * 128], self.HT[:, kc, t0:t0 + 512]) for kc in range(16)],
                                reads=[wtb[b]] + self.HTb[0:16])
                        self.mm(self.ps[pr][:], self.psb[pr],
                                [(wt[b][:, kc, sub * 128:(sub + 1) * 128], self.HT[:, kc, t0:t0 + 512]) for kc in range(16, 32)],
                                reads=[wtb[b]] + self.HTb[16:32])
                        S.op(S.dve, lambda: nc.vector.tensor_tensor(out=t1[i2][:], in0=self.ps[pa][:], in1=ga[i2][:, 0, :], op=ALU.mult),
                             reads=[self.psb[pa], gab[i2]], writes=[t1b[i2]])
                        S.op(S.dve, lambda: nc.vector.tensor_tensor(out=t2[i2][:], in0=self.ps[pr][:], in1=ga[i2][:, 1, :], op=ALU.mult),
                             reads=[self.psb[pr], gab[i2]], writes=[t2b[i2]])
                        S.op(S.dve, lambda: nc.vector.tensor_tensor(out=mo[i2][:], in0=t1[i2][:], in1=t2[i2][:], op=ALU.add),
                             reads=[t1b[i2], t2b[i2]], writes=[mob[i2]])
                        S.dma(S.sp, self.mrg[f0:f0 + 128, t0:t0 + 512], mo[i2][:], owner=mob[i2], reads=[mob[i2]], writes=[dd["mrg"]])
                        u += 1
            S.barrier()
        for kc in range(KC):
            S.dma(S.sp, self.HT[:, kc, :], self.mrg[kc * 128:(kc + 1) * 128, :], owner=self.HTb[kc],
                  reads=[self.dbufs["mrg"]], writes=[self.HTb[kc]])
        S.barrier()

    def p7_out(self):
        nc, S = self.nc, self.S
        with ExitStack() as st:
            sb = self.sb
            wt = [sb(st, f"o_wt{i}", [128, KC, 256], BF16) for i in range(2)]
            wtb = [Buf(f"o_wt{i}") for i in range(2)]
            gm = [sb(st, f"o_gm{i}", [128, 256], F32) for i in range(2)]; gmb = [Buf(f"o_gm{i}") for i in range(2)]
            xt = [sb(st, f"o_x{i}", [128, 256], F32) for i in range(3)]; xtb = [Buf(f"o_x{i}") for i in range(3)]
            tt = [sb(st, f"o_t{i}", [128, 256], F32) for i in range(2)]; ttb = [Buf(f"o_t{i}") for i in range(2)]
            wv = self.w_out.rearrange("(kc p) n -> p kc n", p=128)
            dd = self.dbufs
            u = 0
            for nb in range(16):
                b = nb % 2
                c0 = nb * 256
                S.dma(S.pool, wt[b][:], wv[:, :, c0:c0 + 256], owner=wtb[b], writes=[wtb[b]])
                S.dma(S.sp, gm[b][:], self.modd[2 * D + c0:2 * D + c0 + 256].partition_broadcast(128), owner=gmb[b],
                      reads=[dd["modd"]], writes=[gmb[b]])
                for t in range(16):
                    i3 = u % 3
                    i2 = u % 2
                    S.dma(S.act, xt[i3][:], self.x_own[t * 128:(t + 1) * 128, c0:c0 + 256], owner=xtb[i3], writes=[xtb[i3]])
                    pi = u % 4
                    self.mm(self.ps[pi][:, 0:256], self.psb[pi],
                            [(self.HT[:, kc, t * 128:(t + 1) * 128], wt[b][:, kc, :]) for kc in range(KC)],
                            reads=[wtb[b]] + self.HTb)
                    S.op(S.dve, lambda: nc.vector.tensor_tensor(out=tt[i2][:], in0=self.ps[pi][:, 0:256], in1=gm[b][:], op=ALU.mult),
                         reads=[self.psb[pi], gmb[b]], writes=[ttb[i2]])
                    S.op(S.dve, lambda: nc.vector.tensor_tensor(out=xt[i3][:], in0=xt[i3][:], in1=tt[i2][:], op=ALU.add),
                         reads=[ttb[i2], xtb[i3]], writes=[xtb[i3]])
                    S.dma(S.sp, self.x1[t * 128:(t + 1) * 128, c0:c0 + 256], xt[i3][:], owner=xtb[i3], reads=[xtb[i3]], writes=[dd["x1"]])
                    u += 1
            S.barrier()

    def p8_norm2(self):
        nc, S = self.nc, self.S
        self.norm_T(self.x1, "x1", 16, self.af, self.bf)
        with ExitStack() as st:
            sb = self.sb
            rw = sb(st, "g_rw", [128, KC, NEXP], BF16); rwb = Buf("g_rw")
            rbt = sb(st, "g_rb", [128, NEXP], F32)
            lg = [sb(st, f"g_lg{i}", [128, NEXP], F32) for i in range(2)]; lgb = [Buf(f"g_lg{i}") for i in range(2)]
            m8 = [sb(st, f"g_m8{i}", [128, 12], F32) for i in range(2)]; m8b = [Buf(f"g_m8{i}") for i in range(2)]
            mk = [sb(st, f"g_mk{i}", [128, NEXP], F32) for i in range(2)]; mkb = [Buf(f"g_mk{i}") for i in range(2)]
            ex = [sb(st, f"g_ex{i}", [128, NEXP], F32) for i in range(2)]; exb = [Buf(f"g_ex{i}") for i in range(2)]
            cm = [sb(st, f"g_cm{i}", [128, NEXP], F32) for i in range(2)]; cmb = [Buf(f"g_cm{i}") for i in range(2)]
            cmh = [sb(st, f"g_cmh{i}", [128, NEXP], BF16) for i in range(2)]; cmhb = [Buf(f"g_cmh{i}") for i in range(2)]
            S.dma(S.pool, rw[:], self.router_w.rearrange("(kc p) e -> p kc e", p=128), owner=rwb, writes=[rwb])
            S.dma(S.sp, rbt[:], self.rb, owner=rwb, writes=[rwb])
            dd = self.dbufs
            for t in range(16):
                b = t % 2
                pi = t % 2
                self.mm(self.ps[pi][:, 0:NEXP], self.psb[pi],
                        [(self.HT[:, kc, t * 128:(t + 1) * 128], rw[:, kc, :]) for kc in range(KC)], reads=[rwb] + self.HTb)
                S.op(S.dve, lambda: nc.vector.tensor_tensor(out=lg[b][:], in0=self.ps[pi][:, 0:NEXP], in1=rbt[:], op=ALU.add),
                     reads=[self.psb[pi], rwb], writes=[lgb[b]])
                S.op(S.dve, lambda: nc.vector.max(out=m8[b][:, 0:8], in_=lg[b][:]), reads=[lgb[b]], writes=[m8b[b]])
                S.op(S.dve, lambda: nc.vector.tensor_scalar(out=m8[b][:, 8:9], in0=m8[b][:, 0:1], scalar1=-1.0, scalar2=None, op0=ALU.mult),
                     reads=[m8b[b]], writes=[m8b[b]])
                S.op(S.dve, lambda: nc.vector.tensor_scalar(out=mk[b][:], in0=lg[b][:], scalar1=m8[b][:, 3:4], scalar2=None, op0=ALU.is_ge),
                     reads=[lgb[b], m8b[b]], writes=[mkb[b]])
                S.op(S.act, lambda: nc.scalar.activation(out=ex[b][:], in_=lg[b][:], func=AF.Exp, bias=m8[b][:, 8:9], scale=1.0),
                     reads=[lgb[b], m8b[b]], writes=[exb[b]])
                S.op(S.dve, lambda: nc.vector.tensor_tensor(out=ex[b][:], in0=ex[b][:], in1=mk[b][:], op=ALU.mult),
                     reads=[exb[b], mkb[b]], writes=[exb[b]])
                S.op(S.dve, lambda: nc.vector.reduce_sum(out=m8[b][:, 9:10], in_=ex[b][:], axis=AX.X), reads=[exb[b]], writes=[m8b[b]])
                S.op(S.dve, lambda: nc.vector.reciprocal(out=m8[b][:, 10:11], in_=m8[b][:, 9:10]), reads=[m8b[b]], writes=[m8b[b]])
                S.op(S.dve, lambda: nc.vector.tensor_scalar(out=cm[b][:], in0=ex[b][:], scalar1=m8[b][:, 10:11], scalar2=None, op0=ALU.mult),
                     reads=[exb[b], m8b[b]], writes=[cmb[b]])
                S.op(S.act, lambda: nc.scalar.activation(out=cmh[b][:], in_=cm[b][:], func=AF.Copy), reads=[cmb[b]], writes=[cmhb[b]])
                if "combd" in self.debug:
                    S.dma(S.sp, self.combd[t * 128:(t + 1) * 128, :], cm[b][:], owner=cmb[b], reads=[cmb[b]], writes=[dd["combd"]])
                p_t = 2 + b
                pv = self.ps[p_t][:].bitcast(BF16)
                S.op(S.pe, lambda: nc.tensor.transpose(out=pv[0:NEXP, 0:128], in_=cmh[b][:], identity=self.ident_bf[:]),
                     reads=[cmhb[b], self.cb], writes=[self.psb[p_t]])
                S.op(S.act, lambda: nc.scalar.activation(out=self.combT[:, t * 128:(t + 1) * 128], in_=pv[0:NEXP, 0:128], func=AF.Copy),
                     reads=[self.psb[p_t]], writes=[self.combTb])
            S.dma(S.sp, self.combTd, self.combT[:], owner=self.combTb, reads=[self.combTb], writes=[dd["combTd"]])
            S.dma(S.sp, self.h2d, self.HT[:, :, 1024:2048], owner=self.HTb[0], reads=self.HTb, writes=[dd["h2d"]])
            S.barrier()

    def p9_moe(self):
        nc, S = self.nc, self.S
        NE = self.n_exp
        with ExitStack() as st:
            sb = self.sb
            dd = self.dbufs
            w1t = [sb(st, f"m_w1{i}", [128, KC, 2, 128], BF16) for i in range(2)]; w1b = [Buf(f"m_w1{i}") for i in range(2)]
            selb = Buf("m_sel")
            b1t = sb(st, "m_b1", [128, NE * 16], F32)
            cbe = [sb(st, f"m_cbe{i}", [128, 1024], BF16) for i in range(2)]; cbeb = [Buf(f"m_cbe{i}") for i in range(2)]
            gt = [sb(st, f"m_g{i}", [128, 512], F32) for i in range(2)]; gtb = [Buf(f"m_g{i}") for i in range(2)]
            sg = [sb(st, f"m_s{i}", [128, 512], F32) for i in range(2)]; sgb = [Buf(f"m_s{i}") for i in range(2)]
            ut = [sb(st, f"m_u{i}", [128, 512], F32) for i in range(2)]; utb = [Buf(f"m_u{i}") for i in range(2)]
            yt = [sb(st, f"m_y{i}", [128, 512], F32) for i in range(4)]; ytb = [Buf(f"m_y{i}") for i in range(4)]
            xr = [sb(st, f"m_x{i}", [128, 512], F32) for i in range(2)]; xrb = [Buf(f"m_x{i}") for i in range(2)]
            gf = [sb(st, f"m_gf{i}", [128, 512], F32) for i in range(2)]; gfb = [Buf(f"m_gf{i}") for i in range(2)]
            b2t = [sb(st, f"m_b2{i}", [32, 512], BF16) for i in range(2)]; b2b = [Buf(f"m_b2{i}") for i in range(2)]
            S.dma(S.sp, b1t[:], self.b1T, owner=selb, writes=[selb])
            actT = self.HT[:, 0:16, 1024:2048]
            actb = [Buf("m_act0"), Buf("m_act1")]
            w2t = [self.HT[:, 16:32, 1024:1536], self.HT[:, 16:32, 1536:2048]]
            w2b = [Buf("m_w20"), Buf("m_w21")]
            hb = [Buf("m_h2")]
            ngrp = NE // 2
            cnt = dict(w1=0, blk=0, w2=0, y=0)
            for half in range(2):
                tk0 = half * 1024
                if half == 1:
                    S.dma(S.sp, self.HT[:, :, 0:1024], self.h2d, owner=hb[0], reads=[dd["h2d"]], writes=hb)
                for grp in range(ngrp):
                    for ei in range(2):
                        e = grp * 2 + ei
                        S.dma(S.sp, cbe[ei][:], self.combTd[e, tk0:tk0 + 1024].partition_broadcast(128), owner=cbeb[ei],
                              reads=[dd["combTd"]], writes=[cbeb[ei]])
                        for fb in range(8):
                            wb_ = cnt["w1"] % 2; cnt["w1"] += 1
                            S.dma(S.pool, w1t[wb_][:].rearrange("p k g c -> p (k g c)"), self.w1[e, fb], owner=w1b[wb_], writes=[w1b[wb_]])
                            for tb_ in range(2):
                                i2 = cnt["blk"] % 2; cnt["blk"] += 1
                                pg, pu = (0, 1) if i2 == 0 else (2, 3)
                                rhs_t = slice(tb_ * 512, (tb_ + 1) * 512)
                                self.mm(self.ps[pg][:], self.psb[pg],
                                        [(w1t[wb_][:, kc, 0, :], self.HT[:, kc, rhs_t]) for kc in range(KC)], reads=[w1b[wb_]] + hb)
                                self.mm(self.ps[pu][:], self.psb[pu],
                                        [(w1t[wb_][:, kc, 1, :], self.HT[:, kc, rhs_t]) for kc in range(KC)], reads=[w1b[wb_]] + hb)
                                bg = b1t[:, e * 16 + fb:e * 16 + fb + 1]
                                bu = b1t[:, e * 16 + 8 + fb:e * 16 + 8 + fb + 1]
                                S.op(S.dve, lambda: nc.vector.tensor_scalar(out=gt[i2][:], in0=self.ps[pg][:], scalar1=bg, scalar2=7.0,
                                                                            op0=ALU.add, op1=ALU.min), reads=[self.psb[pg], selb], writes=[gtb[i2]])
                                S.op(S.act, lambda: nc.scalar.activation(out=sg[i2][:], in_=gt[i2][:], func=AF.Sigmoid, scale=1.702),
                                     reads=[gtb[i2]], writes=[sgb[i2]])
                                S.op(S.dve, lambda: nc.vector.tensor_scalar(out=ut[i2][:], in0=self.ps[pu][:], scalar1=bu, scalar2=7.0,
                                                                            op0=ALU.add, op1=ALU.min), reads=[self.psb[pu], selb], writes=[utb[i2]])
                                S.op(S.dve, lambda: nc.vector.tensor_scalar(out=ut[i2][:], in0=ut[i2][:], scalar1=-7.0, scalar2=1.0,
                                                                            op0=ALU.max, op1=ALU.add), reads=[utb[i2]], writes=[utb[i2]])
                                S.op(S.dve, lambda: nc.vector.tensor_tensor(out=gt[i2][:], in0=gt[i2][:], in1=sg[i2][:], op=ALU.mult),
                                     reads=[gtb[i2], sgb[i2]], writes=[gtb[i2]])
                                S.op(S.dve, lambda: nc.vector.tensor_tensor(out=gt[i2][:], in0=gt[i2][:], in1=ut[i2][:], op=ALU.mult),
                                     reads=[gtb[i2], utb[i2]], writes=[gtb[i2]])
                                S.op(S.dve, lambda: nc.vector.tensor_tensor(out=actT[:, ei * 8 + fb, rhs_t], in0=gt[i2][:],
                                                                            in1=cbe[ei][:, rhs_t], op=ALU.mult),
                                     reads=[gtb[i2], cbeb[ei]], writes=[actb[ei]])
                    first, last = (grp == 0), (grp == ngrp - 1)
                    for nb in range(8):
                        wb_ = cnt["w2"] % 2; cnt["w2"] += 1
                        c0 = nb * 512
                        for ei in range(2):
                            e = grp * 2 + ei
                            S.dma(S.pool, w2t[wb_][:, ei * 8:(ei + 1) * 8, :], self.w2[e, nb].rearrange("p (k c) -> p k c", k=8),
                                  owner=w2b[wb_], writes=[w2b[wb_]])
                        if first:
                            S.dma(S.pool, b2t[wb_][:], self.b2[:, c0:c0 + 512], owner=b2b[wb_], writes=[b2b[wb_]])
                        if last:
                            S.dma(S.sp, gf[wb_][:], self.modd[5 * D + c0:5 * D + c0 + 512].partition_broadcast(128), owner=gfb[wb_],
                                  reads=[dd["modd"]], writes=[gfb[wb_]])
                        for t in range(8):
                            r0 = tk0 + t * 128
                            i3 = cnt["y"] % 4; i2 = cnt["y"] % 2; cnt["y"] += 1
                            pi = 6 + i2
                            if not first:
                                S.dma(S.act, yt[i3][:], self.yd[r0:r0 + 128, c0:c0 + 512], owner=ytb[i3], reads=[dd["yd"]], writes=[ytb[i3]])
                            if last:
                                S.dma(S.act, xr[i2][:], self.x1[r0:r0 + 128, c0:c0 + 512], owner=xrb[i2], reads=[dd["x1"]], writes=[xrb[i2]])
                            pairs = [(actT[:, kc, t * 128:(t + 1) * 128], w2t[wb_][:, kc, :]) for kc in range(16)]
                            rd = actb + [w2b[wb_]]
                            if first:
                                pairs.append((self.combT[:, r0:r0 + 128], b2t[wb_][:]))
                                rd = rd + [self.combTb, b2b[wb_]]
                            self.mm(self.ps[pi][:], self.psb[pi], pairs, reads=rd)
                            if first:
                                S.op(S.act, lambda: nc.scalar.activation(out=yt[i3][:], in_=self.ps[pi][:], func=AF.Copy),
                                     reads=[self.psb[pi]], writes=[ytb[i3]])
                            else:
                                S.op(S.dve, lambda: nc.vector.tensor_tensor(out=yt[i3][:], in0=self.ps[pi][:], in1=yt[i3][:], op=ALU.add),
                                     reads=[self.psb[pi], ytb[i3]], writes=[ytb[i3]])
                            if not last:
                                S.dma(S.sp, self.yd[r0:r0 + 128, c0:c0 + 512], yt[i3][:], owner=ytb[i3], reads=[ytb[i3]], writes=[dd["yd"]])
                            else:
                                S.op(S.dve, lambda: nc.vector.tensor_tensor(out=yt[i3][:], in0=yt[i3][:], in1=gf[wb_][:], op=ALU.mult),
                                     reads=[ytb[i3], gfb[wb_]], writes=[ytb[i3]])
                                S.op(S.dve, lambda: nc.vector.tensor_tensor(out=yt[i3][:], in0=yt[i3][:], in1=xr[i2][:], op=ALU.add),
                                     reads=[ytb[i3], xrb[i2]], writes=[ytb[i3]])
                                S.dma(S.sp, self.out[r0:r0 + 128, c0:c0 + 512], yt[i3][:], owner=ytb[i3], reads=[ytb[i3]], writes=[dd["out"]])
            S.barrier()

    def finish(self):
        pass


def _rope_tables(hf):
    l = np.arange(SEQ, dtype=np.float32)
    pos = l if hf == 0 else (np.float32(SEQ - 1) - l)
    inv = (np.float32(10000.0) ** (-np.arange(0, 128, 2, dtype=np.float32) / np.float32(128))).astype(np.float32)
    ang = pos[:, None] * inv[None, :]
    cos = np.cos(ang).astype(np.float32).T
    sin = np.sin(ang).astype(np.float32).T
    cosT = np.concatenate([cos, cos], 0)
    sinT = np.concatenate([sin, sin], 0)
    return np.ascontiguousarray(cosT), np.ascontiguousarray(sinT)


def _consts():
    r = np.zeros((128, 128), np.float32)
    for m in range(64):
        r[m + 64, m] = -1.0
    for m in range(64, 128):
        r[m - 64, m] = 1.0
    return r, np.eye(128, dtype=np.float32)


def prepare(inputs, names):
    g = lambda k: np.asarray(inputs[k])
    x = g("x") if "x" in inputs else None
    c = g("c") if "c" in inputs else None
    rperm, ident = _consts()
    tabs = [_rope_tables(0), _rope_tables(1)]
    ks = np.float32(128.0 ** -0.5)
    shared = {}
    lazy = {
        "ada_w": lambda: g("ada_w")[0], "ada_b": lambda: g("ada_b"), "w_in": lambda: g("w_in")[0],
        "nw_m": lambda: np.ascontiguousarray(g("norm_mix_w")[0].reshape(KC, 128).T),
        "nw_f": lambda: np.ascontiguousarray(g("norm_ffn_w")[0].reshape(KC, 128).T),
        "qnw": lambda: np.ascontiguousarray(g("q_norm_w")[0].reshape(128, 1)),
        "knw": lambda: np.ascontiguousarray(g("k_norm_w")[0].reshape(128, 1)),
        "rperm": lambda: rperm, "ident": lambda: ident,
        "w_ua": lambda: g("w_up_attn")[0], "w_ur": lambda: g("w_up_ret")[0], "w_out": lambda: g("w_out")[0],
        "router_w": lambda: g("router_w")[0],
        "rb": lambda: np.ascontiguousarray(np.tile(g("router_b")[0][None, :], (128, 1))),
        "w1": lambda: np.ascontiguousarray(g("expert_w1")[0].reshape(-1, KC, 128, 2, 8, 128).transpose(0, 4, 2, 1, 3, 5)
                                           ).reshape(-1, 8, 128, KC * 2 * 128),
        "w2": lambda: np.ascontiguousarray(g("expert_w2")[0].reshape(-1, 8, 128, 8, 512).transpose(0, 3, 2, 1, 4)
                                           ).reshape(-1, 8, 128, 8 * 512),
        "b2": lambda: g("expert_b2")[0],
        "b1T": lambda: np.ascontiguousarray(g("expert_b1")[0].reshape(-1, 16, 128).transpose(2, 0, 1).reshape(128, -1)),
    }
    for k_, f_ in lazy.items():
        if k_ in names:
            shared[k_] = f_()
    def t5_bucket(rel):
        nb, me = 16, 8
        base = np.where(rel > 0, nb, 0)
        n = np.abs(rel)
        nf = np.maximum(n, 1).astype(np.float32)
        large = me + (np.log(nf / me) / math.log(128 / me) * (nb - me)).astype(np.int32)
        large = np.minimum(large, nb - 1)
        return base + np.where(n < me, n, large)
    rb = g("rel_bias") if "rel_bias" in inputs else None
    kk = np.arange(128)[:, None, None]
    kb_ = np.arange(3)[None, :, None]
    qq = np.arange(128)[None, None, :]
    rel_loc = (kb_ - 1) * 128 + kk - qq
    valid = np.abs(rel_loc) <= 128
    jj = np.arange(128, dtype=np.float32)
    cpos = np.maximum(jj[None, :] - jj[:, None], 0).astype(np.float32)
    cneg = np.maximum(jj[:, None] - jj[None, :], 0).astype(np.float32)
    ccol = np.stack([127 - jj, jj], 1).astype(np.float32)
    crow = np.tile(np.concatenate([jj + 1, 128 - jj])[None, :], (128, 1)).astype(np.float32)
    if "gnw" in names:
        shared.update({
            "cpos": cpos, "cneg": cneg, "ccol": ccol, "crow": crow,
            "gnw": np.ascontiguousarray(np.tile(g("ret_gn_w")[0][None, :], (128, 1))),
            "gnb": np.ascontiguousarray(np.tile(g("ret_gn_b")[0][None, :], (128, 1))),
            "sinkr": np.ascontiguousarray(np.tile(g("attn_sink")[0][None, :], (128, 1))),
        })
    maps = []
    for b in range(4):
        m = dict(shared)
        if "cT" in names:
            m["cT"] = np.ascontiguousarray(c[b].reshape(KC, 128).T)
        for hf in (0, 1):
            sfx = str(hf)
            xl = None if x is None else (x[b] if hf == 0 else x[b][::-1])
            cosT, sinT = tabs[hf]
            if "x_own" + sfx in names:
                m["x_own" + sfx] = np.ascontiguousarray(xl[:TOK])
            if "x_oth" + sfx in names:
                m["x_oth" + sfx] = np.ascontiguousarray(xl[TOK:])
            if "cosq" + sfx in names:
                m["cosq" + sfx] = np.ascontiguousarray(cosT[:, :TOK])
                m["sinq" + sfx] = np.ascontiguousarray(sinT[:, :TOK])
            if "cosk" + sfx in names:
                m["cosk" + sfx] = cosT * ks
                m["sink" + sfx] = sinT * ks
            if "biasT" + sfx in names:
                rel_o = rel_loc if hf == 0 else -rel_loc
                bt = rb[t5_bucket(rel_o)]
                bt = np.where(valid[..., None], bt, np.float32(-1e30)).astype(np.float32)
                bt = bt.reshape(128, 3, 128, 4, 4).transpose(3, 0, 1, 4, 2)
                m["biasT" + sfx] = np.ascontiguousarray(bt.reshape(4, 128, 1536))
                df, db = g("ret_decay_fwd")[0], g("ret_decay_bwd")[0]
                if hf == 1:
                    df, db = db, df
                m["dec_f" + sfx] = np.ascontiguousarray(np.tile(df[None, :], (128, 1)))
                m["dec_b" + sfx] = np.ascontiguousarray(np.tile(db[None, :], (128, 1)))
        for k in names:
            if k in inputs and k not in m:
                m[k] = inputs[k][b] if isinstance(inputs[k], (list, tuple)) else inputs[k]
        maps.append({k: m[k] for k in names})
    return maps


_NC_CACHE = {}


def kernel(**inputs):
    if "k" not in _NC_CACHE:
        k = K(halves=(0,))
        k.build()
        _NC_CACHE["k"] = k
    k = _NC_CACHE["k"]
    names0 = list(k.inputs.keys())
    per_half = [n[:-1] for n in names0 if n.endswith("0") and n[:-1] in K.USED]
    names_all = [n for n in names0 if not (n.endswith("0") and n[:-1] in K.USED)]
    names_all += [p + s for p in per_half for s in ("0", "1")]
    bmaps = prepare(inputs, names_all)
    maps = []
    for i in range(8):
        b, hf = i // 2, i % 2
        m = {}
        for n in names0:
            if n.endswith("0") and n[:-1] in K.USED:
                m[n] = bmaps[b][n[:-1] + str(hf)]
            else:
                m[n] = bmaps[b][n]
        maps.append(m)
    del bmaps
    res = run_bass_kernel_spmd(k.nc, maps, core_ids=list(range(8)))
    del maps
    out = np.empty((4, SEQ, D), np.float32)
    for i in range(8):
        b, hf = i // 2, i % 2
        o = np.asarray(res.results[i]["out0"])
        if hf == 0:
            out[b, :TOK] = o
        else:
            out[b, TOK:] = o[::-1]
    return out
```
